# Optimizing a Trainium2 kernel written in Bass

```python
import math
import jax, jax.numpy as jnp
from jax import lax
import numpy as np

D_MODEL = 2048
BATCH = 2
SEQ = 4096
DEPTH = 1

CHUNK = 64
BLK = 128
HEAD_DIM = 64
A_HEADS = 16
A_KV_HEADS = 2
A_REP = A_HEADS // A_KV_HEADS
WINDOW = 128
WIN_CHUNKS = WINDOW // CHUNK
B_HEADS = 16
A_WIDTH = A_HEADS * HEAD_DIM
A_KV_WIDTH = A_KV_HEADS * HEAD_DIM
B_WIDTH = B_HEADS * HEAD_DIM
IN_SIZES = (A_WIDTH, A_KV_WIDTH, A_KV_WIDTH, B_WIDTH, B_WIDTH, B_WIDTH, D_MODEL, D_MODEL)
N_IN = sum(IN_SIZES)
IN_CUTS = tuple(int(c) for c in np.cumsum(IN_SIZES)[:-1])
REL_BUCKETS = 32
REL_MAX_DIST = 128
MEM_LEN = 256
X_HEADS = 4
X_HEAD_DIM = D_MODEL // X_HEADS
N_EXPERTS = 32
TOP_K = 4
D_FF = D_MODEL
SWIGLU_LIMIT = 7.0
SWIGLU_ALPHA = 1.702
EXPERT_BLOCK = 128
LN_EPS = 1e-5
DEEPNORM_ALPHA = (2.0 * DEPTH) ** 0.25
DEEPNORM_BETA = (8.0 * DEPTH) ** -0.25

kernel_name = "hybrid_swa_stickbreaking_moe_streaming_block"


def layer_norm(x, g, b):
    xf = x.astype(jnp.float32)
    mu = xf.mean(-1, keepdims=True)
    var = jnp.square(xf - mu).mean(-1, keepdims=True)
    y = (xf - mu) * lax.rsqrt(var + LN_EPS)
    return (y * g.astype(jnp.float32) + b.astype(jnp.float32)).astype(x.dtype)


def t5_bucket(rel):
    half = REL_BUCKETS // 2
    max_exact = half // 2
    base = jnp.where(rel > 0, half, 0)
    n = jnp.abs(rel)
    nf = jnp.maximum(n, 1).astype(jnp.float32)
    large = max_exact + (jnp.log(nf / max_exact) / math.log(REL_MAX_DIST / max_exact)
                         * (half - max_exact)).astype(jnp.int32)
    large = jnp.minimum(large, half - 1)
    return base + jnp.where(n < max_exact, n, large)


def sliding_window_attn(q, k, v, sinks, rel_bias):
    b_, s_ = q.shape[0], q.shape[1]
    nb = s_ // BLK
    qb = q.reshape(b_, nb, BLK, A_KV_HEADS, A_REP, HEAD_DIM)
    kb = k.reshape(b_, nb, BLK, A_KV_HEADS, HEAD_DIM)
    vb = v.reshape(b_, nb, BLK, A_KV_HEADS, HEAD_DIM)

    def with_prev(t):
        prev = jnp.concatenate([jnp.zeros_like(t[:, :1]), t[:, :-1]], axis=1)
        return jnp.concatenate([prev, t], axis=2)

    kk, vv = with_prev(kb), with_prev(vb)
    logits = jnp.einsum('bnqgrd,bnkgd->bngrqk', qb, kk).astype(jnp.float32) * (HEAD_DIM ** -0.5)
    qi = jnp.arange(BLK)[:, None]
    kj = jnp.arange(2 * BLK)[None, :]
    bias = rel_bias[t5_bucket(kj - BLK - qi)]
    bias = jnp.transpose(bias, (2, 0, 1)).reshape(A_KV_HEADS, A_REP, BLK, 2 * BLK).astype(jnp.float32)
    dchunk = (kj // CHUNK - BLK // CHUNK) - qi // CHUNK
    band = (dchunk <= 0) & (dchunk >= -WIN_CHUNKS)
    key_exists = (jnp.arange(nb)[:, None] > 0) | (jnp.arange(2 * BLK)[None, :] >= BLK)
    mask = band[None] & key_exists[:, None, :]
    logits = jnp.where(mask[None, :, None, None], logits + bias, -jnp.inf)
    sink = sinks.astype(jnp.float32).reshape(1, 1, A_KV_HEADS, A_REP, 1, 1)
    m = jnp.maximum(logits.max(-1, keepdims=True), sink)
    p = jnp.exp(logits - m)
    p = p / (p.sum(-1, keepdims=True) + jnp.exp(sink - m))
    out = jnp.einsum('bngrqk,bnkgd->bnqgrd', p.astype(v.dtype), vv)
    return out.reshape(b_, s_, A_WIDTH)


def stick_breaking_attn(q, k, v):
    b_, s_ = q.shape[0], q.shape[1]
    nb = s_ // BLK
    qb = jnp.moveaxis(q.reshape(b_, nb, BLK, B_HEADS, HEAD_DIM), 1, 0)
    starts = jnp.arange(nb) * BLK
    key_pos = jnp.arange(s_)
    scale = HEAD_DIM ** -0.5

    def block(args):
        qblk, start = args
        z = jnp.einsum('bqhd,bkhd->bhqk', qblk, k).astype(jnp.float32) * scale
        causal = key_pos[None, :] < (start + jnp.arange(BLK))[:, None]
        log_keep = jnp.where(causal, jax.nn.log_sigmoid(-z), 0.0)
        suffix = lax.cumsum(log_keep, axis=3, reverse=True)
        log_w = jax.nn.log_sigmoid(z) + (suffix - log_keep)
        w = jnp.where(causal, jnp.exp(log_w), 0.0).astype(v.dtype)
        return jnp.einsum('bhqk,bkhd->bqhd', w, v)

    out = lax.map(block, (qb, starts))
    return jnp.moveaxis(out, 0, 1).reshape(b_, s_, B_WIDTH)


def memory_cross_attn(h, mem, w_q, w_kv, w_o):
    b_, s_, _ = h.shape
    q = (h @ w_q).reshape(b_, s_, X_HEADS, X_HEAD_DIM)
    k, v = jnp.split(mem @ w_kv, 2, axis=-1)
    k = k.reshape(b_, MEM_LEN, X_HEADS, X_HEAD_DIM)
    v = v.reshape(b_, MEM_LEN, X_HEADS, X_HEAD_DIM)
    logits = jnp.einsum('bqhd,bkhd->bhqk', q, k).astype(jnp.float32) * (X_HEAD_DIM ** -0.5)
    p = jax.nn.softmax(logits, axis=-1).astype(v.dtype)
    o = jnp.einsum('bhqk,bkhd->bqhd', p, v).reshape(b_, s_, D_MODEL)
    return o @ w_o


def moe_ffn(x2d, w_router, b_router, w_up, b_up, w_down, b_down):
    t_ = x2d.shape[0]
    logits = (x2d @ w_router + b_router).astype(jnp.float32)
    top_val, top_idx = lax.top_k(logits, TOP_K)
    gate = jax.nn.softmax(top_val, axis=-1)
    n_pairs = t_ * TOP_K
    pair_expert = top_idx.reshape(n_pairs)
    pair_token = jnp.arange(n_pairs) // TOP_K
    pair_gate = gate.reshape(n_pairs)
    order = jnp.argsort(pair_expert)
    sorted_e = pair_expert[order]
    counts = jnp.zeros((N_EXPERTS,), jnp.int32).at[pair_expert].add(1)
    padded = (counts + EXPERT_BLOCK - 1) // EXPERT_BLOCK * EXPERT_BLOCK
    offset = jnp.cumsum(counts) - counts
    pad_end = jnp.cumsum(padded)
    pad_offset = pad_end - padded
    dest = pad_offset[sorted_e] + (jnp.arange(n_pairs) - offset[sorted_e])
    n_rows = n_pairs + N_EXPERTS * EXPERT_BLOCK
    n_blocks = n_rows // EXPERT_BLOCK
    row_pair = jnp.zeros((n_rows,), jnp.int32).at[dest].set(order.astype(jnp.int32))
    row_valid = jnp.zeros((n_rows,), bool).at[dest].set(True)
    row_token = pair_token[row_pair]
    row_gate = jnp.where(row_valid, pair_gate[row_pair], 0.0)
    block_expert = jnp.minimum(
        jnp.searchsorted(pad_end, jnp.arange(n_blocks) * EXPERT_BLOCK, side='right'), N_EXPERTS - 1)

    def expert_block(args):
        tok, e = args
        hid = x2d[tok] @ w_up[e] + b_up[e]
        glu, lin = hid[:, :D_FF], hid[:, D_FF:]
        glu = jnp.minimum(glu, SWIGLU_LIMIT)
        lin = jnp.clip(lin, -SWIGLU_LIMIT, SWIGLU_LIMIT)
        act = glu * jax.nn.sigmoid(SWIGLU_ALPHA * glu) * (lin + 1.0)
        return act @ w_down[e] + b_down[e]

    y_rows = lax.map(expert_block, (row_token.reshape(n_blocks, EXPERT_BLOCK), block_expert))
    y_rows = y_rows.reshape(n_rows, D_MODEL)
    return jnp.zeros_like(x2d).at[row_token].add(y_rows * row_gate[:, None].astype(y_rows.dtype))


def setup_inputs(seed: int = 0) -> dict:
    key = jax.random.key(seed)
    ks = jax.random.split(key, 26)
    f32 = jnp.float32

    def nrm(k, shape, scale):
        return jax.random.normal(k, shape, f32) * scale

    L, D, F, G = DEPTH, D_MODEL, D_FF, N_EXPERTS
    beta = DEEPNORM_BETA
    return {
        "x": nrm(ks[0], (BATCH, SEQ, D), 1.0),
        "mem": nrm(ks[1], (BATCH, MEM_LEN, D), 1.0),
        "ln_in_g": 1.0 + nrm(ks[2], (D,), 0.02),
        "ln_in_b": nrm(ks[3], (D,), 0.02),
        "rel_bias": nrm(ks[4], (REL_BUCKETS, A_HEADS), 0.5),
        "w_in": nrm(ks[5], (L, D, N_IN), D ** -0.5),
        "b_in": nrm(ks[6], (L, N_IN), 0.02),
        "attn_sinks": nrm(ks[7], (L, A_HEADS), 0.5),
        "w_a_out": nrm(ks[8], (L, A_WIDTH, D), A_WIDTH ** -0.5),
        "w_b_out": nrm(ks[9], (L, B_WIDTH, D), B_WIDTH ** -0.5),
        "w_mix_out": nrm(ks[10], (L, D, D), beta * D ** -0.5),
        "ln1_g": 1.0 + nrm(ks[11], (L, D), 0.02),
        "ln1_b": nrm(ks[12], (L, D), 0.02),
        "w_xq": nrm(ks[13], (L, D, D), D ** -0.5),
        "w_xkv": nrm(ks[14], (L, D, 2 * D), D ** -0.5),
        "w_xo": nrm(ks[15], (L, D, D), beta * D ** -0.5),
        "ln2_g": 1.0 + nrm(ks[16], (L, D), 0.02),
        "ln2_b": nrm(ks[17], (L, D), 0.02),
        "w_router": nrm(ks[18], (L, D, G), D ** -0.5),
        "b_router": nrm(ks[19], (L, G), 0.01),
        "w_up": nrm(ks[20], (L, G, D, 2 * F), D ** -0.5),
        "b_up": nrm(ks[21], (L, G, 2 * F), 0.02),
        "w_down": nrm(ks[22], (L, G, F, D), beta * F ** -0.5),
        "b_down": nrm(ks[23], (L, G, D), 0.02),
        "ln3_g": 1.0 + nrm(ks[24], (L, D), 0.02),
        "ln3_b": nrm(ks[25], (L, D), 0.02),
    }


def reference(x, mem, ln_in_g, ln_in_b, rel_bias, w_in, b_in, attn_sinks, w_a_out, w_b_out,
              w_mix_out, ln1_g, ln1_b, w_xq, w_xkv, w_xo, ln2_g, ln2_b, w_router, b_router,
              w_up, b_up, w_down, b_down, ln3_g, ln3_b):
    b_, s_, _ = x.shape
    h = layer_norm(x, ln_in_g, ln_in_b)
    for l in range(DEPTH):
        proj = h @ w_in[l] + b_in[l]
        qa, ka, va, qb, kb, vb, ga, gb = jnp.split(proj, IN_CUTS, axis=-1)
        ya = sliding_window_attn(qa.reshape(b_, s_, A_HEADS, HEAD_DIM),
                                 ka.reshape(b_, s_, A_KV_HEADS, HEAD_DIM),
                                 va.reshape(b_, s_, A_KV_HEADS, HEAD_DIM),
                                 attn_sinks[l], rel_bias) @ w_a_out[l]
        yb = stick_breaking_attn(qb.reshape(b_, s_, B_HEADS, HEAD_DIM),
                                 kb.reshape(b_, s_, B_HEADS, HEAD_DIM),
                                 vb.reshape(b_, s_, B_HEADS, HEAD_DIM)) @ w_b_out[l]
        mixed = (jax.nn.sigmoid(ga) * ya + jax.nn.sigmoid(gb) * yb) @ w_mix_out[l]
        h = layer_norm(DEEPNORM_ALPHA * h + mixed, ln1_g[l], ln1_b[l])
        xo = memory_cross_attn(h, mem, w_xq[l], w_xkv[l], w_xo[l])
        h = layer_norm(DEEPNORM_ALPHA * h + xo, ln2_g[l], ln2_b[l])
        ff = moe_ffn(h.reshape(b_ * s_, D_MODEL), w_router[l], b_router[l],
                     w_up[l], b_up[l], w_down[l], b_down[l]).reshape(b_, s_, D_MODEL)
        h = layer_norm(DEEPNORM_ALPHA * h + ff, ln3_g[l], ln3_b[l])
    return h
```

```python
import numpy as np
from contextlib import ExitStack
import concourse.bass as bass
import concourse.mybir as mybir
from concourse.bass_utils import run_bass_kernel_spmd

F32 = mybir.dt.float32
BF16 = mybir.dt.bfloat16
AF = mybir.ActivationFunctionType
ALU = mybir.AluOpType
AX = mybir.AxisListType

D = 2048
NE = 32
CAP = 192
ALPHA = 2.0 ** 0.25
EPS = 1e-5
NEG = -30000.0
ARENA = 206 * 1024


class Sem:
    def __init__(s, h):
        s.h = h
        s.total = 0


class Buf:
    def __init__(s, name, t=None):
        s.name = name
        s.t = t
        s.w = {}
        s.r = {}
        s.dsem = None


class KB:
    def __init__(s, nc, es, ndsem=92):
        s.nc = nc
        s.E = dict(pe=nc.tensor, act=nc.scalar, dve=nc.vector, pool=nc.gpsimd, sp=nc.sync)
        s.sem = {k: es.enter_context(nc.semaphore("es_" + k)) for k in s.E}
        s.cnt = {k: 0 for k in s.E}
        s.known = {k: {} for k in s.E}
        s.free_dsems = [Sem(es.enter_context(nc.semaphore("ds%d" % i))) for i in range(ndsem)]
        s.used_dsems = []
        s.phase_bufs = []
        s.banks = [Buf("bank%d" % i, es.enter_context(nc.psum_tensor("bank%d" % i, [128, 512], F32))) for i in range(8)]
        s.bank_i = 0
        s.es = es
        s.pes = None
        s.arena_bytes = ARENA
        s.arena = es.enter_context(nc.sbuf_tensor("arena", [128, ARENA // 4], F32))
        s.alloc = []

    def bank(s):
        b = s.banks[s.bank_i % 8]
        s.bank_i += 1
        return b

    def sb(s, name, shape, dt, glob=False, grp=None):
        nb = 2 if dt == BF16 else 4
        free = 1
        for d_ in shape[1:]:
            free *= d_
        size = (free * nb + 63) // 64 * 64
        off = None
        cur = 0
        for (o, sz) in sorted(s.alloc):
            if o - cur >= size:
                off = cur
                break
            cur = max(cur, o + sz)
        if off is None:
            if s.arena_bytes - cur >= size:
                off = cur
            else:
                raise AssertionError("arena OOM allocating %s %s (%d B); used=%s" % (name, shape, size, sum(z for _, z in s.alloc)))
        s.alloc.append((off, size))
        ap = s.arena[0:shape[0], off // 4:(off + size) // 4]
        if dt == BF16:
            ap = ap.bitcast(BF16)
        ap = ap[:, 0:free]
        if len(shape) == 3:
            ap = ap.rearrange("p (a b) -> p a b", a=shape[1])
        elif len(shape) == 4:
            ap = ap.rearrange("p (a b c) -> p a b c", a=shape[1], b=shape[2])
        b = Buf(name, ap)
        b.alloc = (off, size)
        if grp is not None:
            grp["bufs"].append(b)
        elif not glob and s.pes is not None:
            s.phase_bufs.append(b)
        return b

    def newgrp(s):
        return dict(bufs=[])

    def freegrp(s, g):
        for b in g["bufs"]:
            s.alloc.remove(b.alloc)
        g["bufs"] = []

    def _wait(s, eng, need):
        E = s.E[eng]
        for key, (h, val) in need.items():
            if s.known[eng].get(key, 0) < val:
                E.wait_ge(h, val)
                s.known[eng][key] = val

    def op(s, eng, fn, reads=(), writes=(), pwrites=()):
        need = {}

        def add(evs, raw):
            for key, (h, val, e) in evs.items():
                if e == eng and (eng == "pe" or not raw):
                    continue
                if key not in need or need[key][1] < val:
                    need[key] = (h, val)
        for b in reads:
            add(b.w, True)
        for b in writes:
            add(b.w, False)
            add(b.r, False)
        for b in pwrites:
            add(b.r, False)
        s._wait(eng, need)
        inst = fn(s.E[eng])
        s.cnt[eng] += 1
        inst.then_inc(s.sem[eng], 1)
        ev = (s.sem[eng], s.cnt[eng], eng)
        for b in reads:
            b.r[eng] = ev
        for b in writes:
            b.w = {eng: ev}
            b.r = {}
        for b in pwrites:
            b.w[eng] = ev

    def dma(s, q, out_ap, in_ap, src, dst, partial=False):
        need = {}

        def add(evs):
            for key, (h, val, e) in evs.items():
                if key not in need or need[key][1] < val:
                    need[key] = (h, val)
        add(src.w)
        add(dst.r)
        if not partial:
            add(dst.w)
        s._wait(q, need)
        inst = s.E[q].dma_start(out=out_ap, in_=in_ap)
        if dst.dsem is None:
            dst.dsem = s.free_dsems.pop()
            s.used_dsems.append((dst, dst.dsem))
        sem = dst.dsem
        sem.total += 16
        inst.then_inc(sem.h, 16)
        key = ("d", id(sem))
        ev = (sem.h, sem.total, None)
        src.r[key] = ev
        if partial:
            dst.w[key] = ev
        else:
            dst.w = {key: ev}
            dst.r = {}

    def barrier(s):
        for eng, E in s.E.items():
            need = {}
            for o in s.E:
                if s.cnt[o] > 0:
                    need[o] = (s.sem[o], s.cnt[o])
            for (b, sem) in s.used_dsems:
                if sem.total > 0:
                    need[("d", id(sem))] = (sem.h, sem.total)
            s._wait(eng, need)
        for (b, sem) in s.used_dsems:
            b.dsem = None
            s.free_dsems.append(sem)
        s.used_dsems = []

    def phase_begin(s):
        s.pes = True
        s.phase_bufs = []

    def phase_end(s):
        s.barrier()
        for b in s.phase_bufs:
            s.alloc.remove(b.alloc)
        s.pes = None
        s.phase_bufs = []


def chunks_of(c_lo, c_hi, step=512):
    out = []
    c = c_lo
    while c < c_hi:
        out.append((c, min(c + step, c_hi)))
        c += step
    return out


class Prog:
    def __init__(self, cfg):
        self.cfg = cfg
        self.nc = bass.Bass("TRN2", target_bir_lowering=False)
        self.ins = {}
        self.outs = {}

    def din(self, name, shape, dt=F32):
        t = self.nc.dram_tensor(name, list(shape), dt, kind="ExternalInput")
        b = Buf(name, t)
        b.ap = t.ap()
        self.ins[name] = b
        return b

    def dout(self, name, shape, dt=F32):
        t = self.nc.dram_tensor(name, list(shape), dt, kind="ExternalOutput")
        b = Buf(name, t)
        b.ap = t.ap()
        self.outs[name] = b
        return b

    def dscr(self, name, shape, dt):
        t = self.nc.dram_tensor(name, list(shape), dt, kind="Internal")
        b = Buf(name, t)
        b.ap = t.ap()
        return b

    def load_consts(self):
        kb = self.kb
        c = self.din("c_ident", [128, 128])
        self.ident_bf = kb.sb("ident_bf", [128, 128], BF16, glob=True)
        kb.dma("pool", self.ident_bf.t[:], c.ap, c, self.ident_bf)
        self.ident_f = kb.sb("ident_f", [128, 128], F32, glob=True)
        kb.dma("sp", self.ident_f.t[:], c.ap, c, self.ident_f)
        c2 = self.din("c_negtri", [128, 128])
        self.negtri = kb.sb("negtri", [128, 128], BF16, glob=True)
        kb.dma("pool", self.negtri.t[:], c2.ap, c2, self.negtri)
        c3 = self.din("c_trix", [128, 128])
        self.trix = kb.sb("trix", [128, 128], BF16, glob=True)
        kb.dma("pool", self.trix.t[:], c3.ap, c3, self.trix)
        self.ones_bf = kb.sb("ones_bf", [128, 128], BF16, glob=True)
        kb.op("dve", lambda e: e.memset(self.ones_bf.t[:], 1.0), writes=[self.ones_bf])
        self.ones_f = kb.sb("ones_f", [128, 128], F32, glob=True)
        kb.op("dve", lambda e: e.memset(self.ones_f.t[:], 1.0), writes=[self.ones_f])
        self.epsc = kb.sb("epsc", [128, 1], F32, glob=True)
        kb.op("dve", lambda e: e.memset(self.epsc.t[:], EPS), writes=[self.epsc])

    def load_rows(self, name_g, name_b):
        kb = self.kb
        g = self.ins[name_g]
        b = self.ins[name_b]
        gr = kb.sb("gr_" + name_g, [128, D], F32)
        br = kb.sb("br_" + name_b, [128, D], F32)
        kb.dma("sp", gr.t[:], g.ap.partition_broadcast(128), g, gr)
        kb.dma("sp", br.t[:], b.ap.partition_broadcast(128), b, br)
        return gr, br

    def make_ln_bufs(self, nset=2):
        kb = self.kb
        sets = []
        for i in range(nset):
            sets.append(dict(
                x=kb.sb("ln_x%d" % i, [128, D], F32),
                xn=kb.sb("ln_xn%d" % i, [128, D], F32),
                hb=kb.sb("ln_hb%d" % i, [128, D], BF16),
                st=kb.sb("ln_st%d" % i, [128, 4, 6], F32),
                mv=kb.sb("ln_mv%d" % i, [128, 2], F32),
                sc=kb.sb("ln_sc%d" % i, [128, 4], F32),
            ))
        self.ln_sets = sets
        self.ln_i = 0

    def ln_core(self, S, gr, br, want_h32, want_hb=True):
        kb = self.kb
        x, xn, hb, st, mv, sc = S["x"], S["xn"], S["hb"], S["st"], S["mv"], S["sc"]
        for j in range(4):
            kb.op("dve", lambda e, j=j: e.bn_stats(out=st.t[:, j, :], in_=x.t[:, j * 512:(j + 1) * 512]),
                  reads=[x], pwrites=[st] if j else (), writes=() if j else [st])
        kb.op("dve", lambda e: e.bn_aggr(out=mv.t[:], in_=st.t[:].rearrange("p a b -> p (a b)")), reads=[st], writes=[mv])
        kb.op("act", lambda e: e.activation(out=sc.t[:, 0:1], in_=mv.t[:, 1:2], func=AF.Ln, bias=self.epsc.t[:, 0:1], scale=1.0),
              reads=[mv, self.epsc], writes=[sc])
        kb.op("act", lambda e: e.activation(out=sc.t[:, 1:2], in_=sc.t[:, 0:1], func=AF.Exp, scale=-0.5),
              reads=[sc], pwrites=[sc])
        kb.op("dve", lambda e: e.tensor_scalar(out=sc.t[:, 2:3], in0=mv.t[:, 0:1], scalar1=sc.t[:, 1:2], scalar2=-1.0,
                                               op0=ALU.mult, op1=ALU.mult), reads=[mv, sc], pwrites=[sc])
        kb.op("act", lambda e: e.activation(out=xn.t[:], in_=x.t[:], func=AF.Identity, bias=sc.t[:, 2:3], scale=sc.t[:, 1:2]),
              reads=[x, sc], writes=[xn])
        kb.op("pool", lambda e: e.tensor_tensor(out=xn.t[:], in0=xn.t[:], in1=gr.t[:], op=ALU.mult), reads=[xn, gr], writes=[xn])
        if want_h32:
            kb.op("dve", lambda e: e.tensor_tensor(out=x.t[:], in0=xn.t[:], in1=br.t[:], op=ALU.add), reads=[xn, br], writes=[x])
            if want_hb:
                kb.op("act", lambda e: e.activation(out=hb.t[:], in_=x.t[:], func=AF.Copy), reads=[x], writes=[hb])
        else:
            kb.op("dve", lambda e: e.tensor_tensor(out=hb.t[:], in0=xn.t[:], in1=br.t[:], op=ALU.add), reads=[xn, br], writes=[hb])

    def to_fm(self, src, dst, off, nchunk=16, src_col0=0, evac_alt=0):
        kb = self.kb
        for h0 in range(0, nchunk, 8):
            n = min(8, nchunk - h0)
            bk = kb.bank()
            bv = bk.t[:].bitcast(BF16)
            for k in range(n):
                kk = h0 + k
                kb.op("pe", lambda e, k=k, kk=kk: e.transpose(out=bv[:, k * 128:(k + 1) * 128],
                                                               in_=src.t[:, src_col0 + kk * 128: src_col0 + (kk + 1) * 128],
                                                               identity=self.ident_bf.t[:]),
                      reads=[src, self.ident_bf], writes=[bk] if k == 0 else (), pwrites=() if k == 0 else [bk])
            srcv = bv[:, 0:n * 128].rearrange("p (a b) -> p a b", a=n)
            dstv = dst.t[:, h0:h0 + n, off:off + 128]
            if (evac_alt + h0 // 8) % 2 == 0:
                kb.op("dve", lambda e: e.tensor_copy(out=dstv, in_=srcv), reads=[bk], pwrites=[dst])
            else:
                kb.op("act", lambda e: e.activation(out=dstv, in_=srcv, func=AF.Copy), reads=[bk], pwrites=[dst])

    def ln_from_dram(self, src_ap, src_buf, gr, br, dst_fm, off, h32_dst=None, h32_buf=None):
        kb = self.kb
        S = self.ln_sets[self.ln_i % len(self.ln_sets)]
        self.ln_i += 1
        kb.dma("sp", S["x"].t[:], src_ap, src_buf, S["x"])
        self.ln_core(S, gr, br, want_h32=h32_dst is not None)
        if h32_dst is not None:
            kb.dma("sp", h32_dst, S["x"].t[:], S["x"], h32_buf, partial=True)
        self.to_fm(S["hb"], dst_fm, off, evac_alt=self.ln_i)

    def load_w(self, dst, w_ap, wbuf, split=1):
        kb = self.kb
        src = w_ap.rearrange("(c p) n -> p c n", p=128)
        nk = src.shape[1]
        step = (nk + split - 1) // split
        for i, k0 in enumerate(range(0, nk, step)):
            k1 = min(nk, k0 + step)
            kb.dma("pool", dst.t[:, k0:k1, :], src[:, k0:k1, :], wbuf, dst, partial=(i > 0))

    def mm_fm(self, bank, w, wcol, xT, tok0, ntok, nk, reads_extra=()):
        kb = self.kb
        for k in range(nk):
            kb.op("pe", lambda e, k=k: e.matmul(bank.t[:, 0:ntok], lhsT=w.t[:, k, wcol:wcol + 128], rhs=xT.t[:, k, tok0:tok0 + ntok],
                                                start=(k == 0), stop=(k == nk - 1)),
                  reads=[w, xT], writes=[bank] if k == 0 else (), pwrites=() if k == 0 else [bank])

    def mm_tm(self, bank, xT, tok0, w, wcol, ncol, nk, bias_row=None, bias_col0=0):
        kb = self.kb
        first = True
        if bias_row is not None:
            kb.op("pe", lambda e: e.matmul(bank.t[:, 0:ncol], lhsT=self.ones_bf.t[0:1, :], rhs=bias_row.t[0:1, bias_col0:bias_col0 + ncol],
                                           start=True, stop=False),
                  reads=[self.ones_bf, bias_row], writes=[bank])
            first = False
        for k in range(nk):
            kb.op("pe", lambda e, k=k, st=(first and k == 0): e.matmul(bank.t[:, 0:ncol], lhsT=xT.t[:, k, tok0:tok0 + 128],
                                                                       rhs=w.t[:, k, wcol:wcol + ncol], start=st, stop=(k == nk - 1)),
                  reads=[w, xT], writes=[bank] if (first and k == 0) else (), pwrites=() if (first and k == 0) else [bank])

    def stage1(self):
        kb = self.kb
        cfg = self.cfg
        x_own, x_prev, x_ctx = self.ins["x_own"], self.ins["x_prev"], self.ins["x_ctx"]
        w_in = self.ins["w_in"]
        QA0, KA0, VA0, QB0, KB0, VB0, GA0, GB0 = 0, 1024, 1152, 1280, 2304, 3328, 4352, 6400
        bcol = self.bcol
        brow = self.brow

        kb.phase_begin()
        gr, br = self.load_rows("ln_in_g", "ln_in_b")
        self.make_ln_bufs(2)
        wk = kb.sb("wk", [128, 16, 1024], BF16)
        wv = kb.sb("wv", [128, 16, 1024], BF16)
        self.load_w(wk, w_in.ap[:, KB0:KB0 + 1024], w_in, split=2)
        self.load_w(wv, w_in.ap[:, VB0:VB0 + 1024], w_in, split=2)
        hTg = [kb.sb("hTg%d" % i, [128, 16, 512], BF16) for i in range(2)]
        kst = [kb.sb("kst%d" % i, [128, 8, 512], BF16) for i in range(2)]
        vst = [kb.sb("vst%d" % i, [128, 4, 1024], BF16) for i in range(2)]
        kT_scr, V_scr = self.kT_scr, self.V_scr
        for g in range(8):
            hT = hTg[g % 2]
            for tt in range(4):
                r0 = g * 512 + tt * 128
                self.ln_from_dram(x_ctx.ap[r0:r0 + 128, :], x_ctx, gr, br, hT, tt * 128)
            ks = kst[g % 2]
            for fc in range(8):
                bk = kb.bank()
                self.mm_fm(bk, wk, fc * 128, hT, 0, 512, 16)
                kb.op("act", lambda e, fc=fc, bk=bk: e.activation(out=ks.t[:, fc, :], in_=bk.t[:, :], func=AF.Identity,
                                                                   bias=bcol.t[:, 18 + fc:19 + fc], scale=1.0),
                      reads=[bk, bcol], pwrites=[ks] if fc else (), writes=() if fc else [ks])
            kb.dma("sp", kT_scr.ap.rearrange("f p t -> p f t")[:, :, g * 512:(g + 1) * 512], ks.t[:], ks, kT_scr, partial=True)
            vs = vst[g % 2]
            for tt in range(4):
                for nch in range(2):
                    bk = kb.bank()
                    self.mm_tm(bk, hT, tt * 128, wv, nch * 512, 512, 16, bias_row=brow, bias_col0=128 + nch * 512)
                    kb.op("dve", lambda e, tt=tt, nch=nch, bk=bk: e.tensor_copy(out=vs.t[:, tt, nch * 512:(nch + 1) * 512], in_=bk.t[:, :]),
                          reads=[bk], pwrites=[vs] if (tt or nch) else (), writes=() if (tt or nch) else [vs])
            kb.dma("sp", V_scr.ap[g * 4:(g + 1) * 4].rearrange("t p n -> p t n"), vs.t[:], vs, V_scr, partial=True)
        kb.phase_end()

        kb.phase_begin()
        gr, br = self.load_rows("ln_in_g", "ln_in_b")
        self.make_ln_bufs(2)
        P = {}
        self.P1 = P
        self.gA = kb.newgrp(); self.gQB = kb.newgrp(); self.gSWA = kb.newgrp(); self.gT = kb.newgrp()
        hT_own = kb.sb("hT_own", [128, 16, 1024], BF16, grp=self.gA)
        P["hT_own"] = hT_own
        gPrev = kb.newgrp()
        hT_prev = kb.sb("hT_prev", [128, 16, 1024], BF16, grp=gPrev)
        for i in range(8):
            self.ln_from_dram(x_own.ap[i * 128:(i + 1) * 128, :], x_own, gr, br, hT_own, i * 128,
                              h32_dst=self.h0_scr.ap[i * 128:(i + 1) * 128, :], h32_buf=self.h0_scr)
        for i in range(8):
            self.ln_from_dram(x_prev.ap[i * 128:(i + 1) * 128, :], x_prev, gr, br, hT_prev, i * 128)
        kb.phase_end()
        kb.phase_begin()
        qBT = kb.sb("qBT", [128, 8, 1024], BF16, grp=self.gQB)
        qAT = kb.sb("qAT", [128, 8, 1024], BF16, grp=self.gSWA)
        kAT = kb.sb("kAT", [128, 2, 1024], BF16, grp=self.gSWA)
        vA = kb.sb("vA", [128, 2, 8, 128], BF16, grp=self.gSWA)
        P.update(qBT=qBT, qAT=qAT, kAT=kAT, vA=vA)
        wq = kb.sb("wq", [128, 16, 1024], BF16)
        self.load_w(wq, w_in.ap[:, QB0:QB0 + 1024], w_in, split=2)
        for fc in range(8):
            for th in range(2):
                bk = kb.bank()
                self.mm_fm(bk, wq, fc * 128, hT_own, th * 512, 512, 16)
                kb.op("dve", lambda e, fc=fc, th=th, bk=bk: e.tensor_scalar(out=qBT.t[:, fc, th * 512:(th + 1) * 512], in0=bk.t[:, :],
                                                                           scalar1=bcol.t[:, 10 + fc:11 + fc], scalar2=0.125,
                                                                           op0=ALU.add, op1=ALU.mult),
                      reads=[bk, bcol], pwrites=[qBT])
        wqa = wq
        wqa_t = wq.t[:].rearrange("p c (j m) -> p c j m", m=128)
        for a in range(2):
            src = w_in.ap[:, QA0 + a * 512:QA0 + (a + 1) * 512].rearrange("(c p) (j d) -> p c j d", p=128, d=64)
            for c0 in range(16):
                kb.dma("pool", wqa_t[:, c0, :, a * 64:(a + 1) * 64], src[:, c0], w_in, wqa, partial=not (a == 0 and c0 == 0))
        wqa_v = Buf("wqa_v", None)
        for fc in range(8):
            for th in range(2):
                bk = kb.bank()
                for k in range(16):
                    kb.op("pe", lambda e, k=k, fc=fc, th=th, bk=bk: e.matmul(bk.t[:, :], lhsT=wqa_t[:, k, fc, :], rhs=hT_own.t[:, k, th * 512:(th + 1) * 512],
                                                                             start=(k == 0), stop=(k == 15)),
                          reads=[wqa, hT_own], writes=[bk] if k == 0 else (), pwrites=() if k == 0 else [bk])
                kb.op("dve", lambda e, fc=fc, th=th, bk=bk: e.tensor_scalar(out=qAT.t[:, fc, th * 512:(th + 1) * 512], in0=bk.t[:, :],
                                                                           scalar1=self.bcol_qa.t[:, fc:fc + 1], scalar2=0.125,
                                                                           op0=ALU.add, op1=ALU.mult),
                      reads=[bk, self.bcol_qa], pwrites=[qAT])
        wkv = kb.sb("wkv", [128, 16, 256], BF16)
        self.load_w(wkv, w_in.ap[:, KA0:KA0 + 256], w_in)
        for which, hT in ((0, hT_prev), (1, hT_own)):
            for th in range(2):
                bk = kb.bank()
                self.mm_fm(bk, wkv, 0, hT, th * 512, 512, 16)
                kb.op("act", lambda e, th=th, bk=bk, which=which: e.activation(out=kAT.t[:, which, th * 512:(th + 1) * 512], in_=bk.t[:, :],
                                                                             func=AF.Identity, bias=bcol.t[:, 8:9], scale=1.0),
                      reads=[bk, bcol], pwrites=[kAT])
            for i in range(8):
                bk = kb.bank()
                self.mm_tm(bk, hT, i * 128, wkv, 128, 128, 16, bias_row=brow, bias_col0=0)
                kb.op("dve", lambda e, i=i, bk=bk, which=which: e.tensor_copy(out=vA.t[:, which, i, :], in_=bk.t[:, 0:128]),
                      reads=[bk], pwrites=[vA])
        kb.phase_end()
        kb.freegrp(gPrev)

        kb.phase_begin()
        swab = kb.sb("swab", [128, 16, 256], F32)
        kb.dma("sp", swab.t[:], self.ins["swab"].ap, self.ins["swab"], swab)
        pm0 = kb.sb("pm0", [128, 256], F32)
        kb.dma("sp", pm0.t[:], self.ins["pm0"].ap, self.ins["pm0"], pm0)
        sinkb = kb.sb("sinkb", [128, 16], F32)
        kb.dma("sp", sinkb.t[:], self.ins["attn_sinks"].ap.partition_broadcast(128), self.ins["attn_sinks"], sinkb)
        swa_tm = [kb.sb("swa_tm%d" % i, [128, 1024], BF16) for i in range(2)]
        swaT = kb.sb("swaT", [128, 8, 1024], BF16, grp=self.gT)
        P["swaT"] = swaT
        NS = 3
        dbgt = [kb.sb('dbgt%d' % i, [128, 1024], F32) for i in range(2)]
        l32 = [kb.sb("l32_%d" % i, [128, 256], F32) for i in range(NS)]
        pbf = [kb.sb("pbf_%d" % i, [128, 256], BF16) for i in range(NS)]
        pT = [kb.sb("pT_%d" % i, [128, 2, 128], BF16) for i in range(NS)]
        sm = [kb.sb("sm_%d" % i, [128, 8], F32) for i in range(NS)]
        qAT, kAT, vA = P["qAT"], P["kAT"], P["vA"]
        it = 0
        for i in range(8):
            so = swa_tm[i % 2]
            for h in range(16):
                s = it % NS
                it += 1
                j, a = h % 8, h // 8
                Z = kb.bank()
                for c in range(2):
                    kb.op("pe", lambda e, c=c: e.matmul(Z.t[:, c * 128:(c + 1) * 128], lhsT=qAT.t[a * 64:(a + 1) * 64, j, i * 128:(i + 1) * 128],
                                                        rhs=kAT.t[a * 64:(a + 1) * 64, c, i * 128:(i + 1) * 128], start=True, stop=True),
                          reads=[qAT, kAT], writes=[Z] if c == 0 else (), pwrites=() if c == 0 else [Z])
                kb.op("dve", lambda e: e.tensor_tensor(out=l32[s].t[:], in0=Z.t[:, 0:256], in1=swab.t[:, h, :], op=ALU.add),
                      reads=[Z, swab], writes=[l32[s]])
                if i == 0:
                    kb.op("dve", lambda e: e.tensor_tensor(out=l32[s].t[:], in0=l32[s].t[:], in1=pm0.t[:], op=ALU.add),
                          reads=[l32[s], pm0], writes=[l32[s]])
                kb.op("dve", lambda e: e.reduce_max(out=sm[s].t[:, 0:1], in_=l32[s].t[:], axis=AX.X), reads=[l32[s]], writes=[sm[s]])
                kb.op("dve", lambda e: e.tensor_scalar(out=sm[s].t[:, 1:2], in0=sm[s].t[:, 0:1], scalar1=-1.0, scalar2=None, op0=ALU.mult),
                      reads=[sm[s]], pwrites=[sm[s]])
                kb.op("act", lambda e: e.activation(out=pbf[s].t[:], in_=l32[s].t[:], func=AF.Exp, bias=sm[s].t[:, 1:2], scale=1.0,
                                                    accum_out=sm[s].t[:, 2:3]),
                      reads=[l32[s], sm[s]], writes=[pbf[s]], pwrites=[sm[s]])
                kb.op("act", lambda e: e.activation(out=sm[s].t[:, 3:4], in_=sinkb.t[:, h:h + 1], func=AF.Exp, bias=sm[s].t[:, 1:2], scale=1.0),
                      reads=[sinkb, sm[s]], pwrites=[sm[s]])
                kb.op("dve", lambda e: e.tensor_tensor(out=sm[s].t[:, 4:5], in0=sm[s].t[:, 2:3], in1=sm[s].t[:, 3:4], op=ALU.add),
                      reads=[sm[s]], pwrites=[sm[s]])
                kb.op("dve", lambda e: e.reciprocal(out=sm[s].t[:, 5:6], in_=sm[s].t[:, 4:5]), reads=[sm[s]], pwrites=[sm[s]])
                PT = kb.bank()
                ptv = PT.t[:].bitcast(BF16)
                for c in range(2):
                    kb.op("pe", lambda e, c=c: e.transpose(out=ptv[:, c * 128:(c + 1) * 128], in_=pbf[s].t[:, c * 128:(c + 1) * 128],
                                                           identity=self.ident_bf.t[:]),
                          reads=[pbf[s], self.ident_bf], writes=[PT] if c == 0 else (), pwrites=() if c == 0 else [PT])
                kb.op("act", lambda e: e.activation(out=pT[s].t[:].rearrange("p a b -> p (a b)"), in_=ptv[:, 0:256], func=AF.Copy),
                      reads=[PT], writes=[pT[s]])
                O = kb.bank()
                for c in range(2):
                    kb.op("pe", lambda e, c=c: e.matmul(O.t[:, 0:64], lhsT=pT[s].t[:, c, :], rhs=vA.t[:, c, i, a * 64:(a + 1) * 64],
                                                        start=(c == 0), stop=(c == 1)),
                          reads=[pT[s], vA], writes=[O] if c == 0 else (), pwrites=() if c == 0 else [O])
                kb.op("dve", lambda e: e.tensor_scalar(out=so.t[:, h * 64:(h + 1) * 64], in0=O.t[:, 0:64], scalar1=sm[s].t[:, 5:6], scalar2=None,
                                                       op0=ALU.mult),
                      reads=[O, sm[s]], pwrites=[so] if h else (), writes=() if h else [so])
            self.to_fm(so, swaT, i * 128, nchunk=8, evac_alt=i)
            if cfg.get("dbg") == "swa":
                dt_ = dbgt[i % 2]
                kb.op("dve", lambda e, dt_=dt_: e.tensor_copy(out=dt_.t[:], in_=so.t[:]), reads=[so], writes=[dt_])
                kb.dma("sp", self.outs["dbg"].ap[i * 128:(i + 1) * 128, 0:1024], dt_.t[:], dt_, self.outs["dbg"], partial=True)
        kb.phase_end()
        kb.freegrp(self.gSWA)

        kb.phase_begin()
        sbmask = kb.sb("sbmask", [128, 4, 128], BF16)
        kb.dma("pool", sbmask.t[:], self.ins["sbmask"].ap, self.ins["sbmask"], sbmask)
        accs = [kb.sb("sbacc%d" % i, [128, 1024], F32) for i in range(8)]
        for i in range(8):
            kb.op("pool", lambda e, i=i: e.memset(accs[i].t[:], 0.0), writes=[accs[i]])
        kTp = [kb.sb("kTp%d" % i, [128, 4096], BF16) for i in range(2)]
        vP = [kb.sb("vP%d" % i, [128, 32, 128], BF16) for i in range(2)]
        carry = kb.sb("carry", [128, 8], F32)
        Eb = [kb.sb("Eb%d" % i, [128, 8], F32) for i in range(2)]
        NU = 3
        e32 = [kb.sb("e32_%d" % i, [128, 512], F32) for i in range(NU)]
        spb = [kb.sb("spb_%d" % i, [128, 512], BF16) for i in range(NU)]
        wb = [kb.sb("wb_%d" % i, [128, 512], BF16) for i in range(NU)]
        n_hp = cfg.get("n_hp", 8)
        units = []
        for hp in range(n_hp):
            for hh in range(2):
                for kbi in range(31, -1, -1):
                    g = kbi // 4
                    chs = chunks_of(g * 128, 1024)
                    for ci, (c0, c1) in enumerate(chs):
                        units.append(dict(hp=hp, hh=hh, kb=kbi, g=g, r=kbi % 4, c0=c0, c1=c1, first=(ci == 0), last=(ci == len(chs) - 1),
                                          newhead=(kbi == 31 and ci == 0), newpair=(kbi == 31 and ci == 0 and hh == 0)))
        Abanks = [kb.banks[0], kb.banks[1], kb.banks[2]]
        Obanks = [kb.banks[3], kb.banks[4]]
        Cbanks = [kb.banks[5], kb.banks[6], kb.banks[7]]
        ecount = [0]

        def stage_a(u, ui):
            hp, hh = u["hp"], u["hh"]
            if u["newpair"]:
                kb.dma("sp", kTp[hp % 2].t[:], self.kT_scr.ap[hp], self.kT_scr, kTp[hp % 2])
                vsrc = self.V_scr.ap.rearrange("t p n -> p t n")
                for t0 in range(0, 32, 8):
                    kb.dma("sp", vP[hp % 2].t[:, t0:t0 + 8, :], vsrc[:, t0:t0 + 8, hp * 128:(hp + 1) * 128], self.V_scr, vP[hp % 2], partial=(t0 > 0))
            A = Abanks[ui % 3]
            s = ui % NU
            n = u["c1"] - u["c0"]
            ps0 = hh * 64
            kk = kTp[hp % 2]
            kb.op("pe", lambda e: e.matmul(A.t[:, 0:n], lhsT=kk.t[ps0:ps0 + 64, u["kb"] * 128:(u["kb"] + 1) * 128],
                                           rhs=qBT_.t[ps0:ps0 + 64, hp, u["c0"]:u["c1"]], start=True, stop=True),
                  reads=[kk, qBT_], writes=[A])
            kb.op("act", lambda e: e.activation(out=e32[s].t[:, 0:n], in_=A.t[:, 0:n], func=AF.Exp), reads=[A], writes=[e32[s]])
            kb.op("act", lambda e: e.activation(out=spb[s].t[:, 0:n], in_=e32[s].t[:, 0:n], func=AF.Ln, bias=self.ones_f.t[:, 0:1], scale=1.0),
                  reads=[e32[s], self.ones_f], writes=[spb[s]])
            if u["first"]:
                kb.op("dve", lambda e: e.tensor_tensor(out=spb[s].t[:, 0:128], in0=spb[s].t[:, 0:128], in1=sbmask.t[:, u["r"], :], op=ALU.mult),
                      reads=[spb[s], sbmask], writes=[spb[s]])
            kb.op("pe", lambda e: e.matmul(A.t[:, 0:n], lhsT=self.negtri.t[:], rhs=spb[s].t[:, 0:n], start=False, stop=True),
                  reads=[self.negtri, spb[s]], pwrites=[A])
            C = Cbanks[ui % 3]
            for t in range(n // 128):
                i = u["c0"] // 128 + t
                kb.op("pe", lambda e, t=t, i=i: e.matmul(C.t[:, i:i + 1], lhsT=spb[s].t[:, t * 128:(t + 1) * 128], rhs=self.ones_bf.t[:, 0:1],
                                                         start=True, stop=True),
                      reads=[spb[s], self.ones_bf], writes=[C] if t == 0 else (), pwrites=() if t == 0 else [C])

        def stage_b(u, ui):
            hp, hh = u["hp"], u["hh"]
            head = hp * 2 + hh
            A = Abanks[ui % 3]
            s = ui % NU
            n = u["c1"] - u["c0"]
            C = Cbanks[ui % 3]
            O = Obanks[ui % 2]
            if u["newhead"]:
                kb.op("dve", lambda e: e.memset(carry.t[:], 0.0), writes=[carry])
            if u["first"]:
                ecount[0] += 1
                Ecur = Eb[ecount[0] % 2]
                kb.op("act", lambda e: e.activation(out=Ecur.t[:], in_=carry.t[:], func=AF.Exp, scale=-1.0), reads=[carry], writes=[Ecur])
            Ecur = Eb[ecount[0] % 2]
            kb.op("act", lambda e: e.activation(out=wb[s].t[:, 0:n], in_=A.t[:, 0:n], func=AF.Exp), reads=[A], writes=[wb[s]])
            if u["first"]:
                kb.op("dve", lambda e: e.tensor_tensor(out=wb[s].t[:, 0:128], in0=wb[s].t[:, 0:128], in1=sbmask.t[:, u["r"], :], op=ALU.mult),
                      reads=[wb[s], sbmask], writes=[wb[s]])
            vv = vP[hp % 2]
            for t in range(n // 128):
                kb.op("pe", lambda e, t=t: e.matmul(O.t[:, t * 64:(t + 1) * 64], lhsT=wb[s].t[:, t * 128:(t + 1) * 128],
                                                    rhs=vv.t[:, u["kb"], hh * 64:(hh + 1) * 64], start=True, stop=True),
                      reads=[wb[s], vv], writes=[O] if t == 0 else (), pwrites=() if t == 0 else [O])
            for t in range(n // 128):
                i = u["c0"] // 128 + t
                kb.op("dve", lambda e, t=t, i=i: e.scalar_tensor_tensor(out=accs[i].t[:, head * 64:(head + 1) * 64], in0=O.t[:, t * 64:(t + 1) * 64],
                                                                        scalar=Ecur.t[:, i:i + 1], in1=accs[i].t[:, head * 64:(head + 1) * 64],
                                                                        op0=ALU.mult, op1=ALU.add),
                      reads=[O, Ecur, accs[i]], writes=[accs[i]])
            i0, i1 = u["c0"] // 128, u["c1"] // 128
            kb.op("dve", lambda e: e.tensor_tensor(out=carry.t[:, i0:i1], in0=carry.t[:, i0:i1], in1=C.t[:, i0:i1], op=ALU.add),
                  reads=[carry, C], writes=[carry])

        qBT_ = P["qBT"]
        for ui in range(len(units) + 1):
            if ui < len(units):
                stage_a(units[ui], ui)
            if ui >= 1:
                stage_b(units[ui - 1], ui - 1)
        sbT = kb.sb("sbT", [128, 8, 1024], BF16, grp=self.gT)
        P["sbT"] = sbT
        cvt = [kb.sb("sbcvt%d" % i, [128, 1024], BF16) for i in range(2)]
        for i in range(8):
            cb = cvt[i % 2]
            kb.op("act", lambda e, i=i, cb=cb: e.activation(out=cb.t[:], in_=accs[i].t[:], func=AF.Copy), reads=[accs[i]], writes=[cb])
            self.to_fm(cb, sbT, i * 128, nchunk=8, evac_alt=i)
        if cfg.get("dbg") == "sb":
            for i in range(8):
                kb.dma("sp", self.outs["dbg"].ap[i * 128:(i + 1) * 128, 0:1024], accs[i].t[:], accs[i], self.outs["dbg"], partial=True)
        kb.phase_end()
        kb.freegrp(self.gQB)

        kb.phase_begin()
        gM = kb.newgrp()
        mixT = kb.sb("mixT", [128, 16, 1024], BF16, grp=gM)
        NW = 2
        wa = [kb.sb("wa%d" % i, [128, 8, 256], BF16) for i in range(NW)]
        wbb = [kb.sb("wbb%d" % i, [128, 8, 256], BF16) for i in range(NW)]
        wga = [kb.sb("wga%d" % i, [128, 16, 256], BF16) for i in range(NW)]
        wgb = [kb.sb("wgb%d" % i, [128, 16, 256], BF16) for i in range(NW)]
        sg = [kb.sb("sg%d" % i, [128, 512], F32) for i in range(4)]
        tm = [kb.sb("tm%d" % i, [128, 512], F32) for i in range(4)]
        w_a, w_b = self.ins["w_a_out"], self.ins["w_b_out"]
        swaT, sbT = P["swaT"], P["sbT"]
        hT_own = P["hT_own"]
        it = 0
        for grp in range(8):
            s = grp % NW
            c0 = grp * 256
            self.load_w(wa[s], w_a.ap[:, c0:c0 + 256], w_a)
            self.load_w(wbb[s], w_b.ap[:, c0:c0 + 256], w_b)
            self.load_w(wga[s], w_in.ap[:, GA0 + c0:GA0 + c0 + 256], w_in)
            self.load_w(wgb[s], w_in.ap[:, GB0 + c0:GB0 + c0 + 256], w_in)
            for f in range(2):
                fc = grp * 2 + f
                for th in range(2):
                    u = it % 2
                    it += 1
                    bya, byb, bga, bgb = kb.bank(), kb.bank(), kb.bank(), kb.bank()
                    self.mm_fm(bga, wga[s], f * 128, hT_own, th * 512, 512, 16)
                    self.mm_fm(bgb, wgb[s], f * 128, hT_own, th * 512, 512, 16)
                    self.mm_fm(bya, wa[s], f * 128, swaT, th * 512, 512, 8)
                    self.mm_fm(byb, wbb[s], f * 128, sbT, th * 512, 512, 8)
                    sga, sgb, t1, t2 = sg[2 * u], sg[2 * u + 1], tm[2 * u], tm[2 * u + 1]
                    kb.op("act", lambda e, fc=fc, sga=sga, bga=bga: e.activation(out=sga.t[:], in_=bga.t[:], func=AF.Sigmoid, bias=bcol.t[:, 34 + fc:35 + fc], scale=1.0),
                          reads=[bga, bcol], writes=[sga])
                    kb.op("act", lambda e, fc=fc, sgb=sgb, bgb=bgb: e.activation(out=sgb.t[:], in_=bgb.t[:], func=AF.Sigmoid, bias=bcol.t[:, 50 + fc:51 + fc], scale=1.0),
                          reads=[bgb, bcol], writes=[sgb])
                    kb.op("dve", lambda e, t1=t1, sga=sga, bya=bya: e.tensor_tensor(out=t1.t[:], in0=sga.t[:], in1=bya.t[:], op=ALU.mult), reads=[sga, bya], writes=[t1])
                    kb.op("dve", lambda e, t2=t2, sgb=sgb, byb=byb: e.tensor_tensor(out=t2.t[:], in0=sgb.t[:], in1=byb.t[:], op=ALU.mult), reads=[sgb, byb], writes=[t2])
                    kb.op("pool", lambda e, fc=fc, th=th, t1=t1, t2=t2: e.tensor_tensor(out=mixT.t[:, fc, th * 512:(th + 1) * 512], in0=t1.t[:], in1=t2.t[:], op=ALU.add),
                          reads=[t1, t2], pwrites=[mixT])
        if cfg.get("dbg") == "mix":
            dbgt = [kb.sb('dbgm%d' % i, [128, 1024], F32) for i in range(2)]
            for fc in range(16):
                dt_ = dbgt[fc % 2]
                kb.op("dve", lambda e, dt_=dt_, fc=fc: e.tensor_copy(out=dt_.t[:], in_=mixT.t[:, fc, :]), reads=[mixT], writes=[dt_])
                kb.dma("sp", self.outs["dbg"].ap[fc * 128:(fc + 1) * 128 if fc < 8 else (fc - 8) * 128 + 128, 0:1024] if fc < 8 else
                       self.outs["dbg"].ap[(fc - 8) * 128:(fc - 7) * 128, 1024:2048], dt_.t[:], dt_, self.outs["dbg"], partial=True)
        kb.phase_end()
        kb.freegrp(self.gA)
        kb.freegrp(self.gT)

        kb.phase_begin()
        gr1, br1 = self.load_rows("ln1_g", "ln1_b")
        self.make_ln_bufs(2)
        wmix = kb.sb("wmix", [128, 16, D], BF16)
        h0t = [kb.sb("h0t%d" % i, [128, D], F32) for i in range(2)]
        w_m = self.ins["w_mix_out"]
        self.load_w(wmix, w_m.ap, w_m, split=4)
        for i in range(8):
            S = self.ln_sets[i % 2]
            hh0 = h0t[i % 2]
            kb.dma("sp", hh0.t[:], self.h0_scr.ap[i * 128:(i + 1) * 128, :], self.h0_scr, hh0)
            for nch in range(4):
                bk = kb.bank()
                self.mm_tm(bk, mixT, i * 128, wmix, nch * 512, 512, 16)
                kb.op("dve", lambda e, bk=bk, S=S, hh0=hh0, nch=nch: e.scalar_tensor_tensor(out=S["x"].t[:, nch * 512:(nch + 1) * 512], in0=hh0.t[:, nch * 512:(nch + 1) * 512],
                                                                                           scalar=ALPHA, in1=bk.t[:, :], op0=ALU.mult, op1=ALU.add),
                      reads=[hh0, bk], writes=[S["x"]] if nch == 0 else (), pwrites=() if nch == 0 else [S["x"]])
            self.ln_core(S, gr1, br1, want_h32=True, want_hb=False)
            kb.dma("sp", self.h1_scr.ap[i * 128:(i + 1) * 128, :], S["x"].t[:], S["x"], self.h1_scr, partial=True)
        kb.phase_end()
        kb.freegrp(gM)

    def cast_load_fm(self, src_ap, src_buf, dst_fm, off, tiles):
        kb = self.kb
        t = tiles[self.cl_i % len(tiles)]
        self.cl_i += 1
        kb.dma("pool", t.t[:], src_ap, src_buf, t)
        self.to_fm(t, dst_fm, off, evac_alt=self.cl_i)

    def stage2(self):
        kb = self.kb
        cfg = self.cfg
        mem = self.ins["mem"]
        w_xq, w_xkv, w_xo = self.ins["w_xq"], self.ins["w_xkv"], self.ins["w_xo"]
        h1 = self.h1_scr
        self.cl_i = 0
        g2 = kb.newgrp()
        kxT = kb.sb("kxT", [128, 16, 256], BF16, grp=g2)
        vx = kb.sb("vx", [128, 2, D], BF16, grp=g2)
        qxT = kb.sb("qxT", [128, 16, 1024], BF16, grp=g2)
        kb.phase_begin()
        memT = kb.sb("memT", [128, 16, 256], BF16)
        h1T = kb.sb("h1T", [128, 16, 1024], BF16)
        ct = [kb.sb("ct%d" % i, [128, D], BF16) for i in range(2)]
        for i in range(2):
            self.cast_load_fm(mem.ap[i * 128:(i + 1) * 128, :], mem, memT, i * 128, ct)
        for i in range(8):
            self.cast_load_fm(h1.ap[i * 128:(i + 1) * 128, :], h1, h1T, i * 128, ct)
        wp = [kb.sb("wp%d" % i, [128, 16, 512], BF16) for i in range(2)]
        pi = 0
        for pc in range(4):
            w = wp[pi % 2]; pi += 1
            self.load_w(w, w_xkv.ap[:, pc * 512:(pc + 1) * 512], w_xkv)
            for f in range(4):
                bk = kb.bank()
                self.mm_fm(bk, w, f * 128, memT, 0, 256, 16)
                kb.op("act", lambda e, bk=bk, fc=pc * 4 + f: e.activation(out=kxT.t[:, fc, :], in_=bk.t[:, 0:256], func=AF.Copy),
                      reads=[bk], pwrites=[kxT])
        for pc in range(4):
            w = wp[pi % 2]; pi += 1
            self.load_w(w, w_xkv.ap[:, D + pc * 512:D + (pc + 1) * 512], w_xkv)
            for mt in range(2):
                bk = kb.bank()
                self.mm_tm(bk, memT, mt * 128, w, 0, 512, 16)
                kb.op("dve", lambda e, bk=bk, mt=mt, pc=pc: e.tensor_copy(out=vx.t[:, mt, pc * 512:(pc + 1) * 512], in_=bk.t[:, :]),
                      reads=[bk], pwrites=[vx])
        for pc in range(4):
            w = wp[pi % 2]; pi += 1
            self.load_w(w, w_xq.ap[:, pc * 512:(pc + 1) * 512], w_xq)
            for f in range(4):
                for th in range(2):
                    bk = kb.bank()
                    self.mm_fm(bk, w, f * 128, h1T, th * 512, 512, 16)
                    kb.op("act", lambda e, bk=bk, fc=pc * 4 + f, th=th: e.activation(out=qxT.t[:, fc, th * 512:(th + 1) * 512], in_=bk.t[:, :],
                                                                                    func=AF.Copy, scale=float(512 ** -0.5)),
                          reads=[bk], pwrites=[qxT])
        kb.phase_end()
        if cfg.get("s2") == "a":
            return
        kb.phase_begin()
        g2o = kb.newgrp()
        oT = kb.sb("oT", [128, 16, 1024], BF16, grp=g2o)
        pT_all = kb.sb("pT_all", [128, 4, 2, 1024], BF16)
        NS = 3
        p32 = [kb.sb("xp32_%d" % i, [128, 256], F32) for i in range(NS)]
        pn = [kb.sb("xpn_%d" % i, [128, 256], BF16) for i in range(NS)]
        sm = [kb.sb("xsm_%d" % i, [128, 8], F32) for i in range(NS)]
        it = 0
        for tt in range(8):
            for h in range(4):
                s = it % NS
                it += 1
                Z = kb.bank()
                for c in range(4):
                    kb.op("pe", lambda e, c=c, Z=Z: e.matmul(Z.t[:, 0:256], lhsT=qxT.t[:, 4 * h + c, tt * 128:(tt + 1) * 128], rhs=kxT.t[:, 4 * h + c, :],
                                                            start=(c == 0), stop=(c == 3)),
                          reads=[qxT, kxT], writes=[Z] if c == 0 else (), pwrites=() if c == 0 else [Z])
                kb.op("dve", lambda e, Z=Z: e.reduce_max(out=sm[s].t[:, 0:1], in_=Z.t[:, 0:256], axis=AX.X), reads=[Z], writes=[sm[s]])
                kb.op("dve", lambda e: e.tensor_scalar(out=sm[s].t[:, 1:2], in0=sm[s].t[:, 0:1], scalar1=-1.0, scalar2=None, op0=ALU.mult),
                      reads=[sm[s]], pwrites=[sm[s]])
                kb.op("act", lambda e, Z=Z: e.activation(out=p32[s].t[:], in_=Z.t[:, 0:256], func=AF.Exp, bias=sm[s].t[:, 1:2], scale=1.0,
                                                         accum_out=sm[s].t[:, 2:3]),
                      reads=[Z, sm[s]], writes=[p32[s]], pwrites=[sm[s]])
                kb.op("dve", lambda e: e.reciprocal(out=sm[s].t[:, 3:4], in_=sm[s].t[:, 2:3]), reads=[sm[s]], pwrites=[sm[s]])
                kb.op("dve", lambda e: e.tensor_scalar(out=pn[s].t[:], in0=p32[s].t[:], scalar1=sm[s].t[:, 3:4], scalar2=None, op0=ALU.mult),
                      reads=[p32[s], sm[s]], writes=[pn[s]])
                PT = kb.bank()
                ptv = PT.t[:].bitcast(BF16)
                for c in range(2):
                    kb.op("pe", lambda e, c=c: e.transpose(out=ptv[:, c * 128:(c + 1) * 128], in_=pn[s].t[:, c * 128:(c + 1) * 128],
                                                           identity=self.ident_bf.t[:]),
                          reads=[pn[s], self.ident_bf], writes=[PT] if c == 0 else (), pwrites=() if c == 0 else [PT])
                kb.op("act", lambda e, ptv=ptv, PT=PT: e.activation(out=pT_all.t[:, h, :, tt * 128:(tt + 1) * 128],
                                                                    in_=ptv[:, 0:256].rearrange("p (a b) -> p a b", a=2), func=AF.Copy),
                      reads=[PT], pwrites=[pT_all])
        for h in range(4):
            for dc in range(4):
                fc = 4 * h + dc
                for th in range(2):
                    bk = kb.bank()
                    for mc in range(2):
                        kb.op("pe", lambda e, mc=mc, bk=bk: e.matmul(bk.t[:, :], lhsT=vx.t[:, mc, fc * 128:(fc + 1) * 128],
                                                                    rhs=pT_all.t[:, h, mc, th * 512:(th + 1) * 512], start=(mc == 0), stop=(mc == 1)),
                              reads=[vx, pT_all], writes=[bk] if mc == 0 else (), pwrites=() if mc == 0 else [bk])
                    if (fc + th) % 2:
                        kb.op("act", lambda e, bk=bk: e.activation(out=oT.t[:, fc, th * 512:(th + 1) * 512], in_=bk.t[:, :], func=AF.Copy),
                              reads=[bk], pwrites=[oT])
                    else:
                        kb.op("dve", lambda e, bk=bk: e.tensor_copy(out=oT.t[:, fc, th * 512:(th + 1) * 512], in_=bk.t[:, :]),
                              reads=[bk], pwrites=[oT])
        kb.phase_end()
        kb.freegrp(g2)
        if cfg.get("s2") == "b":
            return
        kb.phase_begin()
        gr2, br2 = self.load_rows("ln2_g", "ln2_b")
        self.make_ln_bufs(2)
        wxo = kb.sb("wxo", [128, 16, D], BF16)
        h1t = [kb.sb("h1t%d" % i, [128, D], F32) for i in range(2)]
        self.load_w(wxo, w_xo.ap, w_xo, split=4)
        for i in range(8):
            S = self.ln_sets[i % 2]
            hh = h1t[i % 2]
            kb.dma("sp", hh.t[:], h1.ap[i * 128:(i + 1) * 128, :], h1, hh)
            for nch in range(4):
                bk = kb.bank()
                self.mm_tm(bk, oT, i * 128, wxo, nch * 512, 512, 16)
                kb.op("dve", lambda e, bk=bk, S=S, hh=hh, nch=nch: e.scalar_tensor_tensor(out=S["x"].t[:, nch * 512:(nch + 1) * 512], in0=hh.t[:, nch * 512:(nch + 1) * 512],
                                                                                         scalar=ALPHA, in1=bk.t[:, :], op0=ALU.mult, op1=ALU.add),
                      reads=[hh, bk], writes=[S["x"]] if nch == 0 else (), pwrites=() if nch == 0 else [S["x"]])
            self.ln_core(S, gr2, br2, want_h32=True, want_hb=False)
            kb.dma("sp", self.h2_scr.ap[i * 128:(i + 1) * 128, :], S["x"].t[:], S["x"], self.h2_scr, partial=True)
        kb.phase_end()
        kb.freegrp(g2o)

    def stage3(self):
        kb = self.kb
        cfg = self.cfg
        n_exp = cfg.get("n_exp", NE)
        h2 = self.h2_scr
        w_up, w_down = self.ins["w_up"], self.ins["w_down"]
        g3 = kb.newgrp()
        yacc = [kb.sb("yacc%d" % i, [128, D], F32, grp=g3) for i in range(8)]
        kb.phase_begin()
        g3b = kb.newgrp()
        X_bf = kb.sb("X_bf", [128, 8, D], BF16, grp=g3b)
        mask_all = kb.sb("mask_all", [128, 8, NE], F32, grp=g3b)
        gate_all = kb.sb("gate_all", [128, 8, NE], F32, grp=g3b)
        pos_all = kb.sb("pos_all", [128, 8, NE], F32, grp=g3b)
        mask_bf = kb.sb("mask_bf", [128, 8, NE], BF16)
        wr = kb.sb("wr", [128, 16, NE], F32)
        kb.dma("sp", wr.t[:], self.ins["w_router"].ap.rearrange("(c p) n -> p c n", p=128), self.ins["w_router"], wr)
        brt = kb.sb("brt", [128, NE], F32)
        kb.dma("sp", brt.t[:], self.ins["b_router"].ap.partition_broadcast(128), self.ins["b_router"], brt)
        h2t = [kb.sb("h2t%d" % i, [128, D], F32) for i in range(2)]
        hT32 = [kb.sb("hT32_%d" % i, [128, 16, 128], F32) for i in range(2)]
        lg = [kb.sb("lg%d" % i, [128, NE], F32) for i in range(2)]
        t8 = [kb.sb("t8_%d" % i, [128, 16], F32) for i in range(2)]
        eg = [kb.sb("eg%d" % i, [128, NE], F32) for i in range(2)]
        for tt in range(8):
            u = tt % 2
            hh = h2t[u]
            kb.dma("sp", hh.t[:], h2.ap[tt * 128:(tt + 1) * 128, :], h2, hh)
            kb.op("act", lambda e, hh=hh, tt=tt: e.activation(out=X_bf.t[:, tt, :], in_=hh.t[:], func=AF.Copy), reads=[hh], pwrites=[X_bf])
            kb.op("pool", lambda e, hh=hh, tt=tt: e.tensor_scalar(out=yacc[tt].t[:], in0=hh.t[:], scalar1=ALPHA, scalar2=None, op0=ALU.mult),
                  reads=[hh], writes=[yacc[tt]])
            for q4 in range(4):
                bk = kb.bank()
                for k in range(4):
                    kk = q4 * 4 + k
                    kb.op("pe", lambda e, k=k, kk=kk, bk=bk, hh=hh: e.transpose(out=bk.t[:, k * 128:(k + 1) * 128], in_=hh.t[:, kk * 128:(kk + 1) * 128],
                                                                               identity=self.ident_f.t[:]),
                          reads=[hh, self.ident_f], writes=[bk] if k == 0 else (), pwrites=() if k == 0 else [bk])
                kb.op("dve", lambda e, bk=bk, q4=q4, u=u: e.tensor_copy(out=hT32[u].t[:, q4 * 4:(q4 + 1) * 4, :], in_=bk.t[:, :].rearrange("p (a b) -> p a b", a=4)),
                      reads=[bk], pwrites=[hT32[u]] if q4 else (), writes=() if q4 else [hT32[u]])
            L = kb.bank()
            for k in range(16):
                kb.op("pe", lambda e, k=k, L=L, u=u: e.matmul(L.t[:, 0:NE], lhsT=hT32[u].t[:, k, :], rhs=wr.t[:, k, :], start=(k == 0), stop=(k == 15)),
                      reads=[hT32[u], wr], writes=[L] if k == 0 else (), pwrites=() if k == 0 else [L])
            kb.op("dve", lambda e, L=L, u=u: e.tensor_tensor(out=lg[u].t[:], in0=L.t[:, 0:NE], in1=brt.t[:], op=ALU.add), reads=[L, brt], writes=[lg[u]])
            kb.op("dve", lambda e, u=u: e.max(out=t8[u].t[:, 0:8], in_=lg[u].t[:]), reads=[lg[u]], writes=[t8[u]])
            kb.op("dve", lambda e, u=u, tt=tt: e.tensor_scalar(out=mask_all.t[:, tt, :], in0=lg[u].t[:], scalar1=t8[u].t[:, 3:4], scalar2=None, op0=ALU.is_ge),
                  reads=[lg[u], t8[u]], pwrites=[mask_all])
            kb.op("dve", lambda e, u=u: e.tensor_scalar(out=t8[u].t[:, 8:9], in0=t8[u].t[:, 0:1], scalar1=-1.0, scalar2=None, op0=ALU.mult),
                  reads=[t8[u]], pwrites=[t8[u]])
            kb.op("act", lambda e, u=u: e.activation(out=eg[u].t[:], in_=lg[u].t[:], func=AF.Exp, bias=t8[u].t[:, 8:9], scale=1.0),
                  reads=[lg[u], t8[u]], writes=[eg[u]])
            kb.op("dve", lambda e, u=u, tt=tt: e.tensor_tensor(out=eg[u].t[:], in0=eg[u].t[:], in1=mask_all.t[:, tt, :], op=ALU.mult),
                  reads=[eg[u], mask_all], writes=[eg[u]])
            kb.op("dve", lambda e, u=u: e.reduce_sum(out=t8[u].t[:, 9:10], in_=eg[u].t[:], axis=AX.X), reads=[eg[u]], pwrites=[t8[u]])
            kb.op("dve", lambda e, u=u: e.reciprocal(out=t8[u].t[:, 10:11], in_=t8[u].t[:, 9:10]), reads=[t8[u]], pwrites=[t8[u]])
            kb.op("dve", lambda e, u=u, tt=tt: e.tensor_scalar(out=gate_all.t[:, tt, :], in0=eg[u].t[:], scalar1=t8[u].t[:, 10:11], scalar2=None, op0=ALU.mult),
                  reads=[eg[u], t8[u]], pwrites=[gate_all])
            kb.op("dve", lambda e, tt=tt: e.tensor_copy(out=mask_bf.t[:, tt, :], in_=mask_all.t[:, tt, :]), reads=[mask_all], pwrites=[mask_bf])
            Pp = kb.bank()
            for t2 in range(tt + 1):
                lhs = self.trix if t2 == tt else self.ones_bf
                kb.op("pe", lambda e, t2=t2, lhs=lhs, Pp=Pp: e.matmul(Pp.t[:, 0:NE], lhsT=lhs.t[:], rhs=mask_bf.t[:, t2, :], start=(t2 == 0), stop=(t2 == tt)),
                      reads=[lhs, mask_bf], writes=[Pp] if t2 == 0 else (), pwrites=() if t2 == 0 else [Pp])
            kb.op("dve", lambda e, Pp=Pp, tt=tt: e.tensor_copy(out=pos_all.t[:, tt, :], in_=Pp.t[:, 0:NE]), reads=[Pp], pwrites=[pos_all])
        if cfg.get("dbg") == "route":
            for tt in range(8):
                kb.dma("sp", self.outs["dbg"].ap[tt * 128:(tt + 1) * 128, 0:NE], mask_all.t[:, tt, :], mask_all, self.outs["dbg"], partial=True)
                kb.dma("sp", self.outs["dbg"].ap[tt * 128:(tt + 1) * 128, NE:2 * NE], gate_all.t[:, tt, :], gate_all, self.outs["dbg"], partial=True)
                kb.dma("sp", self.outs["dbg"].ap[tt * 128:(tt + 1) * 128, 2 * NE:3 * NE], pos_all.t[:, tt, :], pos_all, self.outs["dbg"], partial=True)
        kb.phase_end()
        kb.phase_begin()
        C = CAP
        cchunks = [(0, 128), (128, C)] if C > 128 else [(0, C)]
        iota_r = kb.sb("iota_r", [128, C], F32)
        kb.dma("sp", iota_r.t[:], self.ins["c_iota"].ap[:, 0:C], self.ins["c_iota"], iota_r)
        bupc = kb.sb("bupc", [128, NE, 32], F32)
        kb.dma("sp", bupc.t[:], self.ins["bup_col"].ap, self.ins["bup_col"], bupc)
        Sel = [kb.sb("Sel%d" % i, [128, 8, C], BF16) for i in range(2)]
        SelG = [kb.sb("SelG%d" % i, [128, 8, C], BF16) for i in range(2)]
        SelGT = [kb.sb("SelGT%d" % i, [128, 2, 1024], BF16) for i in range(2)]
        xeT = [kb.sb("xeT%d" % i, [128, 16, C], BF16) for i in range(1)] * 2
        actT = [kb.sb("actT%d" % i, [128, 16, C], BF16) for i in range(1)] * 2
        Ye = [kb.sb("Ye%d" % i, [128, 2, D], BF16) for i in range(1)] * 2
        wg = [kb.sb("wg%d" % i, [128, 16, 128], BF16) for i in range(2)]
        wl = [kb.sb("wl%d" % i, [128, 16, 128], BF16) for i in range(2)]
        wd = [kb.sb("wd%d" % i, [128, 16, 256], BF16) for i in range(2)]
        bdr = [kb.sb("bdr%d" % i, [1, D], BF16) for i in range(2)]
        NT = 3
        g32 = [kb.sb("g32_%d" % i, [128, C], F32) for i in range(NT)]
        sgm = [kb.sb("sgm_%d" % i, [128, C], F32) for i in range(NT)]
        l32 = [kb.sb("l32m_%d" % i, [128, C], F32) for i in range(NT)]
        wpi = 0
        wdi = 0
        ti = 0
        for e_ in range(n_exp):
            u = e_ % 2
            for tt in range(8):
                kb.op("dve", lambda e, tt=tt: e.tensor_scalar(out=Sel[u].t[:, tt, :], in0=iota_r.t[:], scalar1=pos_all.t[:, tt, e_:e_ + 1],
                                                              scalar2=mask_all.t[:, tt, e_:e_ + 1], op0=ALU.is_equal, op1=ALU.mult),
                      reads=[iota_r, pos_all, mask_all], pwrites=[Sel[u]] if tt else (), writes=() if tt else [Sel[u]])
                kb.op("pool", lambda e, tt=tt: e.tensor_scalar(out=SelG[u].t[:, tt, :], in0=iota_r.t[:], scalar1=pos_all.t[:, tt, e_:e_ + 1],
                                                               scalar2=gate_all.t[:, tt, e_:e_ + 1], op0=ALU.is_equal, op1=ALU.mult),
                      reads=[iota_r, pos_all, gate_all], pwrites=[SelG[u]] if tt else (), writes=() if tt else [SelG[u]])
            for fc in range(16):
                bk = kb.bank()
                for tt in range(8):
                    kb.op("pe", lambda e, tt=tt, bk=bk, fc=fc: e.matmul(bk.t[:, 0:C], lhsT=X_bf.t[:, tt, fc * 128:(fc + 1) * 128], rhs=Sel[u].t[:, tt, :],
                                                                       start=(tt == 0), stop=(tt == 7)),
                          reads=[X_bf, Sel[u]], writes=[bk] if tt == 0 else (), pwrites=() if tt == 0 else [bk])
                if fc % 2:
                    kb.op("act", lambda e, bk=bk, fc=fc: e.activation(out=xeT[u].t[:, fc, :], in_=bk.t[:, 0:C], func=AF.Copy), reads=[bk],
                          pwrites=[xeT[u]] if fc else (), writes=() if fc else [xeT[u]])
                else:
                    kb.op("dve", lambda e, bk=bk, fc=fc: e.tensor_copy(out=xeT[u].t[:, fc, :], in_=bk.t[:, 0:C]), reads=[bk],
                          pwrites=[xeT[u]] if fc else (), writes=() if fc else [xeT[u]])
            for ci, (c0, c1) in enumerate(cchunks):
                m = c1 - c0
                bk = kb.bank()
                bv = bk.t[:].bitcast(BF16)
                for tt in range(8):
                    kb.op("pe", lambda e, tt=tt, bv=bv: e.transpose(out=bv[0:m, tt * 128:(tt + 1) * 128], in_=SelG[u].t[:, tt, c0:c1], identity=self.ident_bf.t[:]),
                          reads=[SelG[u], self.ident_bf], writes=[bk] if tt == 0 else (), pwrites=() if tt == 0 else [bk])
                kb.op("act", lambda e, bv=bv, ci=ci, m=m: e.activation(out=SelGT[u].t[0:m, ci, :], in_=bv[0:m, 0:1024], func=AF.Copy), reads=[bk],
                      pwrites=[SelGT[u]] if ci else (), writes=() if ci else [SelGT[u]])
            for pj in range(16):
                wgt, wlt = wg[wpi % 2], wl[wpi % 2]
                wpi += 1
                self.load_w(wgt, w_up.ap[e_, :, pj * 128:(pj + 1) * 128], w_up)
                self.load_w(wlt, w_up.ap[e_, :, D + pj * 128:D + (pj + 1) * 128], w_up)
                for f in range(1):
                    fc = pj
                    s3 = ti % NT
                    ti += 1
                    bg, bl = kb.bank(), kb.bank()
                    self.mm_fm(bg, wgt, f * 128, xeT[u], 0, C, 16)
                    self.mm_fm(bl, wlt, f * 128, xeT[u], 0, C, 16)
                    kb.op("dve", lambda e, bg=bg, fc=fc, s3=s3: e.tensor_scalar(out=g32[s3].t[:], in0=bg.t[:, 0:C], scalar1=bupc.t[:, e_, fc:fc + 1], scalar2=7.0,
                                                                               op0=ALU.add, op1=ALU.min),
                          reads=[bg, bupc], writes=[g32[s3]])
                    kb.op("act", lambda e, s3=s3: e.activation(out=sgm[s3].t[:], in_=g32[s3].t[:], func=AF.Sigmoid, scale=1.702), reads=[g32[s3]], writes=[sgm[s3]])
                    kb.op("dve", lambda e, bl=bl, fc=fc, s3=s3: e.tensor_scalar(out=l32[s3].t[:], in0=bl.t[:, 0:C], scalar1=bupc.t[:, e_, 16 + fc:17 + fc], scalar2=7.0,
                                                                               op0=ALU.add, op1=ALU.min),
                          reads=[bl, bupc], writes=[l32[s3]])
                    kb.op("pool", lambda e, s3=s3: e.tensor_scalar(out=l32[s3].t[:], in0=l32[s3].t[:], scalar1=-7.0, scalar2=1.0, op0=ALU.max, op1=ALU.add),
                          reads=[l32[s3]], writes=[l32[s3]])
                    kb.op("pool", lambda e, s3=s3: e.tensor_tensor(out=g32[s3].t[:], in0=g32[s3].t[:], in1=sgm[s3].t[:], op=ALU.mult),
                          reads=[g32[s3], sgm[s3]], writes=[g32[s3]])
                    kb.op("dve", lambda e, s3=s3, fc=fc: e.tensor_tensor(out=actT[u].t[:, fc, :], in0=g32[s3].t[:], in1=l32[s3].t[:], op=ALU.mult),
                          reads=[g32[s3], l32[s3]], pwrites=[actT[u]] if fc else (), writes=() if fc else [actT[u]])
            bd = bdr[u]
            kb.dma("pool", bd.t[:], self.ins["b_down"].ap[e_:e_ + 1, :], self.ins["b_down"], bd)
            for pd in range(8):
                wdt = wd[wdi % 2]
                wdi += 1
                self.load_w(wdt, w_down.ap[e_, :, pd * 256:(pd + 1) * 256], w_down)
                for ci, (c0, c1) in enumerate(cchunks):
                    m = c1 - c0
                    bk = kb.bank()
                    kb.op("pe", lambda e, bk=bk, m=m, pd=pd: e.matmul(bk.t[0:m, 0:256], lhsT=self.ones_bf.t[0:1, 0:m], rhs=bd.t[0:1, pd * 256:(pd + 1) * 256],
                                                                     start=True, stop=False),
                          reads=[self.ones_bf, bd], writes=[bk])
                    for k in range(16):
                        kb.op("pe", lambda e, bk=bk, m=m, k=k, c0=c0, c1=c1, wdt=wdt: e.matmul(bk.t[0:m, 0:256], lhsT=actT[u].t[:, k, c0:c1], rhs=wdt.t[:, k, :],
                                                                                              start=False, stop=(k == 15)),
                              reads=[actT[u], wdt], pwrites=[bk])
                    first = (pd == 0 and ci == 0)
                    if ci == 0:
                        kb.op("act", lambda e, bk=bk, m=m, ci=ci, pd=pd: e.activation(out=Ye[u].t[0:m, ci, pd * 256:(pd + 1) * 256], in_=bk.t[0:m, 0:256], func=AF.Copy),
                              reads=[bk], pwrites=() if first else [Ye[u]], writes=[Ye[u]] if first else ())
                    else:
                        kb.op("dve", lambda e, bk=bk, m=m, ci=ci, pd=pd: e.tensor_copy(out=Ye[u].t[0:m, ci, pd * 256:(pd + 1) * 256], in_=bk.t[0:m, 0:256]),
                              reads=[bk], pwrites=[Ye[u]])
            for tt in range(8):
                for nch in range(4):
                    bk = kb.bank()
                    for ci, (c0, c1) in enumerate(cchunks):
                        m = c1 - c0
                        kb.op("pe", lambda e, bk=bk, m=m, ci=ci, tt=tt, nch=nch: e.matmul(bk.t[:, :], lhsT=SelGT[u].t[0:m, ci, tt * 128:(tt + 1) * 128],
                                                                                         rhs=Ye[u].t[0:m, ci, nch * 512:(nch + 1) * 512],
                                                                                         start=(ci == 0), stop=(ci == len(cchunks) - 1)),
                              reads=[SelGT[u], Ye[u]], writes=[bk] if ci == 0 else (), pwrites=() if ci == 0 else [bk])
                    kb.op("dve", lambda e, bk=bk, tt=tt, nch=nch: e.tensor_tensor(out=yacc[tt].t[:, nch * 512:(nch + 1) * 512], in0=yacc[tt].t[:, nch * 512:(nch + 1) * 512],
                                                                                 in1=bk.t[:, :], op=ALU.add),
                          reads=[bk, yacc[tt]], writes=[yacc[tt]])
        kb.phase_end()
        kb.freegrp(g3b)
        kb.phase_begin()
        gr3, br3 = self.load_rows("ln3_g", "ln3_b")
        self.make_ln_bufs(2)
        for tt in range(8):
            S = dict(self.ln_sets[tt % 2])
            S["x"] = yacc[tt]
            self.ln_core(S, gr3, br3, want_h32=True, want_hb=False)
            kb.dma("sp", self.out_buf.ap[tt * 128:(tt + 1) * 128, :], yacc[tt].t[:], yacc[tt], self.out_buf, partial=True)
        kb.phase_end()
        kb.freegrp(g3)

    def build(self):
        cfg = self.cfg
        nc = self.nc
        with ExitStack() as es:
            kb = KB(nc, es)
            self.kb = kb
            if cfg.get("start", 1) <= 1:
                self.din("x_own", [1024, D]); self.din("x_prev", [1024, D]); self.din("x_ctx", [4096, D])
                self.din("w_in", [D, 8448])
                self.din("ln_in_g", [1, D]); self.din("ln_in_b", [1, D]); self.din("ln1_g", [1, D]); self.din("ln1_b", [1, D])
                self.din("sbmask", [128, 4, 128]); self.din("swab", [128, 16, 256]); self.din("pm0", [128, 256]); self.din("attn_sinks", [1, 16])
                self.din("w_a_out", [1024, D]); self.din("w_b_out", [1024, D]); self.din("w_mix_out", [D, D])
            self.din("bcol_in", [128, 66]); self.din("bcol_qa_in", [128, 8]); self.din("b_row_in", [1, 1152])
            self.kT_scr = self.dscr("kT_scr", [8, 128, 4096], BF16)
            self.V_scr = self.dscr("V_scr", [32, 128, 1024], BF16)
            self.h0_scr = self.dscr("h0_scr", [1024, D], F32)
            stop = cfg.get("stop", 3)
            start = cfg.get("start", 1)
            if start <= 2 <= stop:
                self.din("mem", [256, D]); self.din("w_xq", [D, D]); self.din("w_xkv", [D, 2 * D]); self.din("w_xo", [D, D])
                self.din("ln2_g", [1, D]); self.din("ln2_b", [1, D])
            if start <= 3 <= stop:
                ne = cfg.get("n_exp", NE)
                self.din("w_router", [D, NE]); self.din("b_router", [1, NE]); self.din("w_up", [ne, D, 2 * D]); self.din("w_down", [ne, D, D])
                self.din("bup_col", [128, NE, 32]); self.din("b_down", [NE, D]); self.din("ln3_g", [1, D]); self.din("ln3_b", [1, D])
                self.din("c_iota", [128, 256])
            mk = lambda nm, st: (self.dout("out", [1024, D]) if stop == st else self.dscr(nm, [1024, D], F32))
            self.h1_scr = self.din("h1_in", [1024, D]) if start == 2 else mk("h1_scr", 1)
            self.h2_scr = self.din("h2_in", [1024, D]) if start == 3 else mk("h2_scr", 2)
            self.out_buf = mk("out_scr", 3)
            if cfg.get("dbg"):
                self.dout("dbg", [1024, D])
            self.load_consts()
            self.bcol = kb.sb("bcol", [128, 66], F32, glob=True)
            kb.dma("sp", self.bcol.t[:], self.ins["bcol_in"].ap, self.ins["bcol_in"], self.bcol)
            self.bcol_qa = kb.sb("bcol_qa", [128, 8], F32, glob=True)
            kb.dma("sp", self.bcol_qa.t[:], self.ins["bcol_qa_in"].ap, self.ins["bcol_qa_in"], self.bcol_qa)
            self.brow = kb.sb("brow", [1, 1152], BF16, glob=True)
            kb.dma("pool", self.brow.t[:], self.ins["b_row_in"].ap, self.ins["b_row_in"], self.brow)
            kb.barrier()
            if start <= 1 <= stop:
                self.stage1()
            if start <= 2 <= stop:
                self.stage2()
            if start <= 3 <= stop:
                self.stage3()
            kb.barrier()
        return nc


def t5_bucket_np(rel):
    half, max_exact = 16, 8
    base = np.where(rel > 0, half, 0)
    n = np.abs(rel)
    nf = np.maximum(n, 1).astype(np.float32)
    large = max_exact + (np.log(nf / max_exact) / np.float32(np.log(128 / max_exact)) * (half - max_exact)).astype(np.int32)
    large = np.minimum(large, half - 1)
    return base + np.where(n < max_exact, n, large)


def host_consts():
    ident = np.eye(128, dtype=np.float32)
    sp, s = np.meshgrid(np.arange(128), np.arange(128), indexing="ij")
    negtri = -(sp >= s).astype(np.float32)
    trix = (sp < s).astype(np.float32)
    return dict(c_ident=ident, c_negtri=negtri, c_trix=trix)


def prep_core_inputs(c, inp):
    b, cp = c // 4, c % 4
    x = inp["x"][b]
    blocks = [4 * i + cp for i in range(8)]
    x_own = np.concatenate([x[q * 128:(q + 1) * 128] for q in blocks], 0)
    x_prev = np.concatenate([x[(q - 1) * 128:q * 128] if q > 0 else np.zeros((128, D), np.float32) for q in blocks], 0)
    d = dict(x_own=np.ascontiguousarray(x_own), x_prev=np.ascontiguousarray(x_prev), x_ctx=np.ascontiguousarray(x))
    if "mem" in inp:
        d["mem"] = inp["mem"][b]
    s_, q_ = np.meshgrid(np.arange(128), np.arange(128), indexing="ij")
    m = np.zeros((128, 4, 128), np.float32)
    for r in range(4):
        if r < cp:
            m[:, r, :] = 1.0
        elif r == cp:
            m[:, r, :] = (s_ < q_).astype(np.float32)
    d["sbmask"] = m
    qi = np.arange(128)[:, None]
    kj = np.arange(256)[None, :]
    bucket = t5_bucket_np(kj - 128 - qi)
    bias = inp["rel_bias"][bucket]
    dchunk = (kj // 64 - 2) - qi // 64
    band = (dchunk <= 0) & (dchunk >= -2)
    tab = np.where(band[:, :, None], bias, np.float32(NEG)).astype(np.float32)
    d["swab"] = np.ascontiguousarray(np.transpose(tab, (0, 2, 1)))
    pm0 = np.zeros((128, 256), np.float32)
    if cp == 0:
        pm0[:, :128] = NEG
    d["pm0"] = pm0
    return d


def common_inputs(inp):
    b_in = inp["b_in"][0]
    bcol = np.ascontiguousarray(b_in.reshape(66, 128).T)
    qa = b_in[0:1024].reshape(2, 8, 64)
    bcol_qa = np.ascontiguousarray(np.transpose(qa, (1, 0, 2)).reshape(8, 128).T)
    d = dict(w_in=inp["w_in"][0], bcol_in=bcol, bcol_qa_in=bcol_qa, b_row_in=np.concatenate([b_in[1152:1280], b_in[3328:4352]])[None, :],
             ln_in_g=inp["ln_in_g"][None, :], ln_in_b=inp["ln_in_b"][None, :], ln1_g=inp["ln1_g"], ln1_b=inp["ln1_b"],
             attn_sinks=inp["attn_sinks"], w_a_out=inp["w_a_out"][0], w_b_out=inp["w_b_out"][0], w_mix_out=inp["w_mix_out"][0])
    d.update(host_consts())
    d["c_iota"] = np.tile(np.arange(256, dtype=np.float32)[None, :], (128, 1))
    for k in ("w_xq", "w_xkv", "w_xo", "w_router", "w_up", "w_down"):
        if k in inp:
            d[k] = inp[k][0]
    for k in ("ln2_g", "ln2_b", "ln3_g", "ln3_b", "b_router"):
        if k in inp:
            d[k] = inp[k]
    if "b_up" in inp:
        d["bup_col"] = np.ascontiguousarray(np.transpose(inp["b_up"][0].reshape(NE, 32, 128), (2, 0, 1)))
        d["b_down"] = inp["b_down"][0]
    return d


def run(inp, cfg):
    prog = Prog(cfg)
    nc = prog.build()
    com = common_inputs(inp)
    in_maps = []
    for c in range(8):
        d = dict(com)
        d.update(prep_core_inputs(c, inp))
        in_maps.append({k: np.ascontiguousarray(v, dtype=np.float32) for k, v in d.items() if k in prog.ins})
    res = run_bass_kernel_spmd(nc, in_maps, core_ids=list(range(8)))
    return res.results


def assemble(results, key="out"):
    out = np.zeros((2, 4096, D), np.float32)
    for c in range(8):
        b, cp = c // 4, c % 4
        r = results[c][key]
        for i in range(8):
            q = 4 * i + cp
            out[b, q * 128:(q + 1) * 128] = r[i * 128:(i + 1) * 128]
    return out


def kernel(**inputs):
    inp = {k: np.asarray(v) for k, v in inputs.items()}
    results = run(inp, dict())
    return assemble(results)
```

```python
import numpy as np
from contextlib import ExitStack
import concourse.bass as bass
import concourse.mybir as mybir
from concourse.bass_utils import run_bass_kernel_spmd

F32 = mybir.dt.float32
BF16 = mybir.dt.bfloat16
AF = mybir.ActivationFunctionType
ALU = mybir.AluOpType
AX = mybir.AxisListType

D = 2048
NE = 32
CAP = 192
ALPHA = 2.0 ** 0.25
EPS = 1e-5
NEG = -30000.0
ARENA = 206 * 1024


class Sem:
    def __init__(s, h):
        s.h = h
        s.total = 0


class Buf:
    def __init__(s, name, t=None):
        s.name = name
        s.t = t
        s.w = {}
        s.r = {}
        s.dsem = None


class KB:
    def __init__(s, nc, es, ndsem=92):
        s.nc = nc
        s.E = dict(pe=nc.tensor, act=nc.scalar, dve=nc.vector, pool=nc.gpsimd, sp=nc.sync)
        s.sem = {k: es.enter_context(nc.semaphore("es_" + k)) for k in s.E}
        s.cnt = {k: 0 for k in s.E}
        s.known = {k: {} for k in s.E}
        s.free_dsems = [Sem(es.enter_context(nc.semaphore("ds%d" % i))) for i in range(ndsem)]
        s.used_dsems = []
        s.phase_bufs = []
        s.banks = [Buf("bank%d" % i, es.enter_context(nc.psum_tensor("bank%d" % i, [128, 512], F32))) for i in range(8)]
        s.bank_i = 0
        s.es = es
        s.pes = None
        s.arena_bytes = ARENA
        s.arena = es.enter_context(nc.sbuf_tensor("arena", [128, ARENA // 4], F32))
        s.alloc = []

    def bank(s):
        b = s.banks[s.bank_i % 8]
        s.bank_i += 1
        return b

    def sb(s, name, shape, dt, glob=False, grp=None):
        nb = 2 if dt == BF16 else 4
        free = 1
        for d_ in shape[1:]:
            free *= d_
        size = (free * nb + 63) // 64 * 64
        off = None
        cur = 0
        for (o, sz) in sorted(s.alloc):
            if o - cur >= size:
                off = cur
                break
            cur = max(cur, o + sz)
        if off is None:
            if s.arena_bytes - cur >= size:
                off = cur
            else:
                raise AssertionError("arena OOM allocating %s %s (%d B); used=%s" % (name, shape, size, sum(z for _, z in s.alloc)))
        s.alloc.append((off, size))
        ap = s.arena[0:shape[0], off // 4:(off + size) // 4]
        if dt == BF16:
            ap = ap.bitcast(BF16)
        ap = ap[:, 0:free]
        if len(shape) == 3:
            ap = ap.rearrange("p (a b) -> p a b", a=shape[1])
        elif len(shape) == 4:
            ap = ap.rearrange("p (a b c) -> p a b c", a=shape[1], b=shape[2])
        b = Buf(name, ap)
        b.alloc = (off, size)
        if grp is not None:
            grp["bufs"].append(b)
        elif not glob and s.pes is not None:
            s.phase_bufs.append(b)
        return b

    def newgrp(s):
        return dict(bufs=[])

    def freegrp(s, g):
        for b in g["bufs"]:
            s.alloc.remove(b.alloc)
        g["bufs"] = []

    def _wait(s, eng, need):
        E = s.E[eng]
        for key, (h, val) in need.items():
            if s.known[eng].get(key, 0) < val:
                E.wait_ge(h, val)
                s.known[eng][key] = val

    def op(s, eng, fn, reads=(), writes=(), pwrites=()):
        need = {}

        def add(evs, raw):
            for key, (h, val, e) in evs.items():
                if e == eng and (eng == "pe" or not raw):
                    continue
                if key not in need or need[key][1] < val:
                    need[key] = (h, val)
        for b in reads:
            add(b.w, True)
        for b in writes:
            add(b.w, False)
            add(b.r, False)
        for b in pwrites:
            add(b.r, False)
        s._wait(eng, need)
        inst = fn(s.E[eng])
        s.cnt[eng] += 1
        inst.then_inc(s.sem[eng], 1)
        ev = (s.sem[eng], s.cnt[eng], eng)
        for b in reads:
            b.r[eng] = ev
        for b in writes:
            b.w = {eng: ev}
            b.r = {}
        for b in pwrites:
            b.w[eng] = ev

    def dma(s, q, out_ap, in_ap, src, dst, partial=False):
        need = {}

        def add(evs):
            for key, (h, val, e) in evs.items():
                if key not in need or need[key][1] < val:
                    need[key] = (h, val)
        add(src.w)
        add(dst.r)
        if not partial:
            add(dst.w)
        s._wait(q, need)
        inst = s.E[q].dma_start(out=out_ap, in_=in_ap)
        if dst.dsem is None:
            dst.dsem = s.free_dsems.pop()
            s.used_dsems.append((dst, dst.dsem))
        sem = dst.dsem
        sem.total += 16
        inst.then_inc(sem.h, 16)
        key = ("d", id(sem))
        ev = (sem.h, sem.total, None)
        src.r[key] = ev
        if partial:
            dst.w[key] = ev
        else:
            dst.w = {key: ev}
            dst.r = {}

    def barrier(s):
        for eng, E in s.E.items():
            need = {}
            for o in s.E:
                if s.cnt[o] > 0:
                    need[o] = (s.sem[o], s.cnt[o])
            for (b, sem) in s.used_dsems:
                if sem.total > 0:
                    need[("d", id(sem))] = (sem.h, sem.total)
            s._wait(eng, need)
        for (b, sem) in s.used_dsems:
            b.dsem = None
            s.free_dsems.append(sem)
        s.used_dsems = []

    def phase_begin(s):
        s.pes = True
        s.phase_bufs = []

    def phase_end(s):
        s.barrier()
        for b in s.phase_bufs:
            s.alloc.remove(b.alloc)
        s.pes = None
        s.phase_bufs = []


def chunks_of(c_lo, c_hi, step=512):
    out = []
    c = c_lo
    while c < c_hi:
        out.append((c, min(c + step, c_hi)))
        c += step
    return out


class Prog:
    def __init__(self, cfg):
        self.cfg = cfg
        self.nc = bass.Bass("TRN2", target_bir_lowering=False)
        self.ins = {}
        self.outs = {}

    def din(self, name, shape, dt=F32):
        t = self.nc.dram_tensor(name, list(shape), dt, kind="ExternalInput")
        b = Buf(name, t)
        b.ap = t.ap()
        self.ins[name] = b
        return b

    def dout(self, name, shape, dt=F32):
        t = self.nc.dram_tensor(name, list(shape), dt, kind="ExternalOutput")
        b = Buf(name, t)
        b.ap = t.ap()
        self.outs[name] = b
        return b

    def dscr(self, name, shape, dt):
        t = self.nc.dram_tensor(name, list(shape), dt, kind="Internal")
        b = Buf(name, t)
        b.ap = t.ap()
        return b

    def load_consts(self):
        kb = self.kb
        c = self.din("c_ident", [128, 128])
        self.ident_bf = kb.sb("ident_bf", [128, 128], BF16, glob=True)
        kb.dma("pool", self.ident_bf.t[:], c.ap, c, self.ident_bf)
        self.ident_f = kb.sb("ident_f", [128, 128], F32, glob=True)
        kb.dma("sp", self.ident_f.t[:], c.ap, c, self.ident_f)
        c2 = self.din("c_negtri", [128, 128])
        self.negtri = kb.sb("negtri", [128, 128], BF16, glob=True)
        kb.dma("pool", self.negtri.t[:], c2.ap, c2, self.negtri)
        c3 = self.din("c_trix", [128, 128])
        self.trix = kb.sb("trix", [128, 128], BF16, glob=True)
        kb.dma("pool", self.trix.t[:], c3.ap, c3, self.trix)
        self.ones_bf = kb.sb("ones_bf", [128, 128], BF16, glob=True)
        kb.op("dve", lambda e: e.memset(self.ones_bf.t[:], 1.0), writes=[self.ones_bf])
        self.ones_f = kb.sb("ones_f", [128, 128], F32, glob=True)
        kb.op("dve", lambda e: e.memset(self.ones_f.t[:], 1.0), writes=[self.ones_f])
        self.epsc = kb.sb("epsc", [128, 1], F32, glob=True)
        kb.op("dve", lambda e: e.memset(self.epsc.t[:], EPS), writes=[self.epsc])

    def load_rows(self, name_g, name_b):
        kb = self.kb
        g = self.ins[name_g]
        b = self.ins[name_b]
        gr = kb.sb("gr_" + name_g, [128, D], F32)
        br = kb.sb("br_" + name_b, [128, D], F32)
        kb.dma("sp", gr.t[:], g.ap.partition_broadcast(128), g, gr)
        kb.dma("sp", br.t[:], b.ap.partition_broadcast(128), b, br)
        return gr, br

    def make_ln_bufs(self, nset=2):
        kb = self.kb
        sets = []
        for i in range(nset):
            sets.append(dict(
                x=kb.sb("ln_x%d" % i, [128, D], F32),
                xn=kb.sb("ln_xn%d" % i, [128, D], F32),
                hb=kb.sb("ln_hb%d" % i, [128, D], BF16),
                st=kb.sb("ln_st%d" % i, [128, 4, 6], F32),
                mv=kb.sb("ln_mv%d" % i, [128, 2], F32),
                sc=kb.sb("ln_sc%d" % i, [128, 4], F32),
            ))
        self.ln_sets = sets
        self.ln_i = 0

    def ln_core(self, S, gr, br, want_h32, want_hb=True):
        kb = self.kb
        x, xn, hb, st, mv, sc = S["x"], S["xn"], S["hb"], S["st"], S["mv"], S["sc"]
        for j in range(4):
            kb.op("dve", lambda e, j=j: e.bn_stats(out=st.t[:, j, :], in_=x.t[:, j * 512:(j + 1) * 512]),
                  reads=[x], pwrites=[st] if j else (), writes=() if j else [st])
        kb.op("dve", lambda e: e.bn_aggr(out=mv.t[:], in_=st.t[:].rearrange("p a b -> p (a b)")), reads=[st], writes=[mv])
        kb.op("act", lambda e: e.activation(out=sc.t[:, 0:1], in_=mv.t[:, 1:2], func=AF.Ln, bias=self.epsc.t[:, 0:1], scale=1.0),
              reads=[mv, self.epsc], writes=[sc])
        kb.op("act", lambda e: e.activation(out=sc.t[:, 1:2], in_=sc.t[:, 0:1], func=AF.Exp, scale=-0.5),
              reads=[sc], pwrites=[sc])
        kb.op("dve", lambda e: e.tensor_scalar(out=sc.t[:, 2:3], in0=mv.t[:, 0:1], scalar1=sc.t[:, 1:2], scalar2=-1.0,
                                               op0=ALU.mult, op1=ALU.mult), reads=[mv, sc], pwrites=[sc])
        kb.op("act", lambda e: e.activation(out=xn.t[:], in_=x.t[:], func=AF.Identity, bias=sc.t[:, 2:3], scale=sc.t[:, 1:2]),
              reads=[x, sc], writes=[xn])
        kb.op("pool", lambda e: e.tensor_tensor(out=xn.t[:], in0=xn.t[:], in1=gr.t[:], op=ALU.mult), reads=[xn, gr], writes=[xn])
        if want_h32:
            kb.op("dve", lambda e: e.tensor_tensor(out=x.t[:], in0=xn.t[:], in1=br.t[:], op=ALU.add), reads=[xn, br], writes=[x])
            if want_hb:
                kb.op("act", lambda e: e.activation(out=hb.t[:], in_=x.t[:], func=AF.Copy), reads=[x], writes=[hb])
        else:
            kb.op("dve", lambda e: e.tensor_tensor(out=hb.t[:], in0=xn.t[:], in1=br.t[:], op=ALU.add), reads=[xn, br], writes=[hb])

    def to_fm(self, src, dst, off, nchunk=16, src_col0=0, evac_alt=0):
        kb = self.kb
        for h0 in range(0, nchunk, 8):
            n = min(8, nchunk - h0)
            bk = kb.bank()
            bv = bk.t[:].bitcast(BF16)
            for k in range(n):
                kk = h0 + k
                kb.op("pe", lambda e, k=k, kk=kk: e.transpose(out=bv[:, k * 128:(k + 1) * 128],
                                                               in_=src.t[:, src_col0 + kk * 128: src_col0 + (kk + 1) * 128],
                                                               identity=self.ident_bf.t[:]),
                      reads=[src, self.ident_bf], writes=[bk] if k == 0 else (), pwrites=() if k == 0 else [bk])
            srcv = bv[:, 0:n * 128].rearrange("p (a b) -> p a b", a=n)
            dstv = dst.t[:, h0:h0 + n, off:off + 128]
            if (evac_alt + h0 // 8) % 2 == 0:
                kb.op("dve", lambda e: e.tensor_copy(out=dstv, in_=srcv), reads=[bk], pwrites=[dst])
            else:
                kb.op("act", lambda e: e.activation(out=dstv, in_=srcv, func=AF.Copy), reads=[bk], pwrites=[dst])

    def ln_from_dram(self, src_ap, src_buf, gr, br, dst_fm, off, h32_dst=None, h32_buf=None):
        kb = self.kb
        S = self.ln_sets[self.ln_i % len(self.ln_sets)]
        self.ln_i += 1
        kb.dma("sp", S["x"].t[:], src_ap, src_buf, S["x"])
        self.ln_core(S, gr, br, want_h32=h32_dst is not None)
        if h32_dst is not None:
            kb.dma("sp", h32_dst, S["x"].t[:], S["x"], h32_buf, partial=True)
        self.to_fm(S["hb"], dst_fm, off, evac_alt=self.ln_i)

    def load_w(self, dst, w_ap, wbuf, split=1):
        kb = self.kb
        src = w_ap.rearrange("(c p) n -> p c n", p=128)
        nk = src.shape[1]
        step = (nk + split - 1) // split
        for i, k0 in enumerate(range(0, nk, step)):
            k1 = min(nk, k0 + step)
            kb.dma("pool", dst.t[:, k0:k1, :], src[:, k0:k1, :], wbuf, dst, partial=(i > 0))

    def mm_fm(self, bank, w, wcol, xT, tok0, ntok, nk, reads_extra=()):
        kb = self.kb
        for k in range(nk):
            kb.op("pe", lambda e, k=k: e.matmul(bank.t[:, 0:ntok], lhsT=w.t[:, k, wcol:wcol + 128], rhs=xT.t[:, k, tok0:tok0 + ntok],
                                                start=(k == 0), stop=(k == nk - 1)),
                  reads=[w, xT], writes=[bank] if k == 0 else (), pwrites=() if k == 0 else [bank])

    def mm_tm(self, bank, xT, tok0, w, wcol, ncol, nk, bias_row=None, bias_col0=0):
        kb = self.kb
        first = True
        if bias_row is not None:
            kb.op("pe", lambda e: e.matmul(bank.t[:, 0:ncol], lhsT=self.ones_bf.t[0:1, :], rhs=bias_row.t[0:1, bias_col0:bias_col0 + ncol],
                                           start=True, stop=False),
                  reads=[self.ones_bf, bias_row], writes=[bank])
            first = False
        for k in range(nk):
            kb.op("pe", lambda e, k=k, st=(first and k == 0): e.matmul(bank.t[:, 0:ncol], lhsT=xT.t[:, k, tok0:tok0 + 128],
                                                                       rhs=w.t[:, k, wcol:wcol + ncol], start=st, stop=(k == nk - 1)),
                  reads=[w, xT], writes=[bank] if (first and k == 0) else (), pwrites=() if (first and k == 0) else [bank])

    def stage1(self):
        kb = self.kb
        cfg = self.cfg
        x_own, x_prev, x_ctx = self.ins["x_own"], self.ins["x_prev"], self.ins["x_ctx"]
        w_in = self.ins["w_in"]
        QA0, KA0, VA0, QB0, KB0, VB0, GA0, GB0 = 0, 1024, 1152, 1280, 2304, 3328, 4352, 6400
        bcol = self.bcol
        brow = self.brow

        kb.phase_begin()
        gr, br = self.load_rows("ln_in_g", "ln_in_b")
        self.make_ln_bufs(2)
        wk = kb.sb("wk", [128, 16, 1024], BF16)
        wv = kb.sb("wv", [128, 16, 1024], BF16)
        self.load_w(wk, w_in.ap[:, KB0:KB0 + 1024], w_in, split=2)
        self.load_w(wv, w_in.ap[:, VB0:VB0 + 1024], w_in, split=2)
        hTg = [kb.sb("hTg%d" % i, [128, 16, 512], BF16) for i in range(2)]
        kst = [kb.sb("kst%d" % i, [128, 8, 512], BF16) for i in range(2)]
        vst = [kb.sb("vst%d" % i, [128, 4, 1024], BF16) for i in range(2)]
        kT_scr, V_scr = self.kT_scr, self.V_scr
        for g in range(8):
            hT = hTg[g % 2]
            for tt in range(4):
                r0 = g * 512 + tt * 128
                self.ln_from_dram(x_ctx.ap[r0:r0 + 128, :], x_ctx, gr, br, hT, tt * 128)
            ks = kst[g % 2]
            for fc in range(8):
                bk = kb.bank()
                self.mm_fm(bk, wk, fc * 128, hT, 0, 512, 16)
                kb.op("act", lambda e, fc=fc, bk=bk: e.activation(out=ks.t[:, fc, :], in_=bk.t[:, :], func=AF.Identity,
                                                                   bias=bcol.t[:, 18 + fc:19 + fc], scale=1.0),
                      reads=[bk, bcol], pwrites=[ks] if fc else (), writes=() if fc else [ks])
            kb.dma("sp", kT_scr.ap.rearrange("f p t -> p f t")[:, :, g * 512:(g + 1) * 512], ks.t[:], ks, kT_scr, partial=True)
            vs = vst[g % 2]
            for tt in range(4):
                for nch in range(2):
                    bk = kb.bank()
                    self.mm_tm(bk, hT, tt * 128, wv, nch * 512, 512, 16, bias_row=brow, bias_col0=128 + nch * 512)
                    kb.op("dve", lambda e, tt=tt, nch=nch, bk=bk: e.tensor_copy(out=vs.t[:, tt, nch * 512:(nch + 1) * 512], in_=bk.t[:, :]),
                          reads=[bk], pwrites=[vs] if (tt or nch) else (), writes=() if (tt or nch) else [vs])
            kb.dma("sp", V_scr.ap[g * 4:(g + 1) * 4].rearrange("t p n -> p t n"), vs.t[:], vs, V_scr, partial=True)
        kb.phase_end()

        kb.phase_begin()
        gr, br = self.load_rows("ln_in_g", "ln_in_b")
        self.make_ln_bufs(2)
        P = {}
        self.P1 = P
        self.gA = kb.newgrp(); self.gQB = kb.newgrp(); self.gSWA = kb.newgrp(); self.gT = kb.newgrp()
        hT_own = kb.sb("hT_own", [128, 16, 1024], BF16, grp=self.gA)
        P["hT_own"] = hT_own
        gPrev = kb.newgrp()
        hT_prev = kb.sb("hT_prev", [128, 16, 1024], BF16, grp=gPrev)
        for i in range(8):
            self.ln_from_dram(x_own.ap[i * 128:(i + 1) * 128, :], x_own, gr, br, hT_own, i * 128,
                              h32_dst=self.h0_scr.ap[i * 128:(i + 1) * 128, :], h32_buf=self.h0_scr)
        for i in range(8):
            self.ln_from_dram(x_prev.ap[i * 128:(i + 1) * 128, :], x_prev, gr, br, hT_prev, i * 128)
        kb.phase_end()
        kb.phase_begin()
        qBT = kb.sb("qBT", [128, 8, 1024], BF16, grp=self.gQB)
        qAT = kb.sb("qAT", [128, 8, 1024], BF16, grp=self.gSWA)
        kAT = kb.sb("kAT", [128, 2, 1024], BF16, grp=self.gSWA)
        vA = kb.sb("vA", [128, 2, 8, 128], BF16, grp=self.gSWA)
        P.update(qBT=qBT, qAT=qAT, kAT=kAT, vA=vA)
        wq = kb.sb("wq", [128, 16, 1024], BF16)
        self.load_w(wq, w_in.ap[:, QB0:QB0 + 1024], w_in, split=2)
        for fc in range(8):
            for th in range(2):
                bk = kb.bank()
                self.mm_fm(bk, wq, fc * 128, hT_own, th * 512, 512, 16)
                kb.op("dve", lambda e, fc=fc, th=th, bk=bk: e.tensor_scalar(out=qBT.t[:, fc, th * 512:(th + 1) * 512], in0=bk.t[:, :],
                                                                           scalar1=bcol.t[:, 10 + fc:11 + fc], scalar2=0.125,
                                                                           op0=ALU.add, op1=ALU.mult),
                      reads=[bk, bcol], pwrites=[qBT])
        wqa = wq
        wqa_t = wq.t[:].rearrange("p c (j m) -> p c j m", m=128)
        for a in range(2):
            src = w_in.ap[:, QA0 + a * 512:QA0 + (a + 1) * 512].rearrange("(c p) (j d) -> p c j d", p=128, d=64)
            for c0 in range(16):
                kb.dma("pool", wqa_t[:, c0, :, a * 64:(a + 1) * 64], src[:, c0], w_in, wqa, partial=not (a == 0 and c0 == 0))
        wqa_v = Buf("wqa_v", None)
        for fc in range(8):
            for th in range(2):
                bk = kb.bank()
                for k in range(16):
                    kb.op("pe", lambda e, k=k, fc=fc, th=th, bk=bk: e.matmul(bk.t[:, :], lhsT=wqa_t[:, k, fc, :], rhs=hT_own.t[:, k, th * 512:(th + 1) * 512],
                                                                             start=(k == 0), stop=(k == 15)),
                          reads=[wqa, hT_own], writes=[bk] if k == 0 else (), pwrites=() if k == 0 else [bk])
                kb.op("dve", lambda e, fc=fc, th=th, bk=bk: e.tensor_scalar(out=qAT.t[:, fc, th * 512:(th + 1) * 512], in0=bk.t[:, :],
                                                                           scalar1=self.bcol_qa.t[:, fc:fc + 1], scalar2=0.125,
                                                                           op0=ALU.add, op1=ALU.mult),
                      reads=[bk, self.bcol_qa], pwrites=[qAT])
        wkv = kb.sb("wkv", [128, 16, 256], BF16)
        self.load_w(wkv, w_in.ap[:, KA0:KA0 + 256], w_in)
        for which, hT in ((0, hT_prev), (1, hT_own)):
            for th in range(2):
                bk = kb.bank()
                self.mm_fm(bk, wkv, 0, hT, th * 512, 512, 16)
                kb.op("act", lambda e, th=th, bk=bk, which=which: e.activation(out=kAT.t[:, which, th * 512:(th + 1) * 512], in_=bk.t[:, :],
                                                                             func=AF.Identity, bias=bcol.t[:, 8:9], scale=1.0),
                      reads=[bk, bcol], pwrites=[kAT])
            for i in range(8):
                bk = kb.bank()
                self.mm_tm(bk, hT, i * 128, wkv, 128, 128, 16, bias_row=brow, bias_col0=0)
                kb.op("dve", lambda e, i=i, bk=bk, which=which: e.tensor_copy(out=vA.t[:, which, i, :], in_=bk.t[:, 0:128]),
                      reads=[bk], pwrites=[vA])
        kb.phase_end()
        kb.freegrp(gPrev)

        kb.phase_begin()
        swab = kb.sb("swab", [128, 16, 256], F32)
        kb.dma("sp", swab.t[:], self.ins["swab"].ap, self.ins["swab"], swab)
        pm0 = kb.sb("pm0", [128, 256], F32)
        kb.dma("sp", pm0.t[:], self.ins["pm0"].ap, self.ins["pm0"], pm0)
        sinkb = kb.sb("sinkb", [128, 16], F32)
        kb.dma("sp", sinkb.t[:], self.ins["attn_sinks"].ap.partition_broadcast(128), self.ins["attn_sinks"], sinkb)
        swa_tm = [kb.sb("swa_tm%d" % i, [128, 1024], BF16) for i in range(2)]
        swaT = kb.sb("swaT", [128, 8, 1024], BF16, grp=self.gT)
        P["swaT"] = swaT
        NS = 3
        dbgt = [kb.sb('dbgt%d' % i, [128, 1024], F32) for i in range(2)]
        l32 = [kb.sb("l32_%d" % i, [128, 256], F32) for i in range(NS)]
        pbf = [kb.sb("pbf_%d" % i, [128, 256], BF16) for i in range(NS)]
        pT = [kb.sb("pT_%d" % i, [128, 2, 128], BF16) for i in range(NS)]
        sm = [kb.sb("sm_%d" % i, [128, 8], F32) for i in range(NS)]
        qAT, kAT, vA = P["qAT"], P["kAT"], P["vA"]
        it = 0
        for i in range(8):
            so = swa_tm[i % 2]
            for h in range(16):
                s = it % NS
                it += 1
                j, a = h % 8, h // 8
                Z = kb.bank()
                for c in range(2):
                    kb.op("pe", lambda e, c=c: e.matmul(Z.t[:, c * 128:(c + 1) * 128], lhsT=qAT.t[a * 64:(a + 1) * 64, j, i * 128:(i + 1) * 128],
                                                        rhs=kAT.t[a * 64:(a + 1) * 64, c, i * 128:(i + 1) * 128], start=True, stop=True),
                          reads=[qAT, kAT], writes=[Z] if c == 0 else (), pwrites=() if c == 0 else [Z])
                kb.op("dve", lambda e: e.tensor_tensor(out=l32[s].t[:], in0=Z.t[:, 0:256], in1=swab.t[:, h, :], op=ALU.add),
                      reads=[Z, swab], writes=[l32[s]])
                if i == 0:
                    kb.op("dve", lambda e: e.tensor_tensor(out=l32[s].t[:], in0=l32[s].t[:], in1=pm0.t[:], op=ALU.add),
                          reads=[l32[s], pm0], writes=[l32[s]])
                kb.op("dve", lambda e: e.reduce_max(out=sm[s].t[:, 0:1], in_=l32[s].t[:], axis=AX.X), reads=[l32[s]], writes=[sm[s]])
                kb.op("dve", lambda e: e.tensor_scalar(out=sm[s].t[:, 1:2], in0=sm[s].t[:, 0:1], scalar1=-1.0, scalar2=None, op0=ALU.mult),
                      reads=[sm[s]], pwrites=[sm[s]])
                kb.op("act", lambda e: e.activation(out=pbf[s].t[:], in_=l32[s].t[:], func=AF.Exp, bias=sm[s].t[:, 1:2], scale=1.0,
                                                    accum_out=sm[s].t[:, 2:3]),
                      reads=[l32[s], sm[s]], writes=[pbf[s]], pwrites=[sm[s]])
                kb.op("act", lambda e: e.activation(out=sm[s].t[:, 3:4], in_=sinkb.t[:, h:h + 1], func=AF.Exp, bias=sm[s].t[:, 1:2], scale=1.0),
                      reads=[sinkb, sm[s]], pwrites=[sm[s]])
                kb.op("dve", lambda e: e.tensor_tensor(out=sm[s].t[:, 4:5], in0=sm[s].t[:, 2:3], in1=sm[s].t[:, 3:4], op=ALU.add),
                      reads=[sm[s]], pwrites=[sm[s]])
                kb.op("dve", lambda e: e.reciprocal(out=sm[s].t[:, 5:6], in_=sm[s].t[:, 4:5]), reads=[sm[s]], pwrites=[sm[s]])
                PT = kb.bank()
                ptv = PT.t[:].bitcast(BF16)
                for c in range(2):
                    kb.op("pe", lambda e, c=c: e.transpose(out=ptv[:, c * 128:(c + 1) * 128], in_=pbf[s].t[:, c * 128:(c + 1) * 128],
                                                           identity=self.ident_bf.t[:]),
                          reads=[pbf[s], self.ident_bf], writes=[PT] if c == 0 else (), pwrites=() if c == 0 else [PT])
                kb.op("act", lambda e: e.activation(out=pT[s].t[:].rearrange("p a b -> p (a b)"), in_=ptv[:, 0:256], func=AF.Copy),
                      reads=[PT], writes=[pT[s]])
                O = kb.bank()
                for c in range(2):
                    kb.op("pe", lambda e, c=c: e.matmul(O.t[:, 0:64], lhsT=pT[s].t[:, c, :], rhs=vA.t[:, c, i, a * 64:(a + 1) * 64],
                                                        start=(c == 0), stop=(c == 1)),
                          reads=[pT[s], vA], writes=[O] if c == 0 else (), pwrites=() if c == 0 else [O])
                kb.op("dve", lambda e: e.tensor_scalar(out=so.t[:, h * 64:(h + 1) * 64], in0=O.t[:, 0:64], scalar1=sm[s].t[:, 5:6], scalar2=None,
                                                       op0=ALU.mult),
                      reads=[O, sm[s]], pwrites=[so] if h else (), writes=() if h else [so])
            self.to_fm(so, swaT, i * 128, nchunk=8, evac_alt=i)
            if cfg.get("dbg") == "swa":
                dt_ = dbgt[i % 2]
                kb.op("dve", lambda e, dt_=dt_: e.tensor_copy(out=dt_.t[:], in_=so.t[:]), reads=[so], writes=[dt_])
                kb.dma("sp", self.outs["dbg"].ap[i * 128:(i + 1) * 128, 0:1024], dt_.t[:], dt_, self.outs["dbg"], partial=True)
        kb.phase_end()
        kb.freegrp(self.gSWA)

        kb.phase_begin()
        sbmask = kb.sb("sbmask", [128, 4, 128], BF16)
        kb.dma("pool", sbmask.t[:], self.ins["sbmask"].ap, self.ins["sbmask"], sbmask)
        accs = [kb.sb("sbacc%d" % i, [128, 1024], F32) for i in range(8)]
        for i in range(8):
            kb.op("pool", lambda e, i=i: e.memset(accs[i].t[:], 0.0), writes=[accs[i]])
        kTp = [kb.sb("kTp%d" % i, [128, 4096], BF16) for i in range(2)]
        vP = [kb.sb("vP%d" % i, [128, 32, 128], BF16) for i in range(2)]
        carry = kb.sb("carry", [128, 8], F32)
        Eb = [kb.sb("Eb%d" % i, [128, 8], F32) for i in range(2)]
        NU = 3
        e32 = [kb.sb("e32_%d" % i, [128, 512], F32) for i in range(NU)]
        spb = [kb.sb("spb_%d" % i, [128, 512], BF16) for i in range(NU)]
        wb = [kb.sb("wb_%d" % i, [128, 512], BF16) for i in range(NU)]
        n_hp = cfg.get("n_hp", 8)
        units = []
        for hp in range(n_hp):
            for hh in range(2):
                for kbi in range(31, -1, -1):
                    g = kbi // 4
                    chs = chunks_of(g * 128, 1024)
                    for ci, (c0, c1) in enumerate(chs):
                        units.append(dict(hp=hp, hh=hh, kb=kbi, g=g, r=kbi % 4, c0=c0, c1=c1, first=(ci == 0), last=(ci == len(chs) - 1),
                                          newhead=(kbi == 31 and ci == 0), newpair=(kbi == 31 and ci == 0 and hh == 0)))
        Abanks = [kb.banks[i] for i in range(5)]
        Obanks = [kb.banks[5], kb.banks[6]]
        Crun = kb.banks[7]
        zeros_bf = kb.sb("zeros_bf", [128, 128], BF16)
        kb.op("dve", lambda e: e.memset(zeros_bf.t[:], 0.0), writes=[zeros_bf])
        Eb4 = [kb.sb("Eb4_%d" % i, [128, 8], F32) for i in range(4)]
        ek = -1
        for u in units:
            if u["first"]:
                ek += 1
            u["ek"] = ek

        def st1(u, ui):
            hp, hh = u["hp"], u["hh"]
            if u["newpair"]:
                kb.dma("sp", kTp[hp % 2].t[:], self.kT_scr.ap[hp], self.kT_scr, kTp[hp % 2])
                vsrc = self.V_scr.ap.rearrange("t p n -> p t n")
                for t0 in range(0, 32, 8):
                    kb.dma("sp", vP[hp % 2].t[:, t0:t0 + 8, :], vsrc[:, t0:t0 + 8, hp * 128:(hp + 1) * 128], self.V_scr, vP[hp % 2], partial=(t0 > 0))
            A = Abanks[ui % 5]
            n = u["c1"] - u["c0"]
            ps0 = hh * 64
            kk = kTp[hp % 2]
            kb.op("pe", lambda e: e.matmul(A.t[:, 0:n], lhsT=kk.t[ps0:ps0 + 64, u["kb"] * 128:(u["kb"] + 1) * 128],
                                           rhs=qBT_.t[ps0:ps0 + 64, hp, u["c0"]:u["c1"]], start=True, stop=True),
                  reads=[kk, qBT_], writes=[A])
            if u["first"]:
                kb.op("pe", lambda e: e.matmul(A.t[:, 0:128], lhsT=self.ident_bf.t[:], rhs=sbmask.t[:, u["r"], :], start=False, stop=True),
                      reads=[self.ident_bf, sbmask], pwrites=[A])

        def st2(u, ui):
            A = Abanks[ui % 5]
            s = ui % NU
            n = u["c1"] - u["c0"]
            kb.op("act", lambda e: e.activation(out=e32[s].t[:, 0:n], in_=A.t[:, 0:n], func=AF.Exp), reads=[A], writes=[e32[s]])
            kb.op("act", lambda e: e.activation(out=spb[s].t[:, 0:n], in_=e32[s].t[:, 0:n], func=AF.Ln, bias=self.ones_f.t[:, 0:1], scale=1.0),
                  reads=[e32[s], self.ones_f], writes=[spb[s]])
            kb.op("pe", lambda e: e.matmul(A.t[:, 0:n], lhsT=self.negtri.t[:], rhs=spb[s].t[:, 0:n], start=False, stop=True),
                  reads=[self.negtri, spb[s]], pwrites=[A])
            if u["newhead"]:
                kb.op("pe", lambda e: e.matmul(Crun.t[:, 0:8], lhsT=zeros_bf.t[:], rhs=self.ones_bf.t[:, 0:8], start=True, stop=True),
                      reads=[zeros_bf, self.ones_bf], writes=[Crun])
            if u["first"]:
                Ecur = Eb4[u["ek"] % 4]
                kb.op("act", lambda e: e.activation(out=Ecur.t[:], in_=Crun.t[:, 0:8], func=AF.Exp, scale=-1.0), reads=[Crun], writes=[Ecur])
            for t in range(n // 128):
                i = u["c0"] // 128 + t
                kb.op("pe", lambda e, t=t, i=i: e.matmul(Crun.t[:, i:i + 1], lhsT=spb[s].t[:, t * 128:(t + 1) * 128], rhs=self.ones_bf.t[:, 0:1],
                                                         start=False, stop=True),
                      reads=[spb[s], self.ones_bf], pwrites=[Crun])

        def st3(u, ui):
            hp, hh = u["hp"], u["hh"]
            head = hp * 2 + hh
            A = Abanks[ui % 5]
            s = ui % NU
            n = u["c1"] - u["c0"]
            O = Obanks[ui % 2]
            Ecur = Eb4[u["ek"] % 4]
            kb.op("act", lambda e: e.activation(out=wb[s].t[:, 0:n], in_=A.t[:, 0:n], func=AF.Exp), reads=[A], writes=[wb[s]])
            vv = vP[hp % 2]
            for t in range(n // 128):
                kb.op("pe", lambda e, t=t: e.matmul(O.t[:, t * 64:(t + 1) * 64], lhsT=wb[s].t[:, t * 128:(t + 1) * 128],
                                                    rhs=vv.t[:, u["kb"], hh * 64:(hh + 1) * 64], start=True, stop=True),
                      reads=[wb[s], vv], writes=[O] if t == 0 else (), pwrites=() if t == 0 else [O])
            for t in range(n // 128):
                i = u["c0"] // 128 + t
                kb.op("dve", lambda e, t=t, i=i: e.scalar_tensor_tensor(out=accs[i].t[:, head * 64:(head + 1) * 64], in0=O.t[:, t * 64:(t + 1) * 64],
                                                                        scalar=Ecur.t[:, i:i + 1], in1=accs[i].t[:, head * 64:(head + 1) * 64],
                                                                        op0=ALU.mult, op1=ALU.add),
                      reads=[O, Ecur, accs[i]], writes=[accs[i]])

        qBT_ = P["qBT"]
        NUU = len(units)
        for it_ in range(NUU + 3):
            if it_ < NUU:
                st1(units[it_], it_)
            if 1 <= it_ <= NUU:
                st2(units[it_ - 1], it_ - 1)
            if it_ >= 3:
                st3(units[it_ - 3], it_ - 3)
        sbT = kb.sb("sbT", [128, 8, 1024], BF16, grp=self.gT)
        P["sbT"] = sbT
        cvt = [kb.sb("sbcvt%d" % i, [128, 1024], BF16) for i in range(2)]
        for i in range(8):
            cb = cvt[i % 2]
            kb.op("act", lambda e, i=i, cb=cb: e.activation(out=cb.t[:], in_=accs[i].t[:], func=AF.Copy), reads=[accs[i]], writes=[cb])
            self.to_fm(cb, sbT, i * 128, nchunk=8, evac_alt=i)
        if cfg.get("dbg") == "sb":
            for i in range(8):
                kb.dma("sp", self.outs["dbg"].ap[i * 128:(i + 1) * 128, 0:1024], accs[i].t[:], accs[i], self.outs["dbg"], partial=True)
        kb.phase_end()
        kb.freegrp(self.gQB)

        kb.phase_begin()
        gM = kb.newgrp()
        mixT = kb.sb("mixT", [128, 16, 1024], BF16, grp=gM)
        NW = 2
        wa = [kb.sb("wa%d" % i, [128, 8, 256], BF16) for i in range(NW)]
        wbb = [kb.sb("wbb%d" % i, [128, 8, 256], BF16) for i in range(NW)]
        wga = [kb.sb("wga%d" % i, [128, 16, 256], BF16) for i in range(NW)]
        wgb = [kb.sb("wgb%d" % i, [128, 16, 256], BF16) for i in range(NW)]
        sg = [kb.sb("sg%d" % i, [128, 512], F32) for i in range(4)]
        tm = [kb.sb("tm%d" % i, [128, 512], F32) for i in range(4)]
        w_a, w_b = self.ins["w_a_out"], self.ins["w_b_out"]
        swaT, sbT = P["swaT"], P["sbT"]
        hT_own = P["hT_own"]
        it = 0
        for grp in range(8):
            s = grp % NW
            c0 = grp * 256
            self.load_w(wa[s], w_a.ap[:, c0:c0 + 256], w_a)
            self.load_w(wbb[s], w_b.ap[:, c0:c0 + 256], w_b)
            self.load_w(wga[s], w_in.ap[:, GA0 + c0:GA0 + c0 + 256], w_in)
            self.load_w(wgb[s], w_in.ap[:, GB0 + c0:GB0 + c0 + 256], w_in)
            for f in range(2):
                fc = grp * 2 + f
                for th in range(2):
                    u = it % 2
                    it += 1
                    bya, byb, bga, bgb = kb.bank(), kb.bank(), kb.bank(), kb.bank()
                    self.mm_fm(bga, wga[s], f * 128, hT_own, th * 512, 512, 16)
                    self.mm_fm(bgb, wgb[s], f * 128, hT_own, th * 512, 512, 16)
                    self.mm_fm(bya, wa[s], f * 128, swaT, th * 512, 512, 8)
                    self.mm_fm(byb, wbb[s], f * 128, sbT, th * 512, 512, 8)
                    sga, sgb, t1, t2 = sg[2 * u], sg[2 * u + 1], tm[2 * u], tm[2 * u + 1]
                    kb.op("act", lambda e, fc=fc, sga=sga, bga=bga: e.activation(out=sga.t[:], in_=bga.t[:], func=AF.Sigmoid, bias=bcol.t[:, 34 + fc:35 + fc], scale=1.0),
                          reads=[bga, bcol], writes=[sga])
                    kb.op("act", lambda e, fc=fc, sgb=sgb, bgb=bgb: e.activation(out=sgb.t[:], in_=bgb.t[:], func=AF.Sigmoid, bias=bcol.t[:, 50 + fc:51 + fc], scale=1.0),
                          reads=[bgb, bcol], writes=[sgb])
                    kb.op("dve", lambda e, t1=t1, sga=sga, bya=bya: e.tensor_tensor(out=t1.t[:], in0=sga.t[:], in1=bya.t[:], op=ALU.mult), reads=[sga, bya], writes=[t1])
                    kb.op("dve", lambda e, t2=t2, sgb=sgb, byb=byb: e.tensor_tensor(out=t2.t[:], in0=sgb.t[:], in1=byb.t[:], op=ALU.mult), reads=[sgb, byb], writes=[t2])
                    kb.op("pool", lambda e, fc=fc, th=th, t1=t1, t2=t2: e.tensor_tensor(out=mixT.t[:, fc, th * 512:(th + 1) * 512], in0=t1.t[:], in1=t2.t[:], op=ALU.add),
                          reads=[t1, t2], pwrites=[mixT])
        if cfg.get("dbg") == "mix":
            dbgt = [kb.sb('dbgm%d' % i, [128, 1024], F32) for i in range(2)]
            for fc in range(16):
                dt_ = dbgt[fc % 2]
                kb.op("dve", lambda e, dt_=dt_, fc=fc: e.tensor_copy(out=dt_.t[:], in_=mixT.t[:, fc, :]), reads=[mixT], writes=[dt_])
                kb.dma("sp", self.outs["dbg"].ap[fc * 128:(fc + 1) * 128 if fc < 8 else (fc - 8) * 128 + 128, 0:1024] if fc < 8 else
                       self.outs["dbg"].ap[(fc - 8) * 128:(fc - 7) * 128, 1024:2048], dt_.t[:], dt_, self.outs["dbg"], partial=True)
        kb.phase_end()
        kb.freegrp(self.gA)
        kb.freegrp(self.gT)

        kb.phase_begin()
        gr1, br1 = self.load_rows("ln1_g", "ln1_b")
        self.make_ln_bufs(2)
        wmix = kb.sb("wmix", [128, 16, D], BF16)
        h0t = [kb.sb("h0t%d" % i, [128, D], F32) for i in range(2)]
        w_m = self.ins["w_mix_out"]
        self.load_w(wmix, w_m.ap, w_m, split=4)
        for i in range(8):
            S = self.ln_sets[i % 2]
            hh0 = h0t[i % 2]
            kb.dma("sp", hh0.t[:], self.h0_scr.ap[i * 128:(i + 1) * 128, :], self.h0_scr, hh0)
            for nch in range(4):
                bk = kb.bank()
                self.mm_tm(bk, mixT, i * 128, wmix, nch * 512, 512, 16)
                kb.op("dve", lambda e, bk=bk, S=S, hh0=hh0, nch=nch: e.scalar_tensor_tensor(out=S["x"].t[:, nch * 512:(nch + 1) * 512], in0=hh0.t[:, nch * 512:(nch + 1) * 512],
                                                                                           scalar=ALPHA, in1=bk.t[:, :], op0=ALU.mult, op1=ALU.add),
                      reads=[hh0, bk], writes=[S["x"]] if nch == 0 else (), pwrites=() if nch == 0 else [S["x"]])
            self.ln_core(S, gr1, br1, want_h32=True, want_hb=False)
            kb.dma("sp", self.h1_scr.ap[i * 128:(i + 1) * 128, :], S["x"].t[:], S["x"], self.h1_scr, partial=True)
        kb.phase_end()
        kb.freegrp(gM)

    def cast_load_fm(self, src_ap, src_buf, dst_fm, off, tiles):
        kb = self.kb
        t = tiles[self.cl_i % len(tiles)]
        self.cl_i += 1
        kb.dma("pool", t.t[:], src_ap, src_buf, t)
        self.to_fm(t, dst_fm, off, evac_alt=self.cl_i)

    def stage2(self):
        kb = self.kb
        cfg = self.cfg
        mem = self.ins["mem"]
        w_xq, w_xkv, w_xo = self.ins["w_xq"], self.ins["w_xkv"], self.ins["w_xo"]
        h1 = self.h1_scr
        self.cl_i = 0
        g2 = kb.newgrp()
        kxT = kb.sb("kxT", [128, 16, 256], BF16, grp=g2)
        vx = kb.sb("vx", [128, 2, D], BF16, grp=g2)
        qxT = kb.sb("qxT", [128, 16, 1024], BF16, grp=g2)
        kb.phase_begin()
        memT = kb.sb("memT", [128, 16, 256], BF16)
        h1T = kb.sb("h1T", [128, 16, 1024], BF16)
        ct = [kb.sb("ct%d" % i, [128, D], BF16) for i in range(2)]
        for i in range(2):
            self.cast_load_fm(mem.ap[i * 128:(i + 1) * 128, :], mem, memT, i * 128, ct)
        for i in range(8):
            self.cast_load_fm(h1.ap[i * 128:(i + 1) * 128, :], h1, h1T, i * 128, ct)
        wp = [kb.sb("wp%d" % i, [128, 16, 512], BF16) for i in range(2)]
        pi = 0
        for pc in range(4):
            w = wp[pi % 2]; pi += 1
            self.load_w(w, w_xkv.ap[:, pc * 512:(pc + 1) * 512], w_xkv)
            for f in range(4):
                bk = kb.bank()
                self.mm_fm(bk, w, f * 128, memT, 0, 256, 16)
                kb.op("act", lambda e, bk=bk, fc=pc * 4 + f: e.activation(out=kxT.t[:, fc, :], in_=bk.t[:, 0:256], func=AF.Copy),
                      reads=[bk], pwrites=[kxT])
        for pc in range(4):
            w = wp[pi % 2]; pi += 1
            self.load_w(w, w_xkv.ap[:, D + pc * 512:D + (pc + 1) * 512], w_xkv)
            for mt in range(2):
                bk = kb.bank()
                self.mm_tm(bk, memT, mt * 128, w, 0, 512, 16)
                kb.op("dve", lambda e, bk=bk, mt=mt, pc=pc: e.tensor_copy(out=vx.t[:, mt, pc * 512:(pc + 1) * 512], in_=bk.t[:, :]),
                      reads=[bk], pwrites=[vx])
        for pc in range(4):
            w = wp[pi % 2]; pi += 1
            self.load_w(w, w_xq.ap[:, pc * 512:(pc + 1) * 512], w_xq)
            for f in range(4):
                for th in range(2):
                    bk = kb.bank()
                    self.mm_fm(bk, w, f * 128, h1T, th * 512, 512, 16)
                    kb.op("act", lambda e, bk=bk, fc=pc * 4 + f, th=th: e.activation(out=qxT.t[:, fc, th * 512:(th + 1) * 512], in_=bk.t[:, :],
                                                                                    func=AF.Copy, scale=float(512 ** -0.5)),
                          reads=[bk], pwrites=[qxT])
        kb.phase_end()
        if cfg.get("s2") == "a":
            return
        kb.phase_begin()
        g2o = kb.newgrp()
        oT = kb.sb("oT", [128, 16, 1024], BF16, grp=g2o)
        pT_all = kb.sb("pT_all", [128, 4, 2, 1024], BF16)
        NS = 3
        p32 = [kb.sb("xp32_%d" % i, [128, 256], F32) for i in range(NS)]
        pn = [kb.sb("xpn_%d" % i, [128, 256], BF16) for i in range(NS)]
        sm = [kb.sb("xsm_%d" % i, [128, 8], F32) for i in range(NS)]
        it = 0
        for tt in range(8):
            for h in range(4):
                s = it % NS
                it += 1
                Z = kb.bank()
                for c in range(4):
                    kb.op("pe", lambda e, c=c, Z=Z: e.matmul(Z.t[:, 0:256], lhsT=qxT.t[:, 4 * h + c, tt * 128:(tt + 1) * 128], rhs=kxT.t[:, 4 * h + c, :],
                                                            start=(c == 0), stop=(c == 3)),
                          reads=[qxT, kxT], writes=[Z] if c == 0 else (), pwrites=() if c == 0 else [Z])
                kb.op("dve", lambda e, Z=Z: e.reduce_max(out=sm[s].t[:, 0:1], in_=Z.t[:, 0:256], axis=AX.X), reads=[Z], writes=[sm[s]])
                kb.op("dve", lambda e: e.tensor_scalar(out=sm[s].t[:, 1:2], in0=sm[s].t[:, 0:1], scalar1=-1.0, scalar2=None, op0=ALU.mult),
                      reads=[sm[s]], pwrites=[sm[s]])
                kb.op("act", lambda e, Z=Z: e.activation(out=p32[s].t[:], in_=Z.t[:, 0:256], func=AF.Exp, bias=sm[s].t[:, 1:2], scale=1.0,
                                                         accum_out=sm[s].t[:, 2:3]),
                      reads=[Z, sm[s]], writes=[p32[s]], pwrites=[sm[s]])
                kb.op("dve", lambda e: e.reciprocal(out=sm[s].t[:, 3:4], in_=sm[s].t[:, 2:3]), reads=[sm[s]], pwrites=[sm[s]])
                kb.op("dve", lambda e: e.tensor_scalar(out=pn[s].t[:], in0=p32[s].t[:], scalar1=sm[s].t[:, 3:4], scalar2=None, op0=ALU.mult),
                      reads=[p32[s], sm[s]], writes=[pn[s]])
                PT = kb.bank()
                ptv = PT.t[:].bitcast(BF16)
                for c in range(2):
                    kb.op("pe", lambda e, c=c: e.transpose(out=ptv[:, c * 128:(c + 1) * 128], in_=pn[s].t[:, c * 128:(c + 1) * 128],
                                                           identity=self.ident_bf.t[:]),
                          reads=[pn[s], self.ident_bf], writes=[PT] if c == 0 else (), pwrites=() if c == 0 else [PT])
                kb.op("act", lambda e, ptv=ptv, PT=PT: e.activation(out=pT_all.t[:, h, :, tt * 128:(tt + 1) * 128],
                                                                    in_=ptv[:, 0:256].rearrange("p (a b) -> p a b", a=2), func=AF.Copy),
                      reads=[PT], pwrites=[pT_all])
        for h in range(4):
            for dc in range(4):
                fc = 4 * h + dc
                for th in range(2):
                    bk = kb.bank()
                    for mc in range(2):
                        kb.op("pe", lambda e, mc=mc, bk=bk: e.matmul(bk.t[:, :], lhsT=vx.t[:, mc, fc * 128:(fc + 1) * 128],
                                                                    rhs=pT_all.t[:, h, mc, th * 512:(th + 1) * 512], start=(mc == 0), stop=(mc == 1)),
                              reads=[vx, pT_all], writes=[bk] if mc == 0 else (), pwrites=() if mc == 0 else [bk])
                    if (fc + th) % 2:
                        kb.op("act", lambda e, bk=bk: e.activation(out=oT.t[:, fc, th * 512:(th + 1) * 512], in_=bk.t[:, :], func=AF.Copy),
                              reads=[bk], pwrites=[oT])
                    else:
                        kb.op("dve", lambda e, bk=bk: e.tensor_copy(out=oT.t[:, fc, th * 512:(th + 1) * 512], in_=bk.t[:, :]),
                              reads=[bk], pwrites=[oT])
        kb.phase_end()
        kb.freegrp(g2)
        if cfg.get("s2") == "b":
            return
        kb.phase_begin()
        gr2, br2 = self.load_rows("ln2_g", "ln2_b")
        self.make_ln_bufs(2)
        wxo = kb.sb("wxo", [128, 16, D], BF16)
        h1t = [kb.sb("h1t%d" % i, [128, D], F32) for i in range(2)]
        self.load_w(wxo, w_xo.ap, w_xo, split=4)
        for i in range(8):
            S = self.ln_sets[i % 2]
            hh = h1t[i % 2]
            kb.dma("sp", hh.t[:], h1.ap[i * 128:(i + 1) * 128, :], h1, hh)
            for nch in range(4):
                bk = kb.bank()
                self.mm_tm(bk, oT, i * 128, wxo, nch * 512, 512, 16)
                kb.op("dve", lambda e, bk=bk, S=S, hh=hh, nch=nch: e.scalar_tensor_tensor(out=S["x"].t[:, nch * 512:(nch + 1) * 512], in0=hh.t[:, nch * 512:(nch + 1) * 512],
                                                                                         scalar=ALPHA, in1=bk.t[:, :], op0=ALU.mult, op1=ALU.add),
                      reads=[hh, bk], writes=[S["x"]] if nch == 0 else (), pwrites=() if nch == 0 else [S["x"]])
            self.ln_core(S, gr2, br2, want_h32=True, want_hb=False)
            kb.dma("sp", self.h2_scr.ap[i * 128:(i + 1) * 128, :], S["x"].t[:], S["x"], self.h2_scr, partial=True)
        kb.phase_end()
        kb.freegrp(g2o)

    def stage3(self):
        kb = self.kb
        cfg = self.cfg
        n_exp = cfg.get("n_exp", NE)
        h2 = self.h2_scr
        w_up, w_down = self.ins["w_up"], self.ins["w_down"]
        g3 = kb.newgrp()
        yacc = [kb.sb("yacc%d" % i, [128, D], F32, grp=g3) for i in range(8)]
        kb.phase_begin()
        g3b = kb.newgrp()
        X_bf = kb.sb("X_bf", [128, 8, D], BF16, grp=g3b)
        mask_all = kb.sb("mask_all", [128, 8, NE], F32, grp=g3b)
        gate_all = kb.sb("gate_all", [128, 8, NE], F32, grp=g3b)
        pos_all = kb.sb("pos_all", [128, 8, NE], F32, grp=g3b)
        mask_bf = kb.sb("mask_bf", [128, 8, NE], BF16)
        wr = kb.sb("wr", [128, 16, NE], F32)
        kb.dma("sp", wr.t[:], self.ins["w_router"].ap.rearrange("(c p) n -> p c n", p=128), self.ins["w_router"], wr)
        brt = kb.sb("brt", [128, NE], F32)
        kb.dma("sp", brt.t[:], self.ins["b_router"].ap.partition_broadcast(128), self.ins["b_router"], brt)
        h2t = [kb.sb("h2t%d" % i, [128, D], F32) for i in range(2)]
        hT32 = [kb.sb("hT32_%d" % i, [128, 16, 128], F32) for i in range(2)]
        lg = [kb.sb("lg%d" % i, [128, NE], F32) for i in range(2)]
        t8 = [kb.sb("t8_%d" % i, [128, 16], F32) for i in range(2)]
        eg = [kb.sb("eg%d" % i, [128, NE], F32) for i in range(2)]
        for tt in range(8):
            u = tt % 2
            hh = h2t[u]
            kb.dma("sp", hh.t[:], h2.ap[tt * 128:(tt + 1) * 128, :], h2, hh)
            kb.op("act", lambda e, hh=hh, tt=tt: e.activation(out=X_bf.t[:, tt, :], in_=hh.t[:], func=AF.Copy), reads=[hh], pwrites=[X_bf])
            kb.op("pool", lambda e, hh=hh, tt=tt: e.tensor_scalar(out=yacc[tt].t[:], in0=hh.t[:], scalar1=ALPHA, scalar2=None, op0=ALU.mult),
                  reads=[hh], writes=[yacc[tt]])
            for q4 in range(4):
                bk = kb.bank()
                for k in range(4):
                    kk = q4 * 4 + k
                    kb.op("pe", lambda e, k=k, kk=kk, bk=bk, hh=hh: e.transpose(out=bk.t[:, k * 128:(k + 1) * 128], in_=hh.t[:, kk * 128:(kk + 1) * 128],
                                                                               identity=self.ident_f.t[:]),
                          reads=[hh, self.ident_f], writes=[bk] if k == 0 else (), pwrites=() if k == 0 else [bk])
                kb.op("dve", lambda e, bk=bk, q4=q4, u=u: e.tensor_copy(out=hT32[u].t[:, q4 * 4:(q4 + 1) * 4, :], in_=bk.t[:, :].rearrange("p (a b) -> p a b", a=4)),
                      reads=[bk], pwrites=[hT32[u]] if q4 else (), writes=() if q4 else [hT32[u]])
            L = kb.bank()
            for k in range(16):
                kb.op("pe", lambda e, k=k, L=L, u=u: e.matmul(L.t[:, 0:NE], lhsT=hT32[u].t[:, k, :], rhs=wr.t[:, k, :], start=(k == 0), stop=(k == 15)),
                      reads=[hT32[u], wr], writes=[L] if k == 0 else (), pwrites=() if k == 0 else [L])
            kb.op("dve", lambda e, L=L, u=u: e.tensor_tensor(out=lg[u].t[:], in0=L.t[:, 0:NE], in1=brt.t[:], op=ALU.add), reads=[L, brt], writes=[lg[u]])
            kb.op("dve", lambda e, u=u: e.max(out=t8[u].t[:, 0:8], in_=lg[u].t[:]), reads=[lg[u]], writes=[t8[u]])
            kb.op("dve", lambda e, u=u, tt=tt: e.tensor_scalar(out=mask_all.t[:, tt, :], in0=lg[u].t[:], scalar1=t8[u].t[:, 3:4], scalar2=None, op0=ALU.is_ge),
                  reads=[lg[u], t8[u]], pwrites=[mask_all])
            kb.op("dve", lambda e, u=u: e.tensor_scalar(out=t8[u].t[:, 8:9], in0=t8[u].t[:, 0:1], scalar1=-1.0, scalar2=None, op0=ALU.mult),
                  reads=[t8[u]], pwrites=[t8[u]])
            kb.op("act", lambda e, u=u: e.activation(out=eg[u].t[:], in_=lg[u].t[:], func=AF.Exp, bias=t8[u].t[:, 8:9], scale=1.0),
                  reads=[lg[u], t8[u]], writes=[eg[u]])
            kb.op("dve", lambda e, u=u, tt=tt: e.tensor_tensor(out=eg[u].t[:], in0=eg[u].t[:], in1=mask_all.t[:, tt, :], op=ALU.mult),
                  reads=[eg[u], mask_all], writes=[eg[u]])
            kb.op("dve", lambda e, u=u: e.reduce_sum(out=t8[u].t[:, 9:10], in_=eg[u].t[:], axis=AX.X), reads=[eg[u]], pwrites=[t8[u]])
            kb.op("dve", lambda e, u=u: e.reciprocal(out=t8[u].t[:, 10:11], in_=t8[u].t[:, 9:10]), reads=[t8[u]], pwrites=[t8[u]])
            kb.op("dve", lambda e, u=u, tt=tt: e.tensor_scalar(out=gate_all.t[:, tt, :], in0=eg[u].t[:], scalar1=t8[u].t[:, 10:11], scalar2=None, op0=ALU.mult),
                  reads=[eg[u], t8[u]], pwrites=[gate_all])
            kb.op("dve", lambda e, tt=tt: e.tensor_copy(out=mask_bf.t[:, tt, :], in_=mask_all.t[:, tt, :]), reads=[mask_all], pwrites=[mask_bf])
            Pp = kb.bank()
            for t2 in range(tt + 1):
                lhs = self.trix if t2 == tt else self.ones_bf
                kb.op("pe", lambda e, t2=t2, lhs=lhs, Pp=Pp: e.matmul(Pp.t[:, 0:NE], lhsT=lhs.t[:], rhs=mask_bf.t[:, t2, :], start=(t2 == 0), stop=(t2 == tt)),
                      reads=[lhs, mask_bf], writes=[Pp] if t2 == 0 else (), pwrites=() if t2 == 0 else [Pp])
            kb.op("dve", lambda e, Pp=Pp, tt=tt: e.tensor_copy(out=pos_all.t[:, tt, :], in_=Pp.t[:, 0:NE]), reads=[Pp], pwrites=[pos_all])
        if cfg.get("dbg") == "route":
            for tt in range(8):
                kb.dma("sp", self.outs["dbg"].ap[tt * 128:(tt + 1) * 128, 0:NE], mask_all.t[:, tt, :], mask_all, self.outs["dbg"], partial=True)
                kb.dma("sp", self.outs["dbg"].ap[tt * 128:(tt + 1) * 128, NE:2 * NE], gate_all.t[:, tt, :], gate_all, self.outs["dbg"], partial=True)
                kb.dma("sp", self.outs["dbg"].ap[tt * 128:(tt + 1) * 128, 2 * NE:3 * NE], pos_all.t[:, tt, :], pos_all, self.outs["dbg"], partial=True)
        kb.phase_end()
        kb.phase_begin()
        C = CAP
        cchunks = [(0, 128), (128, C)] if C > 128 else [(0, C)]
        iota_r = kb.sb("iota_r", [128, C], F32)
        kb.dma("sp", iota_r.t[:], self.ins["c_iota"].ap[:, 0:C], self.ins["c_iota"], iota_r)
        bupc = kb.sb("bupc", [128, NE, 32], F32)
        kb.dma("sp", bupc.t[:], self.ins["bup_col"].ap, self.ins["bup_col"], bupc)
        Sel = [kb.sb("Sel%d" % i, [128, 8, C], BF16) for i in range(1)] * 2
        SelG = [kb.sb("SelG%d" % i, [128, 8, C], BF16) for i in range(1)] * 2
        SelGT = [kb.sb("SelGT%d" % i, [128, 2, 1024], BF16) for i in range(2)]
        xeT = [kb.sb("xeT%d" % i, [128, 16, C], BF16) for i in range(1)] * 2
        actT = [kb.sb("actT%d" % i, [128, 16, C], BF16) for i in range(1)] * 2
        Ye = [kb.sb("Ye%d" % i, [128, 2, D], BF16) for i in range(1)] * 2
        NR = 6
        ring = [kb.sb("wring%d" % i, [128, 16, 256], BF16) for i in range(NR)]
        bdr = [kb.sb("bdr%d" % i, [1, D], BF16) for i in range(2)]
        pieces = []
        for e2 in range(n_exp):
            for pj in range(8):
                pieces.append((e2, "g", pj))
                pieces.append((e2, "l", pj))
            for pd in range(8):
                pieces.append((e2, "d", pd))

        def issue(p):
            if p >= len(pieces):
                return
            e2, kind, j = pieces[p]
            if kind == "g":
                self.load_w(ring[p % NR], w_up.ap[e2, :, j * 256:(j + 1) * 256], w_up)
            elif kind == "l":
                self.load_w(ring[p % NR], w_up.ap[e2, :, D + j * 256:D + (j + 1) * 256], w_up)
            else:
                self.load_w(ring[p % NR], w_down.ap[e2, :, j * 256:(j + 1) * 256], w_down)
        for p in range(NR):
            issue(p)
        pidx = 0
        kb.dma("pool", bdr[0].t[:], self.ins["b_down"].ap[0:1, :], self.ins["b_down"], bdr[0])
        NT = 3
        g32 = [kb.sb("g32_%d" % i, [128, C], F32) for i in range(NT)]
        sgm = [kb.sb("sgm_%d" % i, [128, C], F32) for i in range(NT)]
        l32 = [kb.sb("l32m_%d" % i, [128, C], F32) for i in range(NT)]
        wpi = 0
        wdi = 0
        ti = 0
        for e_ in range(n_exp):
            u = e_ % 2
            for tt in range(8):
                kb.op("dve", lambda e, tt=tt: e.tensor_scalar(out=Sel[u].t[:, tt, :], in0=iota_r.t[:], scalar1=pos_all.t[:, tt, e_:e_ + 1],
                                                              scalar2=mask_all.t[:, tt, e_:e_ + 1], op0=ALU.is_equal, op1=ALU.mult),
                      reads=[iota_r, pos_all, mask_all], pwrites=[Sel[u]] if tt else (), writes=() if tt else [Sel[u]])
                kb.op("dve", lambda e, tt=tt: e.tensor_scalar(out=SelG[u].t[:, tt, :], in0=iota_r.t[:], scalar1=pos_all.t[:, tt, e_:e_ + 1],
                                                               scalar2=gate_all.t[:, tt, e_:e_ + 1], op0=ALU.is_equal, op1=ALU.mult),
                      reads=[iota_r, pos_all, gate_all], pwrites=[SelG[u]] if tt else (), writes=() if tt else [SelG[u]])
            for fc in range(16):
                bk = kb.bank()
                for tt in range(8):
                    kb.op("pe", lambda e, tt=tt, bk=bk, fc=fc: e.matmul(bk.t[:, 0:C], lhsT=X_bf.t[:, tt, fc * 128:(fc + 1) * 128], rhs=Sel[u].t[:, tt, :],
                                                                       start=(tt == 0), stop=(tt == 7)),
                          reads=[X_bf, Sel[u]], writes=[bk] if tt == 0 else (), pwrites=() if tt == 0 else [bk])
                if fc % 2:
                    kb.op("act", lambda e, bk=bk, fc=fc: e.activation(out=xeT[u].t[:, fc, :], in_=bk.t[:, 0:C], func=AF.Copy), reads=[bk],
                          pwrites=[xeT[u]] if fc else (), writes=() if fc else [xeT[u]])
                else:
                    kb.op("dve", lambda e, bk=bk, fc=fc: e.tensor_copy(out=xeT[u].t[:, fc, :], in_=bk.t[:, 0:C]), reads=[bk],
                          pwrites=[xeT[u]] if fc else (), writes=() if fc else [xeT[u]])
            for ci, (c0, c1) in enumerate(cchunks):
                m = c1 - c0
                bk = kb.bank()
                bv = bk.t[:].bitcast(BF16)
                for tt in range(8):
                    kb.op("pe", lambda e, tt=tt, bv=bv: e.transpose(out=bv[0:m, tt * 128:(tt + 1) * 128], in_=SelG[u].t[:, tt, c0:c1], identity=self.ident_bf.t[:]),
                          reads=[SelG[u], self.ident_bf], writes=[bk] if tt == 0 else (), pwrites=() if tt == 0 else [bk])
                kb.op("act", lambda e, bv=bv, ci=ci, m=m: e.activation(out=SelGT[u].t[0:m, ci, :], in_=bv[0:m, 0:1024], func=AF.Copy), reads=[bk],
                      pwrites=[SelGT[u]] if ci else (), writes=() if ci else [SelGT[u]])
            if e_ + 1 < n_exp:
                kb.dma("pool", bdr[(e_ + 1) % 2].t[:], self.ins["b_down"].ap[e_ + 1:e_ + 2, :], self.ins["b_down"], bdr[(e_ + 1) % 2])
            for pj in range(8):
                wgt, wlt = ring[pidx % NR], ring[(pidx + 1) % NR]
                for f in range(2):
                    fc = pj * 2 + f
                    s3 = ti % NT
                    ti += 1
                    bg, bl = kb.bank(), kb.bank()
                    self.mm_fm(bg, wgt, f * 128, xeT[u], 0, C, 16)
                    self.mm_fm(bl, wlt, f * 128, xeT[u], 0, C, 16)
                    kb.op("dve", lambda e, bg=bg, fc=fc, s3=s3: e.tensor_scalar(out=g32[s3].t[:], in0=bg.t[:, 0:C], scalar1=bupc.t[:, e_, fc:fc + 1], scalar2=7.0,
                                                                               op0=ALU.add, op1=ALU.min),
                          reads=[bg, bupc], writes=[g32[s3]])
                    kb.op("act", lambda e, s3=s3: e.activation(out=sgm[s3].t[:], in_=g32[s3].t[:], func=AF.Sigmoid, scale=1.702), reads=[g32[s3]], writes=[sgm[s3]])
                    kb.op("dve", lambda e, bl=bl, fc=fc, s3=s3: e.tensor_scalar(out=l32[s3].t[:], in0=bl.t[:, 0:C], scalar1=bupc.t[:, e_, 16 + fc:17 + fc], scalar2=7.0,
                                                                               op0=ALU.add, op1=ALU.min),
                          reads=[bl, bupc], writes=[l32[s3]])
                    kb.op("dve", lambda e, s3=s3: e.tensor_scalar(out=l32[s3].t[:], in0=l32[s3].t[:], scalar1=-7.0, scalar2=1.0, op0=ALU.max, op1=ALU.add),
                          reads=[l32[s3]], writes=[l32[s3]])
                    kb.op("dve", lambda e, s3=s3: e.tensor_tensor(out=g32[s3].t[:], in0=g32[s3].t[:], in1=sgm[s3].t[:], op=ALU.mult),
                          reads=[g32[s3], sgm[s3]], writes=[g32[s3]])
                    kb.op("dve", lambda e, s3=s3, fc=fc: e.tensor_tensor(out=actT[u].t[:, fc, :], in0=g32[s3].t[:], in1=l32[s3].t[:], op=ALU.mult),
                          reads=[g32[s3], l32[s3]], pwrites=[actT[u]] if fc else (), writes=() if fc else [actT[u]])
                issue(pidx + NR)
                issue(pidx + 1 + NR)
                pidx += 2
            bd = bdr[u]
            for pd in range(8):
                wdt = ring[pidx % NR]
                for ci, (c0, c1) in enumerate(cchunks):
                    m = c1 - c0
                    bk = kb.bank()
                    kb.op("pe", lambda e, bk=bk, m=m, pd=pd: e.matmul(bk.t[0:m, 0:256], lhsT=self.ones_bf.t[0:1, 0:m], rhs=bd.t[0:1, pd * 256:(pd + 1) * 256],
                                                                     start=True, stop=False),
                          reads=[self.ones_bf, bd], writes=[bk])
                    for k in range(16):
                        kb.op("pe", lambda e, bk=bk, m=m, k=k, c0=c0, c1=c1, wdt=wdt: e.matmul(bk.t[0:m, 0:256], lhsT=actT[u].t[:, k, c0:c1], rhs=wdt.t[:, k, :],
                                                                                              start=False, stop=(k == 15)),
                              reads=[actT[u], wdt], pwrites=[bk])
                    first = (pd == 0 and ci == 0)
                    if ci == 0:
                        kb.op("act", lambda e, bk=bk, m=m, ci=ci, pd=pd: e.activation(out=Ye[u].t[0:m, ci, pd * 256:(pd + 1) * 256], in_=bk.t[0:m, 0:256], func=AF.Copy),
                              reads=[bk], pwrites=() if first else [Ye[u]], writes=[Ye[u]] if first else ())
                    else:
                        kb.op("dve", lambda e, bk=bk, m=m, ci=ci, pd=pd: e.tensor_copy(out=Ye[u].t[0:m, ci, pd * 256:(pd + 1) * 256], in_=bk.t[0:m, 0:256]),
                              reads=[bk], pwrites=[Ye[u]])
                issue(pidx + NR)
                pidx += 1
            for tt in range(8):
                for nch in range(4):
                    bk = kb.bank()
                    for ci, (c0, c1) in enumerate(cchunks):
                        m = c1 - c0
                        kb.op("pe", lambda e, bk=bk, m=m, ci=ci, tt=tt, nch=nch: e.matmul(bk.t[:, :], lhsT=SelGT[u].t[0:m, ci, tt * 128:(tt + 1) * 128],
                                                                                         rhs=Ye[u].t[0:m, ci, nch * 512:(nch + 1) * 512],
                                                                                         start=(ci == 0), stop=(ci == len(cchunks) - 1)),
                              reads=[SelGT[u], Ye[u]], writes=[bk] if ci == 0 else (), pwrites=() if ci == 0 else [bk])
                    kb.op("dve", lambda e, bk=bk, tt=tt, nch=nch: e.tensor_tensor(out=yacc[tt].t[:, nch * 512:(nch + 1) * 512], in0=yacc[tt].t[:, nch * 512:(nch + 1) * 512],
                                                                                 in1=bk.t[:, :], op=ALU.add),
                          reads=[bk, yacc[tt]], writes=[yacc[tt]])
        kb.phase_end()
        kb.freegrp(g3b)
        kb.phase_begin()
        gr3, br3 = self.load_rows("ln3_g", "ln3_b")
        self.make_ln_bufs(2)
        for tt in range(8):
            S = dict(self.ln_sets[tt % 2])
            S["x"] = yacc[tt]
            self.ln_core(S, gr3, br3, want_h32=True, want_hb=False)
            kb.dma("sp", self.out_buf.ap[tt * 128:(tt + 1) * 128, :], yacc[tt].t[:], yacc[tt], self.out_buf, partial=True)
        kb.phase_end()
        kb.freegrp(g3)

    def build(self):
        cfg = self.cfg
        nc = self.nc
        with ExitStack() as es:
            kb = KB(nc, es)
            self.kb = kb
            if cfg.get("start", 1) <= 1:
                self.din("x_own", [1024, D]); self.din("x_prev", [1024, D]); self.din("x_ctx", [4096, D])
                self.din("w_in", [D, 8448])
                self.din("ln_in_g", [1, D]); self.din("ln_in_b", [1, D]); self.din("ln1_g", [1, D]); self.din("ln1_b", [1, D])
                self.din("sbmask", [128, 4, 128]); self.din("swab", [128, 16, 256]); self.din("pm0", [128, 256]); self.din("attn_sinks", [1, 16])
                self.din("w_a_out", [1024, D]); self.din("w_b_out", [1024, D]); self.din("w_mix_out", [D, D])
            self.din("bcol_in", [128, 66]); self.din("bcol_qa_in", [128, 8]); self.din("b_row_in", [1, 1152])
            self.kT_scr = self.dscr("kT_scr", [8, 128, 4096], BF16)
            self.V_scr = self.dscr("V_scr", [32, 128, 1024], BF16)
            self.h0_scr = self.dscr("h0_scr", [1024, D], F32)
            stop = cfg.get("stop", 3)
            start = cfg.get("start", 1)
            if start <= 2 <= stop:
                self.din("mem", [256, D]); self.din("w_xq", [D, D]); self.din("w_xkv", [D, 2 * D]); self.din("w_xo", [D, D])
                self.din("ln2_g", [1, D]); self.din("ln2_b", [1, D])
            if start <= 3 <= stop:
                ne = cfg.get("n_exp", NE)
                self.din("w_router", [D, NE]); self.din("b_router", [1, NE]); self.din("w_up", [ne, D, 2 * D]); self.din("w_down", [ne, D, D])
                self.din("bup_col", [128, NE, 32]); self.din("b_down", [NE, D]); self.din("ln3_g", [1, D]); self.din("ln3_b", [1, D])
                self.din("c_iota", [128, 256])
            mk = lambda nm, st: (self.dout("out", [1024, D]) if stop == st else self.dscr(nm, [1024, D], F32))
            self.h1_scr = self.din("h1_in", [1024, D]) if start == 2 else mk("h1_scr", 1)
            self.h2_scr = self.din("h2_in", [1024, D]) if start == 3 else mk("h2_scr", 2)
            self.out_buf = mk("out_scr", 3)
            if cfg.get("dbg"):
                self.dout("dbg", [1024, D])
            self.load_consts()
            self.bcol = kb.sb("bcol", [128, 66], F32, glob=True)
            kb.dma("sp", self.bcol.t[:], self.ins["bcol_in"].ap, self.ins["bcol_in"], self.bcol)
            self.bcol_qa = kb.sb("bcol_qa", [128, 8], F32, glob=True)
            kb.dma("sp", self.bcol_qa.t[:], self.ins["bcol_qa_in"].ap, self.ins["bcol_qa_in"], self.bcol_qa)
            self.brow = kb.sb("brow", [1, 1152], BF16, glob=True)
            kb.dma("pool", self.brow.t[:], self.ins["b_row_in"].ap, self.ins["b_row_in"], self.brow)
            kb.barrier()
            if start <= 1 <= stop:
                self.stage1()
            if start <= 2 <= stop:
                self.stage2()
            if start <= 3 <= stop:
                self.stage3()
            kb.barrier()
        return nc


def t5_bucket_np(rel):
    half, max_exact = 16, 8
    base = np.where(rel > 0, half, 0)
    n = np.abs(rel)
    nf = np.maximum(n, 1).astype(np.float32)
    large = max_exact + (np.log(nf / max_exact) / np.float32(np.log(128 / max_exact)) * (half - max_exact)).astype(np.int32)
    large = np.minimum(large, half - 1)
    return base + np.where(n < max_exact, n, large)


def host_consts():
    ident = np.eye(128, dtype=np.float32)
    sp, s = np.meshgrid(np.arange(128), np.arange(128), indexing="ij")
    negtri = -(sp >= s).astype(np.float32)
    trix = (sp < s).astype(np.float32)
    return dict(c_ident=ident, c_negtri=negtri, c_trix=trix)


def prep_core_inputs(c, inp):
    b, cp = c // 4, c % 4
    x = inp["x"][b]
    blocks = [4 * i + cp for i in range(8)]
    x_own = np.concatenate([x[q * 128:(q + 1) * 128] for q in blocks], 0)
    x_prev = np.concatenate([x[(q - 1) * 128:q * 128] if q > 0 else np.zeros((128, D), np.float32) for q in blocks], 0)
    d = dict(x_own=np.ascontiguousarray(x_own), x_prev=np.ascontiguousarray(x_prev), x_ctx=np.ascontiguousarray(x))
    if "mem" in inp:
        d["mem"] = inp["mem"][b]
    s_, q_ = np.meshgrid(np.arange(128), np.arange(128), indexing="ij")
    m = np.full((128, 4, 128), NEG, np.float32)
    for r in range(4):
        if r < cp:
            m[:, r, :] = 0.0
        elif r == cp:
            m[:, r, :] = np.where(s_ < q_, 0.0, NEG)
    d["sbmask"] = m
    qi = np.arange(128)[:, None]
    kj = np.arange(256)[None, :]
    bucket = t5_bucket_np(kj - 128 - qi)
    bias = inp["rel_bias"][bucket]
    dchunk = (kj // 64 - 2) - qi // 64
    band = (dchunk <= 0) & (dchunk >= -2)
    tab = np.where(band[:, :, None], bias, np.float32(NEG)).astype(np.float32)
    d["swab"] = np.ascontiguousarray(np.transpose(tab, (0, 2, 1)))
    pm0 = np.zeros((128, 256), np.float32)
    if cp == 0:
        pm0[:, :128] = NEG
    d["pm0"] = pm0
    return d


def common_inputs(inp):
    b_in = inp["b_in"][0]
    bcol = np.ascontiguousarray(b_in.reshape(66, 128).T)
    qa = b_in[0:1024].reshape(2, 8, 64)
    bcol_qa = np.ascontiguousarray(np.transpose(qa, (1, 0, 2)).reshape(8, 128).T)
    d = dict(w_in=inp["w_in"][0], bcol_in=bcol, bcol_qa_in=bcol_qa, b_row_in=np.concatenate([b_in[1152:1280], b_in[3328:4352]])[None, :],
             ln_in_g=inp["ln_in_g"][None, :], ln_in_b=inp["ln_in_b"][None, :], ln1_g=inp["ln1_g"], ln1_b=inp["ln1_b"],
             attn_sinks=inp["attn_sinks"], w_a_out=inp["w_a_out"][0], w_b_out=inp["w_b_out"][0], w_mix_out=inp["w_mix_out"][0])
    d.update(host_consts())
    d["c_iota"] = np.tile(np.arange(256, dtype=np.float32)[None, :], (128, 1))
    for k in ("w_xq", "w_xkv", "w_xo", "w_router", "w_up", "w_down"):
        if k in inp:
            d[k] = inp[k][0]
    for k in ("ln2_g", "ln2_b", "ln3_g", "ln3_b", "b_router"):
        if k in inp:
            d[k] = inp[k]
    if "b_up" in inp:
        d["bup_col"] = np.ascontiguousarray(np.transpose(inp["b_up"][0].reshape(NE, 32, 128), (2, 0, 1)))
        d["b_down"] = inp["b_down"][0]
    return d


def run(inp, cfg):
    prog = Prog(cfg)
    nc = prog.build()
    com = common_inputs(inp)
    in_maps = []
    for c in range(8):
        d = dict(com)
        d.update(prep_core_inputs(c, inp))
        in_maps.append({k: np.ascontiguousarray(v, dtype=np.float32) for k, v in d.items() if k in prog.ins})
    res = run_bass_kernel_spmd(nc, in_maps, core_ids=list(range(8)))
    return res.results


def assemble(results, key="out"):
    out = np.zeros((2, 4096, D), np.float32)
    for c in range(8):
        b, cp = c // 4, c % 4
        r = results[c][key]
        for i in range(8):
            q = 4 * i + cp
            out[b, q * 128:(q + 1) * 128] = r[i * 128:(i + 1) * 128]
    return out


def kernel(**inputs):
    inp = {k: np.asarray(v) for k, v in inputs.items()}
    results = run(inp, dict())
    return assemble(results)
```

```python
import numpy as np
from contextlib import ExitStack
import concourse.bass as bass
import concourse.mybir as mybir
from concourse.bass_utils import run_bass_kernel_spmd

F32 = mybir.dt.float32
BF16 = mybir.dt.bfloat16
AF = mybir.ActivationFunctionType
ALU = mybir.AluOpType
AX = mybir.AxisListType

D = 2048
NE = 32
CAP = 192
ALPHA = 2.0 ** 0.25
EPS = 1e-5
NEG = -30000.0
ARENA = 206 * 1024


class Sem:
    def __init__(s, h):
        s.h = h
        s.total = 0


class Buf:
    def __init__(s, name, t=None):
        s.name = name
        s.t = t
        s.w = {}
        s.r = {}
        s.dsem = None


class KB:
    def __init__(s, nc, es, ndsem=92):
        s.nc = nc
        s.E = dict(pe=nc.tensor, act=nc.scalar, dve=nc.vector, pool=nc.gpsimd, sp=nc.sync)
        s.sem = {k: es.enter_context(nc.semaphore("es_" + k)) for k in s.E}
        s.cnt = {k: 0 for k in s.E}
        s.known = {k: {} for k in s.E}
        s.free_dsems = [Sem(es.enter_context(nc.semaphore("ds%d" % i))) for i in range(ndsem)]
        s.used_dsems = []
        s.phase_bufs = []
        s.banks = [Buf("bank%d" % i, es.enter_context(nc.psum_tensor("bank%d" % i, [128, 512], F32))) for i in range(8)]
        s.bank_i = 0
        s.es = es
        s.pes = None
        s.arena_bytes = ARENA
        s.arena = es.enter_context(nc.sbuf_tensor("arena", [128, ARENA // 4], F32))
        s.alloc = []

    def bank(s):
        b = s.banks[s.bank_i % 8]
        s.bank_i += 1
        return b

    def sb(s, name, shape, dt, glob=False, grp=None):
        nb = 2 if dt == BF16 else 4
        free = 1
        for d_ in shape[1:]:
            free *= d_
        size = (free * nb + 63) // 64 * 64
        off = None
        cur = 0
        for (o, sz) in sorted(s.alloc):
            if o - cur >= size:
                off = cur
                break
            cur = max(cur, o + sz)
        if off is None:
            if s.arena_bytes - cur >= size:
                off = cur
            else:
                raise AssertionError("arena OOM allocating %s %s (%d B); used=%s" % (name, shape, size, sum(z for _, z in s.alloc)))
        s.alloc.append((off, size))
        ap = s.arena[0:shape[0], off // 4:(off + size) // 4]
        if dt == BF16:
            ap = ap.bitcast(BF16)
        ap = ap[:, 0:free]
        if len(shape) == 3:
            ap = ap.rearrange("p (a b) -> p a b", a=shape[1])
        elif len(shape) == 4:
            ap = ap.rearrange("p (a b c) -> p a b c", a=shape[1], b=shape[2])
        b = Buf(name, ap)
        b.alloc = (off, size)
        if grp is not None:
            grp["bufs"].append(b)
        elif not glob and s.pes is not None:
            s.phase_bufs.append(b)
        return b

    def newgrp(s):
        return dict(bufs=[])

    def freegrp(s, g):
        for b in g["bufs"]:
            s.alloc.remove(b.alloc)
        g["bufs"] = []

    def _wait(s, eng, need):
        E = s.E[eng]
        for key, (h, val) in need.items():
            if s.known[eng].get(key, 0) < val:
                E.wait_ge(h, val)
                s.known[eng][key] = val

    def op(s, eng, fn, reads=(), writes=(), pwrites=()):
        need = {}

        def add(evs, raw):
            for key, (h, val, e) in evs.items():
                if e == eng and (eng == "pe" or not raw):
                    continue
                if key not in need or need[key][1] < val:
                    need[key] = (h, val)
        for b in reads:
            add(b.w, True)
        for b in writes:
            add(b.w, False)
            add(b.r, False)
        for b in pwrites:
            add(b.r, False)
        s._wait(eng, need)
        inst = fn(s.E[eng])
        s.cnt[eng] += 1
        inst.then_inc(s.sem[eng], 1)
        ev = (s.sem[eng], s.cnt[eng], eng)
        for b in reads:
            b.r[eng] = ev
        for b in writes:
            b.w = {eng: ev}
            b.r = {}
        for b in pwrites:
            b.w[eng] = ev

    def dma(s, q, out_ap, in_ap, src, dst, partial=False, ndesc=None):
        need = {}
        if q == "pool":
            if ndesc is None:
                shp = list(out_ap.shape)
                ndesc = 1
                for d_ in shp[:-1]:
                    ndesc *= d_
            fifo = s.__dict__.setdefault("pool_fifo", [])
            tot = sum(n for _, n in fifo)
            while fifo and tot + ndesc > 11000:
                (key0, ev0), n0 = fifo.pop(0)
                tot -= n0
                need[key0] = (ev0[0], ev0[1])

        def add(evs):
            for key, (h, val, e) in evs.items():
                if key not in need or need[key][1] < val:
                    need[key] = (h, val)
        add(src.w)
        add(dst.r)
        if not partial:
            add(dst.w)
        s._wait(q, need)
        inst = s.E[q].dma_start(out=out_ap, in_=in_ap)
        if dst.dsem is None:
            dst.dsem = s.free_dsems.pop()
            s.used_dsems.append((dst, dst.dsem))
        sem = dst.dsem
        sem.total += 16
        inst.then_inc(sem.h, 16)
        key = ("d", id(sem))
        ev = (sem.h, sem.total, None)
        if q == "pool":
            s.pool_fifo.append(((key, ev), ndesc))
        src.r[key] = ev
        if partial:
            dst.w[key] = ev
        else:
            dst.w = {key: ev}
            dst.r = {}

    def barrier(s):
        for eng, E in s.E.items():
            need = {}
            for o in s.E:
                if s.cnt[o] > 0:
                    need[o] = (s.sem[o], s.cnt[o])
            for (b, sem) in s.used_dsems:
                if sem.total > 0:
                    need[("d", id(sem))] = (sem.h, sem.total)
            s._wait(eng, need)
        s.pool_fifo = []
        for (b, sem) in s.used_dsems:
            b.dsem = None
            s.free_dsems.append(sem)
        s.used_dsems = []

    def phase_begin(s):
        s.pes = True
        s.phase_bufs = []

    def phase_end(s):
        s.barrier()
        for b in s.phase_bufs:
            s.alloc.remove(b.alloc)
        s.pes = None
        s.phase_bufs = []


def chunks_of(c_lo, c_hi, step=512):
    out = []
    c = c_lo
    while c < c_hi:
        out.append((c, min(c + step, c_hi)))
        c += step
    return out


class Prog:
    def __init__(self, cfg):
        self.cfg = cfg
        self.nc = bass.Bass("TRN2", target_bir_lowering=False)
        self.ins = {}
        self.outs = {}

    def din(self, name, shape, dt=F32):
        t = self.nc.dram_tensor(name, list(shape), dt, kind="ExternalInput")
        b = Buf(name, t)
        b.ap = t.ap()
        self.ins[name] = b
        return b

    def dout(self, name, shape, dt=F32):
        t = self.nc.dram_tensor(name, list(shape), dt, kind="ExternalOutput")
        b = Buf(name, t)
        b.ap = t.ap()
        self.outs[name] = b
        return b

    def dscr(self, name, shape, dt):
        t = self.nc.dram_tensor(name, list(shape), dt, kind="Internal")
        b = Buf(name, t)
        b.ap = t.ap()
        return b

    def load_consts(self):
        kb = self.kb
        c = self.din("c_ident", [128, 128])
        self.ident_bf = kb.sb("ident_bf", [128, 128], BF16, glob=True)
        kb.dma("pool", self.ident_bf.t[:], c.ap, c, self.ident_bf)
        self.ident_f = kb.sb("ident_f", [128, 128], F32, glob=True)
        kb.dma("sp", self.ident_f.t[:], c.ap, c, self.ident_f)
        c2 = self.din("c_negtri", [128, 128])
        self.negtri = kb.sb("negtri", [128, 128], BF16, glob=True)
        kb.dma("pool", self.negtri.t[:], c2.ap, c2, self.negtri)
        c3 = self.din("c_trix", [128, 128])
        self.trix = kb.sb("trix", [128, 128], BF16, glob=True)
        kb.dma("pool", self.trix.t[:], c3.ap, c3, self.trix)
        self.ones_bf = kb.sb("ones_bf", [128, 128], BF16, glob=True)
        kb.op("dve", lambda e: e.memset(self.ones_bf.t[:], 1.0), writes=[self.ones_bf])
        self.ones_f = kb.sb("ones_f", [128, 128], F32, glob=True)
        kb.op("dve", lambda e: e.memset(self.ones_f.t[:], 1.0), writes=[self.ones_f])
        self.epsc = kb.sb("epsc", [128, 1], F32, glob=True)
        kb.op("dve", lambda e: e.memset(self.epsc.t[:], EPS), writes=[self.epsc])

    def load_rows(self, name_g, name_b):
        kb = self.kb
        g = self.ins[name_g]
        b = self.ins[name_b]
        gr = kb.sb("gr_" + name_g, [128, D], F32)
        br = kb.sb("br_" + name_b, [128, D], F32)
        kb.dma("sp", gr.t[:], g.ap.partition_broadcast(128), g, gr)
        kb.dma("sp", br.t[:], b.ap.partition_broadcast(128), b, br)
        return gr, br

    def make_ln_bufs(self, nset=2):
        kb = self.kb
        sets = []
        for i in range(nset):
            sets.append(dict(
                x=kb.sb("ln_x%d" % i, [128, D], F32),
                xn=kb.sb("ln_xn%d" % i, [128, D], F32),
                hb=kb.sb("ln_hb%d" % i, [128, D], BF16),
                st=kb.sb("ln_st%d" % i, [128, 4, 6], F32),
                mv=kb.sb("ln_mv%d" % i, [128, 2], F32),
                sc=kb.sb("ln_sc%d" % i, [128, 4], F32),
            ))
        self.ln_sets = sets
        self.ln_i = 0

    def ln_core(self, S, gr, br, want_h32, want_hb=True):
        kb = self.kb
        x, xn, hb, st, mv, sc = S["x"], S["xn"], S["hb"], S["st"], S["mv"], S["sc"]
        for j in range(4):
            kb.op("dve", lambda e, j=j: e.bn_stats(out=st.t[:, j, :], in_=x.t[:, j * 512:(j + 1) * 512]),
                  reads=[x], pwrites=[st] if j else (), writes=() if j else [st])
        kb.op("dve", lambda e: e.bn_aggr(out=mv.t[:], in_=st.t[:].rearrange("p a b -> p (a b)")), reads=[st], writes=[mv])
        kb.op("act", lambda e: e.activation(out=sc.t[:, 0:1], in_=mv.t[:, 1:2], func=AF.Ln, bias=self.epsc.t[:, 0:1], scale=1.0),
              reads=[mv, self.epsc], writes=[sc])
        kb.op("act", lambda e: e.activation(out=sc.t[:, 1:2], in_=sc.t[:, 0:1], func=AF.Exp, scale=-0.5),
              reads=[sc], pwrites=[sc])
        kb.op("dve", lambda e: e.tensor_scalar(out=sc.t[:, 2:3], in0=mv.t[:, 0:1], scalar1=sc.t[:, 1:2], scalar2=-1.0,
                                               op0=ALU.mult, op1=ALU.mult), reads=[mv, sc], pwrites=[sc])
        kb.op("act", lambda e: e.activation(out=xn.t[:], in_=x.t[:], func=AF.Identity, bias=sc.t[:, 2:3], scale=sc.t[:, 1:2]),
              reads=[x, sc], writes=[xn])
        kb.op("pool", lambda e: e.tensor_tensor(out=xn.t[:], in0=xn.t[:], in1=gr.t[:], op=ALU.mult), reads=[xn, gr], writes=[xn])
        if want_h32:
            kb.op("dve", lambda e: e.tensor_tensor(out=x.t[:], in0=xn.t[:], in1=br.t[:], op=ALU.add), reads=[xn, br], writes=[x])
            if want_hb:
                kb.op("act", lambda e: e.activation(out=hb.t[:], in_=x.t[:], func=AF.Copy), reads=[x], writes=[hb])
        else:
            kb.op("dve", lambda e: e.tensor_tensor(out=hb.t[:], in0=xn.t[:], in1=br.t[:], op=ALU.add), reads=[xn, br], writes=[hb])

    def to_fm(self, src, dst, off, nchunk=16, src_col0=0, evac_alt=0):
        kb = self.kb
        for h0 in range(0, nchunk, 8):
            n = min(8, nchunk - h0)
            bk = kb.bank()
            bv = bk.t[:].bitcast(BF16)
            for k in range(n):
                kk = h0 + k
                kb.op("pe", lambda e, k=k, kk=kk: e.transpose(out=bv[:, k * 128:(k + 1) * 128],
                                                               in_=src.t[:, src_col0 + kk * 128: src_col0 + (kk + 1) * 128],
                                                               identity=self.ident_bf.t[:]),
                      reads=[src, self.ident_bf], writes=[bk] if k == 0 else (), pwrites=() if k == 0 else [bk])
            srcv = bv[:, 0:n * 128].rearrange("p (a b) -> p a b", a=n)
            dstv = dst.t[:, h0:h0 + n, off:off + 128]
            if (evac_alt + h0 // 8) % 2 == 0:
                kb.op("dve", lambda e: e.tensor_copy(out=dstv, in_=srcv), reads=[bk], pwrites=[dst])
            else:
                kb.op("act", lambda e: e.activation(out=dstv, in_=srcv, func=AF.Copy), reads=[bk], pwrites=[dst])

    def lnA(self, src_ap, src_buf, gr, br, hb):
        kb = self.kb
        S = dict(self.ln_sets[self.ln_i % len(self.ln_sets)])
        self.ln_i += 1
        S["hb"] = hb
        kb.dma("sp", S["x"].t[:], src_ap, src_buf, S["x"])
        self.ln_core(S, gr, br, want_h32=False)

    def ln_from_dram(self, src_ap, src_buf, gr, br, dst_fm, off, h32_dst=None, h32_buf=None):
        kb = self.kb
        S = self.ln_sets[self.ln_i % len(self.ln_sets)]
        self.ln_i += 1
        kb.dma("sp", S["x"].t[:], src_ap, src_buf, S["x"])
        self.ln_core(S, gr, br, want_h32=h32_dst is not None)
        if h32_dst is not None:
            kb.dma("sp", h32_dst, S["x"].t[:], S["x"], h32_buf, partial=True)
        self.to_fm(S["hb"], dst_fm, off, evac_alt=self.ln_i)

    def load_w(self, dst, w_ap, wbuf, split=1):
        kb = self.kb
        src = w_ap.rearrange("(c p) n -> p c n", p=128)
        nk = src.shape[1]
        step = (nk + split - 1) // split
        for i, k0 in enumerate(range(0, nk, step)):
            k1 = min(nk, k0 + step)
            kb.dma("pool", dst.t[:, k0:k1, :], src[:, k0:k1, :], wbuf, dst, partial=(i > 0))

    def mm_fm(self, bank, w, wcol, xT, tok0, ntok, nk, reads_extra=()):
        kb = self.kb
        for k in range(nk):
            kb.op("pe", lambda e, k=k: e.matmul(bank.t[:, 0:ntok], lhsT=w.t[:, k, wcol:wcol + 128], rhs=xT.t[:, k, tok0:tok0 + ntok],
                                                start=(k == 0), stop=(k == nk - 1)),
                  reads=[w, xT], writes=[bank] if k == 0 else (), pwrites=() if k == 0 else [bank])

    def mm_tm(self, bank, xT, tok0, w, wcol, ncol, nk, bias_row=None, bias_col0=0):
        kb = self.kb
        first = True
        if bias_row is not None:
            kb.op("pe", lambda e: e.matmul(bank.t[:, 0:ncol], lhsT=self.ones_bf.t[0:1, :], rhs=bias_row.t[0:1, bias_col0:bias_col0 + ncol],
                                           start=True, stop=False),
                  reads=[self.ones_bf, bias_row], writes=[bank])
            first = False
        for k in range(nk):
            kb.op("pe", lambda e, k=k, st=(first and k == 0): e.matmul(bank.t[:, 0:ncol], lhsT=xT.t[:, k, tok0:tok0 + 128],
                                                                       rhs=w.t[:, k, wcol:wcol + ncol], start=st, stop=(k == nk - 1)),
                  reads=[w, xT], writes=[bank] if (first and k == 0) else (), pwrites=() if (first and k == 0) else [bank])

    def stage1(self):
        kb = self.kb
        cfg = self.cfg
        x_own, x_prev, x_ctx = self.ins["x_own"], self.ins["x_prev"], self.ins["x_ctx"]
        w_in = self.ins["w_in"]
        QA0, KA0, VA0, QB0, KB0, VB0, GA0, GB0 = 0, 1024, 1152, 1280, 2304, 3328, 4352, 6400
        bcol = self.bcol
        brow = self.brow

        kb.phase_begin()
        gr, br = self.load_rows("ln_in_g", "ln_in_b")
        self.make_ln_bufs(2)
        wk = kb.sb("wk", [128, 16, 1024], BF16)
        wv = kb.sb("wv", [128, 16, 1024], BF16)
        self.load_w(wk, w_in.ap[:, KB0:KB0 + 1024], w_in, split=2)
        self.load_w(wv, w_in.ap[:, VB0:VB0 + 1024], w_in, split=2)
        hTg = [kb.sb("hTg%d" % i, [128, 16, 512], BF16) for i in range(2)]
        kst = [kb.sb("kst%d" % i, [128, 8, 512], BF16) for i in range(2)]
        vst = [kb.sb("vst%d" % i, [128, 4, 1024], BF16) for i in range(2)]
        kT_scr, V_scr = self.kT_scr, self.V_scr
        hbr = [kb.sb("hbr%d" % i, [128, D], BF16) for i in range(4)]

        def A_(g):
            for tt in range(4):
                r0 = g * 512 + tt * 128
                self.lnA(x_ctx.ap[r0:r0 + 128, :], x_ctx, gr, br, hbr[tt])

        def B_(g):
            for tt in range(4):
                self.to_fm(hbr[tt], hTg[g % 2], tt * 128, evac_alt=tt)
        A_(0)
        B_(0)
        for g in range(8):
            hT = hTg[g % 2]
            if g + 1 < 8:
                A_(g + 1)
            ks = kst[g % 2]
            for fc in range(8):
                bk = kb.bank()
                self.mm_fm(bk, wk, fc * 128, hT, 0, 512, 16)
                kb.op("act", lambda e, fc=fc, bk=bk: e.activation(out=ks.t[:, fc, :], in_=bk.t[:, :], func=AF.Identity,
                                                                   bias=bcol.t[:, 18 + fc:19 + fc], scale=1.0),
                      reads=[bk, bcol], pwrites=[ks] if fc else (), writes=() if fc else [ks])
            kb.dma("sp", kT_scr.ap.rearrange("f p t -> p f t")[:, :, g * 512:(g + 1) * 512], ks.t[:], ks, kT_scr, partial=True)
            if g + 1 < 8:
                B_(g + 1)
            vs = vst[g % 2]
            for tt in range(4):
                for nch in range(2):
                    bk = kb.bank()
                    self.mm_tm(bk, hT, tt * 128, wv, nch * 512, 512, 16, bias_row=brow, bias_col0=128 + nch * 512)
                    kb.op("dve", lambda e, tt=tt, nch=nch, bk=bk: e.tensor_copy(out=vs.t[:, tt, nch * 512:(nch + 1) * 512], in_=bk.t[:, :]),
                          reads=[bk], pwrites=[vs] if (tt or nch) else (), writes=() if (tt or nch) else [vs])
            kb.dma("sp", V_scr.ap[g * 4:(g + 1) * 4].rearrange("t p n -> p t n"), vs.t[:], vs, V_scr, partial=True)
        kb.phase_end()
        if cfg.get("s1") == "a":
            return

        kb.phase_begin()
        gr, br = self.load_rows("ln_in_g", "ln_in_b")
        self.make_ln_bufs(2)
        P = {}
        self.P1 = P
        self.gA = kb.newgrp(); self.gQB = kb.newgrp(); self.gSWA = kb.newgrp(); self.gT = kb.newgrp()
        hT_own = kb.sb("hT_own", [128, 16, 1024], BF16, grp=self.gA)
        P["hT_own"] = hT_own
        gPrev = kb.newgrp()
        hT_prev = kb.sb("hT_prev", [128, 16, 1024], BF16, grp=gPrev)
        for i in range(8):
            self.ln_from_dram(x_own.ap[i * 128:(i + 1) * 128, :], x_own, gr, br, hT_own, i * 128,
                              h32_dst=self.h0_scr.ap[i * 128:(i + 1) * 128, :], h32_buf=self.h0_scr)
        for i in range(8):
            self.ln_from_dram(x_prev.ap[i * 128:(i + 1) * 128, :], x_prev, gr, br, hT_prev, i * 128)
        kb.phase_end()
        kb.phase_begin()
        qBT = kb.sb("qBT", [128, 8, 1024], BF16, grp=self.gQB)
        qAT = kb.sb("qAT", [128, 8, 1024], BF16, grp=self.gSWA)
        kAT = kb.sb("kAT", [128, 2, 1024], BF16, grp=self.gSWA)
        vA = kb.sb("vA", [128, 2, 8, 128], BF16, grp=self.gSWA)
        P.update(qBT=qBT, qAT=qAT, kAT=kAT, vA=vA)
        wkv = kb.sb("wkv", [128, 16, 256], BF16)
        self.load_w(wkv, w_in.ap[:, KA0:KA0 + 256], w_in)
        wq = kb.sb("wq", [128, 16, 1024], BF16)
        self.load_w(wq, w_in.ap[:, QB0:QB0 + 1024], w_in, split=2)
        wqa = kb.sb("wqa", [128, 16, 1024], BF16)
        wqa_t = wqa.t[:].rearrange("p c (j m) -> p c j m", m=128)
        for a in range(2):
            src = w_in.ap[:, QA0 + a * 512:QA0 + (a + 1) * 512].rearrange("(c p) (j d) -> p c j d", p=128, d=64)
            for c0 in range(16):
                kb.dma("pool", wqa_t[:, c0, :, a * 64:(a + 1) * 64], src[:, c0], w_in, wqa, partial=not (a == 0 and c0 == 0))
        for which, hT in ((0, hT_prev), (1, hT_own)):
            for th in range(2):
                bk = kb.bank()
                self.mm_fm(bk, wkv, 0, hT, th * 512, 512, 16)
                kb.op("act", lambda e, th=th, bk=bk, which=which: e.activation(out=kAT.t[:, which, th * 512:(th + 1) * 512], in_=bk.t[:, :],
                                                                             func=AF.Identity, bias=bcol.t[:, 8:9], scale=1.0),
                      reads=[bk, bcol], pwrites=[kAT])
            for i in range(8):
                bk = kb.bank()
                self.mm_tm(bk, hT, i * 128, wkv, 128, 128, 16, bias_row=brow, bias_col0=0)
                kb.op("dve", lambda e, i=i, bk=bk, which=which: e.tensor_copy(out=vA.t[:, which, i, :], in_=bk.t[:, 0:128]),
                      reads=[bk], pwrites=[vA])
        for fc in range(8):
            for th in range(2):
                bk = kb.bank()
                self.mm_fm(bk, wq, fc * 128, hT_own, th * 512, 512, 16)
                kb.op("dve", lambda e, fc=fc, th=th, bk=bk: e.tensor_scalar(out=qBT.t[:, fc, th * 512:(th + 1) * 512], in0=bk.t[:, :],
                                                                           scalar1=bcol.t[:, 10 + fc:11 + fc], scalar2=0.125,
                                                                           op0=ALU.add, op1=ALU.mult),
                      reads=[bk, bcol], pwrites=[qBT])
        wqa_v = Buf("wqa_v", None)
        for fc in range(8):
            for th in range(2):
                bk = kb.bank()
                for k in range(16):
                    kb.op("pe", lambda e, k=k, fc=fc, th=th, bk=bk: e.matmul(bk.t[:, :], lhsT=wqa_t[:, k, fc, :], rhs=hT_own.t[:, k, th * 512:(th + 1) * 512],
                                                                             start=(k == 0), stop=(k == 15)),
                          reads=[wqa, hT_own], writes=[bk] if k == 0 else (), pwrites=() if k == 0 else [bk])
                kb.op("dve", lambda e, fc=fc, th=th, bk=bk: e.tensor_scalar(out=qAT.t[:, fc, th * 512:(th + 1) * 512], in0=bk.t[:, :],
                                                                           scalar1=self.bcol_qa.t[:, fc:fc + 1], scalar2=0.125,
                                                                           op0=ALU.add, op1=ALU.mult),
                      reads=[bk, self.bcol_qa], pwrites=[qAT])
        kb.phase_end()
        kb.freegrp(gPrev)
        if cfg.get("s1") == "b":
            return

        kb.phase_begin()
        swab = kb.sb("swab", [128, 16, 256], F32)
        kb.dma("sp", swab.t[:], self.ins["swab"].ap, self.ins["swab"], swab)
        pm0 = kb.sb("pm0", [128, 256], F32)
        kb.dma("sp", pm0.t[:], self.ins["pm0"].ap, self.ins["pm0"], pm0)
        sinkb = kb.sb("sinkb", [128, 16], F32)
        kb.dma("sp", sinkb.t[:], self.ins["attn_sinks"].ap.partition_broadcast(128), self.ins["attn_sinks"], sinkb)
        swa_tm = [kb.sb("swa_tm%d" % i, [128, 1024], BF16) for i in range(2)]
        swaT = kb.sb("swaT", [128, 8, 1024], BF16, grp=self.gT)
        P["swaT"] = swaT
        NS = 3
        dbgt = [kb.sb('dbgt%d' % i, [128, 1024], F32) for i in range(2)]
        l32 = [kb.sb("l32_%d" % i, [128, 512], F32) for i in range(NS)]
        pbf = [kb.sb("pbf_%d" % i, [128, 512], BF16) for i in range(NS)]
        pT = [kb.sb("pT_%d" % i, [128, 4, 128], BF16) for i in range(NS)]
        sm = [kb.sb("sm_%d" % i, [128, 16], F32) for i in range(NS)]
        qAT, kAT, vA = P["qAT"], P["kAT"], P["vA"]
        it = 0
        for i in range(8):
            so = swa_tm[i % 2]
            for hpair in range(8):
                h = 2 * hpair
                s = it % NS
                it += 1
                a = h // 8
                Z = kb.bank()
                first = True
                for hh in range(2):
                    j = (h + hh) % 8
                    for c in range(2):
                        kb.op("pe", lambda e, c=c, hh=hh, j=j: e.matmul(Z.t[:, hh * 256 + c * 128: hh * 256 + (c + 1) * 128],
                                                                        lhsT=qAT.t[a * 64:(a + 1) * 64, j, i * 128:(i + 1) * 128],
                                                                        rhs=kAT.t[a * 64:(a + 1) * 64, c, i * 128:(i + 1) * 128], start=True, stop=True),
                              reads=[qAT, kAT], writes=[Z] if first else (), pwrites=() if first else [Z])
                        first = False
                kb.op("dve", lambda e: e.tensor_tensor(out=l32[s].t[:], in0=Z.t[:, :], in1=swab.t[:, h:h + 2, :].rearrange("p a b -> p (a b)"), op=ALU.add),
                      reads=[Z, swab], writes=[l32[s]])
                if i == 0:
                    for hh in range(2):
                        kb.op("dve", lambda e, hh=hh: e.tensor_tensor(out=l32[s].t[:, hh * 256:(hh + 1) * 256], in0=l32[s].t[:, hh * 256:(hh + 1) * 256],
                                                                      in1=pm0.t[:], op=ALU.add),
                              reads=[l32[s], pm0], writes=[l32[s]])
                kb.op("dve", lambda e: e.reduce_max(out=sm[s].t[:, 0:2], in_=l32[s].t[:].rearrange("p (a b) -> p a b", a=2), axis=AX.X),
                      reads=[l32[s]], writes=[sm[s]])
                kb.op("dve", lambda e: e.tensor_scalar(out=sm[s].t[:, 2:4], in0=sm[s].t[:, 0:2], scalar1=-1.0, scalar2=None, op0=ALU.mult),
                      reads=[sm[s]], pwrites=[sm[s]])
                kb.op("dve", lambda e: e.tensor_tensor(out=sm[s].t[:, 4:6], in0=sinkb.t[:, h:h + 2], in1=sm[s].t[:, 2:4], op=ALU.add),
                      reads=[sm[s], sinkb], pwrites=[sm[s]])
                for hh in range(2):
                    kb.op("act", lambda e, hh=hh: e.activation(out=pbf[s].t[:, hh * 256:(hh + 1) * 256], in_=l32[s].t[:, hh * 256:(hh + 1) * 256], func=AF.Exp,
                                                               bias=sm[s].t[:, 2 + hh:3 + hh], scale=1.0, accum_out=sm[s].t[:, 6 + hh:7 + hh]),
                          reads=[l32[s], sm[s]], writes=[pbf[s]] if hh == 0 else (), pwrites=[sm[s]] if hh == 0 else [sm[s], pbf[s]])
                kb.op("act", lambda e: e.activation(out=sm[s].t[:, 8:10], in_=sm[s].t[:, 4:6], func=AF.Exp), reads=[sm[s]], pwrites=[sm[s]])
                kb.op("dve", lambda e: e.tensor_tensor(out=sm[s].t[:, 10:12], in0=sm[s].t[:, 6:8], in1=sm[s].t[:, 8:10], op=ALU.add),
                      reads=[sm[s]], pwrites=[sm[s]])
                kb.op("dve", lambda e: e.reciprocal(out=sm[s].t[:, 12:14], in_=sm[s].t[:, 10:12]), reads=[sm[s]], pwrites=[sm[s]])
                PT = kb.bank()
                ptv = PT.t[:].bitcast(BF16)
                for c in range(4):
                    kb.op("pe", lambda e, c=c: e.transpose(out=ptv[:, c * 128:(c + 1) * 128], in_=pbf[s].t[:, c * 128:(c + 1) * 128],
                                                           identity=self.ident_bf.t[:]),
                          reads=[pbf[s], self.ident_bf], writes=[PT] if c == 0 else (), pwrites=() if c == 0 else [PT])
                kb.op("act", lambda e: e.activation(out=pT[s].t[:].rearrange("p a b -> p (a b)"), in_=ptv[:, 0:512], func=AF.Copy),
                      reads=[PT], writes=[pT[s]])
                O = kb.bank()
                first = True
                for hh in range(2):
                    for c in range(2):
                        kb.op("pe", lambda e, c=c, hh=hh: e.matmul(O.t[:, hh * 64:(hh + 1) * 64], lhsT=pT[s].t[:, hh * 2 + c, :], rhs=vA.t[:, c, i, a * 64:(a + 1) * 64],
                                                                   start=(c == 0), stop=(c == 1)),
                              reads=[pT[s], vA], writes=[O] if first else (), pwrites=() if first else [O])
                        first = False
                for hh in range(2):
                    hd = h + hh
                    kb.op("dve", lambda e, hh=hh, hd=hd: e.tensor_scalar(out=so.t[:, hd * 64:(hd + 1) * 64], in0=O.t[:, hh * 64:(hh + 1) * 64],
                                                                        scalar1=sm[s].t[:, 12 + hh:13 + hh], scalar2=None, op0=ALU.mult),
                          reads=[O, sm[s]], pwrites=[so] if hd else (), writes=() if hd else [so])
            self.to_fm(so, swaT, i * 128, nchunk=8, evac_alt=i)
            if cfg.get("dbg") == "swa":
                dt_ = dbgt[i % 2]
                kb.op("dve", lambda e, dt_=dt_: e.tensor_copy(out=dt_.t[:], in_=so.t[:]), reads=[so], writes=[dt_])
                kb.dma("sp", self.outs["dbg"].ap[i * 128:(i + 1) * 128, 0:1024], dt_.t[:], dt_, self.outs["dbg"], partial=True)
        kb.phase_end()
        kb.freegrp(self.gSWA)
        if cfg.get("s1") == "d":
            return

        kb.phase_begin()
        sbmask = kb.sb("sbmask", [128, 4, 128], BF16)
        kb.dma("pool", sbmask.t[:], self.ins["sbmask"].ap, self.ins["sbmask"], sbmask)
        accs = [kb.sb("sbacc%d" % i, [128, 1024], F32) for i in range(8)]
        for i in range(8):
            kb.op("pool", lambda e, i=i: e.memset(accs[i].t[:], 0.0), writes=[accs[i]])
        kTp = [kb.sb("kTp%d" % i, [128, 4096], BF16) for i in range(2)]
        vP = [kb.sb("vP%d" % i, [128, 32, 128], BF16) for i in range(2)]
        carry = kb.sb("carry", [128, 8], F32)
        Eb = [kb.sb("Eb%d" % i, [128, 8], F32) for i in range(2)]
        NU = 3
        e32 = [kb.sb("e32_%d" % i, [128, 512], F32) for i in range(NU)]
        spb = [kb.sb("spb_%d" % i, [128, 512], BF16) for i in range(NU)]
        wb = [kb.sb("wb_%d" % i, [128, 512], BF16) for i in range(NU)]
        n_hp = cfg.get("n_hp", 8)
        units = []
        for hp in range(n_hp):
            for hh in range(2):
                for kbi in range(31, -1, -1):
                    g = kbi // 4
                    chs = chunks_of(g * 128, 1024)
                    for ci, (c0, c1) in enumerate(chs):
                        units.append(dict(hp=hp, hh=hh, kb=kbi, g=g, r=kbi % 4, c0=c0, c1=c1, first=(ci == 0), last=(ci == len(chs) - 1),
                                          newhead=(kbi == 31 and ci == 0), newpair=(kbi == 31 and ci == 0 and hh == 0)))
        Abanks = [kb.banks[i] for i in range(5)]
        Obanks = [kb.banks[5], kb.banks[6]]
        Crun = kb.banks[7]
        zeros_bf = kb.sb("zeros_bf", [128, 128], BF16)
        kb.op("dve", lambda e: e.memset(zeros_bf.t[:], 0.0), writes=[zeros_bf])
        Eb4 = [kb.sb("Eb4_%d" % i, [128, 8], F32) for i in range(4)]
        ek = -1
        for u in units:
            if u["first"]:
                ek += 1
            u["ek"] = ek

        def st1(u, ui):
            hp, hh = u["hp"], u["hh"]
            if u["newpair"]:
                kb.dma("sp", kTp[hp % 2].t[:], self.kT_scr.ap[hp], self.kT_scr, kTp[hp % 2])
                vsrc = self.V_scr.ap.rearrange("t p n -> p t n")
                for t0 in range(0, 32, 8):
                    kb.dma("sp", vP[hp % 2].t[:, t0:t0 + 8, :], vsrc[:, t0:t0 + 8, hp * 128:(hp + 1) * 128], self.V_scr, vP[hp % 2], partial=(t0 > 0))
            A = Abanks[ui % 5]
            n = u["c1"] - u["c0"]
            ps0 = hh * 64
            kk = kTp[hp % 2]
            kb.op("pe", lambda e: e.matmul(A.t[:, 0:n], lhsT=kk.t[ps0:ps0 + 64, u["kb"] * 128:(u["kb"] + 1) * 128],
                                           rhs=qBT_.t[ps0:ps0 + 64, hp, u["c0"]:u["c1"]], start=True, stop=True),
                  reads=[kk, qBT_], writes=[A])
            if u["first"]:
                kb.op("pe", lambda e: e.matmul(A.t[:, 0:128], lhsT=self.ident_bf.t[:], rhs=sbmask.t[:, u["r"], :], start=False, stop=True),
                      reads=[self.ident_bf, sbmask], pwrites=[A])

        def st2(u, ui):
            A = Abanks[ui % 5]
            s = ui % NU
            n = u["c1"] - u["c0"]
            kb.op("act", lambda e: e.activation(out=e32[s].t[:, 0:n], in_=A.t[:, 0:n], func=AF.Exp), reads=[A], writes=[e32[s]])
            kb.op("act", lambda e: e.activation(out=spb[s].t[:, 0:n], in_=e32[s].t[:, 0:n], func=AF.Ln, bias=self.ones_f.t[:, 0:1], scale=1.0),
                  reads=[e32[s], self.ones_f], writes=[spb[s]])
            kb.op("pe", lambda e: e.matmul(A.t[:, 0:n], lhsT=self.negtri.t[:], rhs=spb[s].t[:, 0:n], start=False, stop=True),
                  reads=[self.negtri, spb[s]], pwrites=[A])
            if u["newhead"]:
                kb.op("pe", lambda e: e.matmul(Crun.t[:, 0:8], lhsT=zeros_bf.t[:], rhs=self.ones_bf.t[:, 0:8], start=True, stop=True),
                      reads=[zeros_bf, self.ones_bf], writes=[Crun])
            if u["first"]:
                Ecur = Eb4[u["ek"] % 4]
                kb.op("act", lambda e: e.activation(out=Ecur.t[:], in_=Crun.t[:, 0:8], func=AF.Exp, scale=-1.0), reads=[Crun], writes=[Ecur])
            for t in range(n // 128):
                i = u["c0"] // 128 + t
                kb.op("pe", lambda e, t=t, i=i: e.matmul(Crun.t[:, i:i + 1], lhsT=spb[s].t[:, t * 128:(t + 1) * 128], rhs=self.ones_bf.t[:, 0:1],
                                                         start=False, stop=True),
                      reads=[spb[s], self.ones_bf], pwrites=[Crun])

        def st3(u, ui):
            hp, hh = u["hp"], u["hh"]
            head = hp * 2 + hh
            A = Abanks[ui % 5]
            s = ui % NU
            n = u["c1"] - u["c0"]
            O = Obanks[ui % 2]
            Ecur = Eb4[u["ek"] % 4]
            kb.op("act", lambda e: e.activation(out=wb[s].t[:, 0:n], in_=A.t[:, 0:n], func=AF.Exp), reads=[A], writes=[wb[s]])
            vv = vP[hp % 2]
            for t in range(n // 128):
                kb.op("pe", lambda e, t=t: e.matmul(O.t[:, t * 64:(t + 1) * 64], lhsT=wb[s].t[:, t * 128:(t + 1) * 128],
                                                    rhs=vv.t[:, u["kb"], hh * 64:(hh + 1) * 64], start=True, stop=True),
                      reads=[wb[s], vv], writes=[O] if t == 0 else (), pwrites=() if t == 0 else [O])
            for t in range(n // 128):
                i = u["c0"] // 128 + t
                kb.op("dve", lambda e, t=t, i=i: e.scalar_tensor_tensor(out=accs[i].t[:, head * 64:(head + 1) * 64], in0=O.t[:, t * 64:(t + 1) * 64],
                                                                        scalar=Ecur.t[:, i:i + 1], in1=accs[i].t[:, head * 64:(head + 1) * 64],
                                                                        op0=ALU.mult, op1=ALU.add),
                      reads=[O, Ecur, accs[i]], writes=[accs[i]])

        qBT_ = P["qBT"]
        NUU = len(units)
        for it_ in range(NUU + 3):
            if it_ < NUU:
                st1(units[it_], it_)
            if 1 <= it_ <= NUU:
                st2(units[it_ - 1], it_ - 1)
            if it_ >= 3:
                st3(units[it_ - 3], it_ - 3)
        sbT = kb.sb("sbT", [128, 8, 1024], BF16, grp=self.gT)
        P["sbT"] = sbT
        cvt = [kb.sb("sbcvt%d" % i, [128, 1024], BF16) for i in range(2)]
        for i in range(8):
            cb = cvt[i % 2]
            kb.op("act", lambda e, i=i, cb=cb: e.activation(out=cb.t[:], in_=accs[i].t[:], func=AF.Copy), reads=[accs[i]], writes=[cb])
            self.to_fm(cb, sbT, i * 128, nchunk=8, evac_alt=i)
        if cfg.get("dbg") == "sb":
            for i in range(8):
                kb.dma("sp", self.outs["dbg"].ap[i * 128:(i + 1) * 128, 0:1024], accs[i].t[:], accs[i], self.outs["dbg"], partial=True)
        kb.phase_end()
        kb.freegrp(self.gQB)

        kb.phase_begin()
        gM = kb.newgrp()
        mixT = kb.sb("mixT", [128, 16, 1024], BF16, grp=gM)
        NW = 2
        wa = [kb.sb("wa%d" % i, [128, 8, 256], BF16) for i in range(NW)]
        wbb = [kb.sb("wbb%d" % i, [128, 8, 256], BF16) for i in range(NW)]
        wga = [kb.sb("wga%d" % i, [128, 16, 256], BF16) for i in range(NW)]
        wgb = [kb.sb("wgb%d" % i, [128, 16, 256], BF16) for i in range(NW)]
        sg = [kb.sb("sg%d" % i, [128, 512], F32) for i in range(4)]
        tm = [kb.sb("tm%d" % i, [128, 512], F32) for i in range(4)]
        w_a, w_b = self.ins["w_a_out"], self.ins["w_b_out"]
        swaT, sbT = P["swaT"], P["sbT"]
        hT_own = P["hT_own"]
        it = 0
        for grp in range(8):
            s = grp % NW
            c0 = grp * 256
            self.load_w(wa[s], w_a.ap[:, c0:c0 + 256], w_a)
            self.load_w(wbb[s], w_b.ap[:, c0:c0 + 256], w_b)
            self.load_w(wga[s], w_in.ap[:, GA0 + c0:GA0 + c0 + 256], w_in)
            self.load_w(wgb[s], w_in.ap[:, GB0 + c0:GB0 + c0 + 256], w_in)
            for f in range(2):
                fc = grp * 2 + f
                for th in range(2):
                    u = it % 2
                    it += 1
                    bya, byb, bga, bgb = kb.bank(), kb.bank(), kb.bank(), kb.bank()
                    self.mm_fm(bga, wga[s], f * 128, hT_own, th * 512, 512, 16)
                    self.mm_fm(bgb, wgb[s], f * 128, hT_own, th * 512, 512, 16)
                    self.mm_fm(bya, wa[s], f * 128, swaT, th * 512, 512, 8)
                    self.mm_fm(byb, wbb[s], f * 128, sbT, th * 512, 512, 8)
                    sga, sgb, t1, t2 = sg[2 * u], sg[2 * u + 1], tm[2 * u], tm[2 * u + 1]
                    kb.op("act", lambda e, fc=fc, sga=sga, bga=bga: e.activation(out=sga.t[:], in_=bga.t[:], func=AF.Sigmoid, bias=bcol.t[:, 34 + fc:35 + fc], scale=1.0),
                          reads=[bga, bcol], writes=[sga])
                    kb.op("act", lambda e, fc=fc, sgb=sgb, bgb=bgb: e.activation(out=sgb.t[:], in_=bgb.t[:], func=AF.Sigmoid, bias=bcol.t[:, 50 + fc:51 + fc], scale=1.0),
                          reads=[bgb, bcol], writes=[sgb])
                    kb.op("dve", lambda e, t1=t1, sga=sga, bya=bya: e.tensor_tensor(out=t1.t[:], in0=sga.t[:], in1=bya.t[:], op=ALU.mult), reads=[sga, bya], writes=[t1])
                    kb.op("dve", lambda e, t2=t2, sgb=sgb, byb=byb: e.tensor_tensor(out=t2.t[:], in0=sgb.t[:], in1=byb.t[:], op=ALU.mult), reads=[sgb, byb], writes=[t2])
                    kb.op("pool", lambda e, fc=fc, th=th, t1=t1, t2=t2: e.tensor_tensor(out=mixT.t[:, fc, th * 512:(th + 1) * 512], in0=t1.t[:], in1=t2.t[:], op=ALU.add),
                          reads=[t1, t2], pwrites=[mixT])
        if cfg.get("dbg") == "mix":
            dbgt = [kb.sb('dbgm%d' % i, [128, 1024], F32) for i in range(2)]
            for fc in range(16):
                dt_ = dbgt[fc % 2]
                kb.op("dve", lambda e, dt_=dt_, fc=fc: e.tensor_copy(out=dt_.t[:], in_=mixT.t[:, fc, :]), reads=[mixT], writes=[dt_])
                kb.dma("sp", self.outs["dbg"].ap[fc * 128:(fc + 1) * 128 if fc < 8 else (fc - 8) * 128 + 128, 0:1024] if fc < 8 else
                       self.outs["dbg"].ap[(fc - 8) * 128:(fc - 7) * 128, 1024:2048], dt_.t[:], dt_, self.outs["dbg"], partial=True)
        kb.phase_end()
        kb.freegrp(self.gA)
        kb.freegrp(self.gT)

        kb.phase_begin()
        gr1, br1 = self.load_rows("ln1_g", "ln1_b")
        self.make_ln_bufs(2)
        wmix = kb.sb("wmix", [128, 16, D], BF16)
        h0t = [kb.sb("h0t%d" % i, [128, D], F32) for i in range(2)]
        w_m = self.ins["w_mix_out"]
        self.load_w(wmix, w_m.ap, w_m, split=4)
        for i in range(8):
            S = self.ln_sets[i % 2]
            hh0 = h0t[i % 2]
            kb.dma("sp", hh0.t[:], self.h0_scr.ap[i * 128:(i + 1) * 128, :], self.h0_scr, hh0)
            for nch in range(4):
                bk = kb.bank()
                self.mm_tm(bk, mixT, i * 128, wmix, nch * 512, 512, 16)
                kb.op("dve", lambda e, bk=bk, S=S, hh0=hh0, nch=nch: e.scalar_tensor_tensor(out=S["x"].t[:, nch * 512:(nch + 1) * 512], in0=hh0.t[:, nch * 512:(nch + 1) * 512],
                                                                                           scalar=ALPHA, in1=bk.t[:, :], op0=ALU.mult, op1=ALU.add),
                      reads=[hh0, bk], writes=[S["x"]] if nch == 0 else (), pwrites=() if nch == 0 else [S["x"]])
            self.ln_core(S, gr1, br1, want_h32=True, want_hb=False)
            kb.dma("sp", self.h1_scr.ap[i * 128:(i + 1) * 128, :], S["x"].t[:], S["x"], self.h1_scr, partial=True)
        kb.phase_end()
        kb.freegrp(gM)

    def cast_load_fm(self, src_ap, src_buf, dst_fm, off, tiles):
        kb = self.kb
        t = tiles[self.cl_i % len(tiles)]
        self.cl_i += 1
        kb.dma("pool", t.t[:], src_ap, src_buf, t)
        self.to_fm(t, dst_fm, off, evac_alt=self.cl_i)

    def stage2(self):
        kb = self.kb
        cfg = self.cfg
        mem = self.ins["mem"]
        w_xq, w_xkv, w_xo = self.ins["w_xq"], self.ins["w_xkv"], self.ins["w_xo"]
        h1 = self.h1_scr
        self.cl_i = 0
        g2 = kb.newgrp()
        kxT = kb.sb("kxT", [128, 16, 256], BF16, grp=g2)
        vx = kb.sb("vx", [128, 2, D], BF16, grp=g2)
        qxT = kb.sb("qxT", [128, 16, 1024], BF16, grp=g2)
        kb.phase_begin()
        memT = kb.sb("memT", [128, 16, 256], BF16)
        h1T = kb.sb("h1T", [128, 16, 1024], BF16)
        ct = [kb.sb("ct%d" % i, [128, D], BF16) for i in range(2)]
        for i in range(2):
            self.cast_load_fm(mem.ap[i * 128:(i + 1) * 128, :], mem, memT, i * 128, ct)
        for i in range(8):
            self.cast_load_fm(h1.ap[i * 128:(i + 1) * 128, :], h1, h1T, i * 128, ct)
        wp = [kb.sb("wp%d" % i, [128, 16, 512], BF16) for i in range(2)]
        pi = 0
        for pc in range(4):
            w = wp[pi % 2]; pi += 1
            self.load_w(w, w_xkv.ap[:, pc * 512:(pc + 1) * 512], w_xkv)
            for f in range(4):
                bk = kb.bank()
                self.mm_fm(bk, w, f * 128, memT, 0, 256, 16)
                kb.op("act", lambda e, bk=bk, fc=pc * 4 + f: e.activation(out=kxT.t[:, fc, :], in_=bk.t[:, 0:256], func=AF.Copy),
                      reads=[bk], pwrites=[kxT])
        for pc in range(4):
            w = wp[pi % 2]; pi += 1
            self.load_w(w, w_xkv.ap[:, D + pc * 512:D + (pc + 1) * 512], w_xkv)
            for mt in range(2):
                bk = kb.bank()
                self.mm_tm(bk, memT, mt * 128, w, 0, 512, 16)
                kb.op("dve", lambda e, bk=bk, mt=mt, pc=pc: e.tensor_copy(out=vx.t[:, mt, pc * 512:(pc + 1) * 512], in_=bk.t[:, :]),
                      reads=[bk], pwrites=[vx])
        for pc in range(4):
            w = wp[pi % 2]; pi += 1
            self.load_w(w, w_xq.ap[:, pc * 512:(pc + 1) * 512], w_xq)
            for f in range(4):
                for th in range(2):
                    bk = kb.bank()
                    self.mm_fm(bk, w, f * 128, h1T, th * 512, 512, 16)
                    kb.op("act", lambda e, bk=bk, fc=pc * 4 + f, th=th: e.activation(out=qxT.t[:, fc, th * 512:(th + 1) * 512], in_=bk.t[:, :],
                                                                                    func=AF.Copy, scale=float(512 ** -0.5)),
                          reads=[bk], pwrites=[qxT])
        kb.phase_end()
        if cfg.get("s2") == "a":
            return
        kb.phase_begin()
        g2o = kb.newgrp()
        oT = kb.sb("oT", [128, 16, 1024], BF16, grp=g2o)
        wxo = kb.sb("wxo", [128, 16, D], BF16, grp=g2o)
        self.load_w(wxo, w_xo.ap, w_xo, split=4)
        pT_all = kb.sb("pT_all", [128, 4, 2, 1024], BF16)
        NS = 3
        p32 = [kb.sb("xp32_%d" % i, [128, 256], F32) for i in range(NS)]
        pn = [kb.sb("xpn_%d" % i, [128, 256], BF16) for i in range(NS)]
        sm = [kb.sb("xsm_%d" % i, [128, 8], F32) for i in range(NS)]
        it = 0
        for tt in range(8):
            for h in range(4):
                s = it % NS
                it += 1
                Z = kb.bank()
                for c in range(4):
                    kb.op("pe", lambda e, c=c, Z=Z: e.matmul(Z.t[:, 0:256], lhsT=qxT.t[:, 4 * h + c, tt * 128:(tt + 1) * 128], rhs=kxT.t[:, 4 * h + c, :],
                                                            start=(c == 0), stop=(c == 3)),
                          reads=[qxT, kxT], writes=[Z] if c == 0 else (), pwrites=() if c == 0 else [Z])
                kb.op("dve", lambda e, Z=Z: e.reduce_max(out=sm[s].t[:, 0:1], in_=Z.t[:, 0:256], axis=AX.X), reads=[Z], writes=[sm[s]])
                kb.op("dve", lambda e: e.tensor_scalar(out=sm[s].t[:, 1:2], in0=sm[s].t[:, 0:1], scalar1=-1.0, scalar2=None, op0=ALU.mult),
                      reads=[sm[s]], pwrites=[sm[s]])
                kb.op("act", lambda e, Z=Z: e.activation(out=p32[s].t[:], in_=Z.t[:, 0:256], func=AF.Exp, bias=sm[s].t[:, 1:2], scale=1.0,
                                                         accum_out=sm[s].t[:, 2:3]),
                      reads=[Z, sm[s]], writes=[p32[s]], pwrites=[sm[s]])
                kb.op("dve", lambda e: e.reciprocal(out=sm[s].t[:, 3:4], in_=sm[s].t[:, 2:3]), reads=[sm[s]], pwrites=[sm[s]])
                kb.op("dve", lambda e: e.tensor_scalar(out=pn[s].t[:], in0=p32[s].t[:], scalar1=sm[s].t[:, 3:4], scalar2=None, op0=ALU.mult),
                      reads=[p32[s], sm[s]], writes=[pn[s]])
                PT = kb.bank()
                ptv = PT.t[:].bitcast(BF16)
                for c in range(2):
                    kb.op("pe", lambda e, c=c: e.transpose(out=ptv[:, c * 128:(c + 1) * 128], in_=pn[s].t[:, c * 128:(c + 1) * 128],
                                                           identity=self.ident_bf.t[:]),
                          reads=[pn[s], self.ident_bf], writes=[PT] if c == 0 else (), pwrites=() if c == 0 else [PT])
                kb.op("act", lambda e, ptv=ptv, PT=PT: e.activation(out=pT_all.t[:, h, :, tt * 128:(tt + 1) * 128],
                                                                    in_=ptv[:, 0:256].rearrange("p (a b) -> p a b", a=2), func=AF.Copy),
                      reads=[PT], pwrites=[pT_all])
        for h in range(4):
            for dc in range(4):
                fc = 4 * h + dc
                for th in range(2):
                    bk = kb.bank()
                    for mc in range(2):
                        kb.op("pe", lambda e, mc=mc, bk=bk: e.matmul(bk.t[:, :], lhsT=vx.t[:, mc, fc * 128:(fc + 1) * 128],
                                                                    rhs=pT_all.t[:, h, mc, th * 512:(th + 1) * 512], start=(mc == 0), stop=(mc == 1)),
                              reads=[vx, pT_all], writes=[bk] if mc == 0 else (), pwrites=() if mc == 0 else [bk])
                    if (fc + th) % 2:
                        kb.op("act", lambda e, bk=bk: e.activation(out=oT.t[:, fc, th * 512:(th + 1) * 512], in_=bk.t[:, :], func=AF.Copy),
                              reads=[bk], pwrites=[oT])
                    else:
                        kb.op("dve", lambda e, bk=bk: e.tensor_copy(out=oT.t[:, fc, th * 512:(th + 1) * 512], in_=bk.t[:, :]),
                              reads=[bk], pwrites=[oT])
        kb.phase_end()
        kb.freegrp(g2)
        if cfg.get("s2") == "b":
            return
        kb.phase_begin()
        gr2, br2 = self.load_rows("ln2_g", "ln2_b")
        self.make_ln_bufs(2)
        h1t = [kb.sb("h1t%d" % i, [128, D], F32) for i in range(2)]
        for i in range(8):
            S = self.ln_sets[i % 2]
            hh = h1t[i % 2]
            kb.dma("sp", hh.t[:], h1.ap[i * 128:(i + 1) * 128, :], h1, hh)
            for nch in range(4):
                bk = kb.bank()
                self.mm_tm(bk, oT, i * 128, wxo, nch * 512, 512, 16)
                kb.op("dve", lambda e, bk=bk, S=S, hh=hh, nch=nch: e.scalar_tensor_tensor(out=S["x"].t[:, nch * 512:(nch + 1) * 512], in0=hh.t[:, nch * 512:(nch + 1) * 512],
                                                                                         scalar=ALPHA, in1=bk.t[:, :], op0=ALU.mult, op1=ALU.add),
                      reads=[hh, bk], writes=[S["x"]] if nch == 0 else (), pwrites=() if nch == 0 else [S["x"]])
            self.ln_core(S, gr2, br2, want_h32=True, want_hb=False)
            kb.dma("sp", self.h2_scr.ap[i * 128:(i + 1) * 128, :], S["x"].t[:], S["x"], self.h2_scr, partial=True)
        kb.phase_end()
        kb.freegrp(g2o)

    def stage3(self):
        kb = self.kb
        cfg = self.cfg
        n_exp = cfg.get("n_exp", NE)
        h2 = self.h2_scr
        w_up, w_down = self.ins["w_up"], self.ins["w_down"]
        g3 = kb.newgrp()
        yacc = [kb.sb("yacc%d" % i, [128, D], F32, grp=g3) for i in range(8)]
        kb.phase_begin()
        g3b = kb.newgrp()
        X_bf = kb.sb("X_bf", [128, 8, D], BF16, grp=g3b)
        mask_all = kb.sb("mask_all", [128, 8, NE], F32, grp=g3b)
        gate_all = kb.sb("gate_all", [128, 8, NE], F32, grp=g3b)
        pos_all = kb.sb("pos_all", [128, 8, NE], F32, grp=g3b)
        mask_bf = kb.sb("mask_bf", [128, 8, NE], BF16)
        wr = kb.sb("wr", [128, 16, NE], F32)
        kb.dma("sp", wr.t[:], self.ins["w_router"].ap.rearrange("(c p) n -> p c n", p=128), self.ins["w_router"], wr)
        brt = kb.sb("brt", [128, NE], F32)
        kb.dma("sp", brt.t[:], self.ins["b_router"].ap.partition_broadcast(128), self.ins["b_router"], brt)
        h2t = [kb.sb("h2t%d" % i, [128, D], F32) for i in range(2)]
        hT32 = [kb.sb("hT32_%d" % i, [128, 16, 128], F32) for i in range(2)]
        lg = [kb.sb("lg%d" % i, [128, NE], F32) for i in range(2)]
        t8 = [kb.sb("t8_%d" % i, [128, 16], F32) for i in range(2)]
        eg = [kb.sb("eg%d" % i, [128, NE], F32) for i in range(2)]
        for tt in range(8):
            u = tt % 2
            hh = h2t[u]
            kb.dma("sp", hh.t[:], h2.ap[tt * 128:(tt + 1) * 128, :], h2, hh)
            kb.op("act", lambda e, hh=hh, tt=tt: e.activation(out=X_bf.t[:, tt, :], in_=hh.t[:], func=AF.Copy), reads=[hh], pwrites=[X_bf])
            kb.op("pool", lambda e, hh=hh, tt=tt: e.tensor_scalar(out=yacc[tt].t[:], in0=hh.t[:], scalar1=ALPHA, scalar2=None, op0=ALU.mult),
                  reads=[hh], writes=[yacc[tt]])
            for q4 in range(4):
                bk = kb.bank()
                for k in range(4):
                    kk = q4 * 4 + k
                    kb.op("pe", lambda e, k=k, kk=kk, bk=bk, hh=hh: e.transpose(out=bk.t[:, k * 128:(k + 1) * 128], in_=hh.t[:, kk * 128:(kk + 1) * 128],
                                                                               identity=self.ident_f.t[:]),
                          reads=[hh, self.ident_f], writes=[bk] if k == 0 else (), pwrites=() if k == 0 else [bk])
                kb.op("dve", lambda e, bk=bk, q4=q4, u=u: e.tensor_copy(out=hT32[u].t[:, q4 * 4:(q4 + 1) * 4, :], in_=bk.t[:, :].rearrange("p (a b) -> p a b", a=4)),
                      reads=[bk], pwrites=[hT32[u]] if q4 else (), writes=() if q4 else [hT32[u]])
            L = kb.bank()
            for k in range(16):
                kb.op("pe", lambda e, k=k, L=L, u=u: e.matmul(L.t[:, 0:NE], lhsT=hT32[u].t[:, k, :], rhs=wr.t[:, k, :], start=(k == 0), stop=(k == 15)),
                      reads=[hT32[u], wr], writes=[L] if k == 0 else (), pwrites=() if k == 0 else [L])
            kb.op("dve", lambda e, L=L, u=u: e.tensor_tensor(out=lg[u].t[:], in0=L.t[:, 0:NE], in1=brt.t[:], op=ALU.add), reads=[L, brt], writes=[lg[u]])
            kb.op("dve", lambda e, u=u: e.max(out=t8[u].t[:, 0:8], in_=lg[u].t[:]), reads=[lg[u]], writes=[t8[u]])
            kb.op("dve", lambda e, u=u, tt=tt: e.tensor_scalar(out=mask_all.t[:, tt, :], in0=lg[u].t[:], scalar1=t8[u].t[:, 3:4], scalar2=None, op0=ALU.is_ge),
                  reads=[lg[u], t8[u]], pwrites=[mask_all])
            kb.op("dve", lambda e, u=u: e.tensor_scalar(out=t8[u].t[:, 8:9], in0=t8[u].t[:, 0:1], scalar1=-1.0, scalar2=None, op0=ALU.mult),
                  reads=[t8[u]], pwrites=[t8[u]])
            kb.op("act", lambda e, u=u: e.activation(out=eg[u].t[:], in_=lg[u].t[:], func=AF.Exp, bias=t8[u].t[:, 8:9], scale=1.0),
                  reads=[lg[u], t8[u]], writes=[eg[u]])
            kb.op("dve", lambda e, u=u, tt=tt: e.tensor_tensor(out=eg[u].t[:], in0=eg[u].t[:], in1=mask_all.t[:, tt, :], op=ALU.mult),
                  reads=[eg[u], mask_all], writes=[eg[u]])
            kb.op("dve", lambda e, u=u: e.reduce_sum(out=t8[u].t[:, 9:10], in_=eg[u].t[:], axis=AX.X), reads=[eg[u]], pwrites=[t8[u]])
            kb.op("dve", lambda e, u=u: e.reciprocal(out=t8[u].t[:, 10:11], in_=t8[u].t[:, 9:10]), reads=[t8[u]], pwrites=[t8[u]])
            kb.op("dve", lambda e, u=u, tt=tt: e.tensor_scalar(out=gate_all.t[:, tt, :], in0=eg[u].t[:], scalar1=t8[u].t[:, 10:11], scalar2=None, op0=ALU.mult),
                  reads=[eg[u], t8[u]], pwrites=[gate_all])
            kb.op("dve", lambda e, tt=tt: e.tensor_copy(out=mask_bf.t[:, tt, :], in_=mask_all.t[:, tt, :]), reads=[mask_all], pwrites=[mask_bf])
            Pp = kb.bank()
            for t2 in range(tt + 1):
                lhs = self.trix if t2 == tt else self.ones_bf
                kb.op("pe", lambda e, t2=t2, lhs=lhs, Pp=Pp: e.matmul(Pp.t[:, 0:NE], lhsT=lhs.t[:], rhs=mask_bf.t[:, t2, :], start=(t2 == 0), stop=(t2 == tt)),
                      reads=[lhs, mask_bf], writes=[Pp] if t2 == 0 else (), pwrites=() if t2 == 0 else [Pp])
            kb.op("dve", lambda e, Pp=Pp, tt=tt: e.tensor_copy(out=pos_all.t[:, tt, :], in_=Pp.t[:, 0:NE]), reads=[Pp], pwrites=[pos_all])
        if cfg.get("dbg") == "route":
            for tt in range(8):
                kb.dma("sp", self.outs["dbg"].ap[tt * 128:(tt + 1) * 128, 0:NE], mask_all.t[:, tt, :], mask_all, self.outs["dbg"], partial=True)
                kb.dma("sp", self.outs["dbg"].ap[tt * 128:(tt + 1) * 128, NE:2 * NE], gate_all.t[:, tt, :], gate_all, self.outs["dbg"], partial=True)
                kb.dma("sp", self.outs["dbg"].ap[tt * 128:(tt + 1) * 128, 2 * NE:3 * NE], pos_all.t[:, tt, :], pos_all, self.outs["dbg"], partial=True)
        kb.phase_end()
        kb.phase_begin()
        C = CAP
        cchunks = [(0, 128), (128, C)] if C > 128 else [(0, C)]
        iota_r = kb.sb("iota_r", [128, C], F32)
        kb.dma("sp", iota_r.t[:], self.ins["c_iota"].ap[:, 0:C], self.ins["c_iota"], iota_r)
        bupc = kb.sb("bupc", [128, NE, 32], F32)
        kb.dma("sp", bupc.t[:], self.ins["bup_col"].ap, self.ins["bup_col"], bupc)
        Sel = [kb.sb("Sel%d" % i, [128, 8, C], BF16) for i in range(1)] * 2
        SelG = [kb.sb("SelG%d" % i, [128, 8, C], BF16) for i in range(1)] * 2
        SelGT = [kb.sb("SelGT%d" % i, [128, 2, 1024], BF16) for i in range(2)]
        xeT = [kb.sb("xeT%d" % i, [128, 16, C], BF16) for i in range(1)] * 2
        actT = [kb.sb("actT%d" % i, [128, 16, C], BF16) for i in range(1)] * 2
        Ye = [kb.sb("Ye%d" % i, [128, 2, D], BF16) for i in range(1)] * 2
        NR = 6
        ring = [kb.sb("wring%d" % i, [128, 16, 256], BF16) for i in range(NR)]
        bdr = [kb.sb("bdr%d" % i, [1, D], BF16) for i in range(2)]
        pieces = []
        for e2 in range(n_exp):
            for pj in range(8):
                pieces.append((e2, "g", pj))
                pieces.append((e2, "l", pj))
            for pd in range(8):
                pieces.append((e2, "d", pd))

        def issue(p):
            if p >= len(pieces):
                return
            e2, kind, j = pieces[p]
            if kind == "g":
                self.load_w(ring[p % NR], w_up.ap[e2, :, j * 256:(j + 1) * 256], w_up)
            elif kind == "l":
                self.load_w(ring[p % NR], w_up.ap[e2, :, D + j * 256:D + (j + 1) * 256], w_up)
            else:
                self.load_w(ring[p % NR], w_down.ap[e2, :, j * 256:(j + 1) * 256], w_down)
        for p in range(NR):
            issue(p)
        pidx = 0
        kb.dma("pool", bdr[0].t[:], self.ins["b_down"].ap[0:1, :], self.ins["b_down"], bdr[0])
        NT = 3
        g32 = [kb.sb("g32_%d" % i, [128, C], F32) for i in range(NT)]
        sgm = [kb.sb("sgm_%d" % i, [128, C], F32) for i in range(NT)]
        l32 = [kb.sb("l32m_%d" % i, [128, C], F32) for i in range(NT)]
        wpi = 0
        wdi = 0
        ti = 0
        for e_ in range(n_exp):
            u = e_ % 2
            for tt in range(8):
                kb.op("dve", lambda e, tt=tt: e.tensor_scalar(out=Sel[u].t[:, tt, :], in0=iota_r.t[:], scalar1=pos_all.t[:, tt, e_:e_ + 1],
                                                              scalar2=mask_all.t[:, tt, e_:e_ + 1], op0=ALU.is_equal, op1=ALU.mult),
                      reads=[iota_r, pos_all, mask_all], pwrites=[Sel[u]] if tt else (), writes=() if tt else [Sel[u]])
                kb.op("dve", lambda e, tt=tt: e.tensor_scalar(out=SelG[u].t[:, tt, :], in0=iota_r.t[:], scalar1=pos_all.t[:, tt, e_:e_ + 1],
                                                               scalar2=gate_all.t[:, tt, e_:e_ + 1], op0=ALU.is_equal, op1=ALU.mult),
                      reads=[iota_r, pos_all, gate_all], pwrites=[SelG[u]] if tt else (), writes=() if tt else [SelG[u]])
            for fc in range(16):
                bk = kb.bank()
                for tt in range(8):
                    kb.op("pe", lambda e, tt=tt, bk=bk, fc=fc: e.matmul(bk.t[:, 0:C], lhsT=X_bf.t[:, tt, fc * 128:(fc + 1) * 128], rhs=Sel[u].t[:, tt, :],
                                                                       start=(tt == 0), stop=(tt == 7)),
                          reads=[X_bf, Sel[u]], writes=[bk] if tt == 0 else (), pwrites=() if tt == 0 else [bk])
                if fc % 2:
                    kb.op("act", lambda e, bk=bk, fc=fc: e.activation(out=xeT[u].t[:, fc, :], in_=bk.t[:, 0:C], func=AF.Copy), reads=[bk],
                          pwrites=[xeT[u]] if fc else (), writes=() if fc else [xeT[u]])
                else:
                    kb.op("dve", lambda e, bk=bk, fc=fc: e.tensor_copy(out=xeT[u].t[:, fc, :], in_=bk.t[:, 0:C]), reads=[bk],
                          pwrites=[xeT[u]] if fc else (), writes=() if fc else [xeT[u]])
            for ci, (c0, c1) in enumerate(cchunks):
                m = c1 - c0
                bk = kb.bank()
                bv = bk.t[:].bitcast(BF16)
                for tt in range(8):
                    kb.op("pe", lambda e, tt=tt, bv=bv: e.transpose(out=bv[0:m, tt * 128:(tt + 1) * 128], in_=SelG[u].t[:, tt, c0:c1], identity=self.ident_bf.t[:]),
                          reads=[SelG[u], self.ident_bf], writes=[bk] if tt == 0 else (), pwrites=() if tt == 0 else [bk])
                kb.op("act", lambda e, bv=bv, ci=ci, m=m: e.activation(out=SelGT[u].t[0:m, ci, :], in_=bv[0:m, 0:1024], func=AF.Copy), reads=[bk],
                      pwrites=[SelGT[u]] if ci else (), writes=() if ci else [SelGT[u]])
            if e_ + 1 < n_exp:
                kb.dma("pool", bdr[(e_ + 1) % 2].t[:], self.ins["b_down"].ap[e_ + 1:e_ + 2, :], self.ins["b_down"], bdr[(e_ + 1) % 2])
            for pj in range(8):
                wgt, wlt = ring[pidx % NR], ring[(pidx + 1) % NR]
                for f in range(2):
                    fc = pj * 2 + f
                    s3 = ti % NT
                    ti += 1
                    bg, bl = kb.bank(), kb.bank()
                    self.mm_fm(bg, wgt, f * 128, xeT[u], 0, C, 16)
                    self.mm_fm(bl, wlt, f * 128, xeT[u], 0, C, 16)
                    kb.op("dve", lambda e, bg=bg, fc=fc, s3=s3: e.tensor_scalar(out=g32[s3].t[:], in0=bg.t[:, 0:C], scalar1=bupc.t[:, e_, fc:fc + 1], scalar2=7.0,
                                                                               op0=ALU.add, op1=ALU.min),
                          reads=[bg, bupc], writes=[g32[s3]])
                    kb.op("act", lambda e, s3=s3: e.activation(out=sgm[s3].t[:], in_=g32[s3].t[:], func=AF.Sigmoid, scale=1.702), reads=[g32[s3]], writes=[sgm[s3]])
                    kb.op("dve", lambda e, bl=bl, fc=fc, s3=s3: e.tensor_scalar(out=l32[s3].t[:], in0=bl.t[:, 0:C], scalar1=bupc.t[:, e_, 16 + fc:17 + fc], scalar2=7.0,
                                                                               op0=ALU.add, op1=ALU.min),
                          reads=[bl, bupc], writes=[l32[s3]])
                    kb.op("dve", lambda e, s3=s3: e.tensor_scalar(out=l32[s3].t[:], in0=l32[s3].t[:], scalar1=-7.0, scalar2=1.0, op0=ALU.max, op1=ALU.add),
                          reads=[l32[s3]], writes=[l32[s3]])
                    kb.op("dve", lambda e, s3=s3: e.tensor_tensor(out=g32[s3].t[:], in0=g32[s3].t[:], in1=sgm[s3].t[:], op=ALU.mult),
                          reads=[g32[s3], sgm[s3]], writes=[g32[s3]])
                    kb.op("dve", lambda e, s3=s3, fc=fc: e.tensor_tensor(out=actT[u].t[:, fc, :], in0=g32[s3].t[:], in1=l32[s3].t[:], op=ALU.mult),
                          reads=[g32[s3], l32[s3]], pwrites=[actT[u]] if fc else (), writes=() if fc else [actT[u]])
                issue(pidx + NR)
                issue(pidx + 1 + NR)
                pidx += 2
            bd = bdr[u]
            for pd in range(8):
                wdt = ring[pidx % NR]
                for ci, (c0, c1) in enumerate(cchunks):
                    m = c1 - c0
                    bk = kb.bank()
                    kb.op("pe", lambda e, bk=bk, m=m, pd=pd: e.matmul(bk.t[0:m, 0:256], lhsT=self.ones_bf.t[0:1, 0:m], rhs=bd.t[0:1, pd * 256:(pd + 1) * 256],
                                                                     start=True, stop=False),
                          reads=[self.ones_bf, bd], writes=[bk])
                    for k in range(16):
                        kb.op("pe", lambda e, bk=bk, m=m, k=k, c0=c0, c1=c1, wdt=wdt: e.matmul(bk.t[0:m, 0:256], lhsT=actT[u].t[:, k, c0:c1], rhs=wdt.t[:, k, :],
                                                                                              start=False, stop=(k == 15)),
                              reads=[actT[u], wdt], pwrites=[bk])
                    first = (pd == 0 and ci == 0)
                    if ci == 0:
                        kb.op("act", lambda e, bk=bk, m=m, ci=ci, pd=pd: e.activation(out=Ye[u].t[0:m, ci, pd * 256:(pd + 1) * 256], in_=bk.t[0:m, 0:256], func=AF.Copy),
                              reads=[bk], pwrites=() if first else [Ye[u]], writes=[Ye[u]] if first else ())
                    else:
                        kb.op("dve", lambda e, bk=bk, m=m, ci=ci, pd=pd: e.tensor_copy(out=Ye[u].t[0:m, ci, pd * 256:(pd + 1) * 256], in_=bk.t[0:m, 0:256]),
                              reads=[bk], pwrites=[Ye[u]])
                issue(pidx + NR)
                pidx += 1
            for tt in range(8):
                for nch in range(4):
                    bk = kb.bank()
                    for ci, (c0, c1) in enumerate(cchunks):
                        m = c1 - c0
                        kb.op("pe", lambda e, bk=bk, m=m, ci=ci, tt=tt, nch=nch: e.matmul(bk.t[:, :], lhsT=SelGT[u].t[0:m, ci, tt * 128:(tt + 1) * 128],
                                                                                         rhs=Ye[u].t[0:m, ci, nch * 512:(nch + 1) * 512],
                                                                                         start=(ci == 0), stop=(ci == len(cchunks) - 1)),
                              reads=[SelGT[u], Ye[u]], writes=[bk] if ci == 0 else (), pwrites=() if ci == 0 else [bk])
                    kb.op("dve", lambda e, bk=bk, tt=tt, nch=nch: e.tensor_tensor(out=yacc[tt].t[:, nch * 512:(nch + 1) * 512], in0=yacc[tt].t[:, nch * 512:(nch + 1) * 512],
                                                                                 in1=bk.t[:, :], op=ALU.add),
                          reads=[bk, yacc[tt]], writes=[yacc[tt]])
        kb.phase_end()
        kb.freegrp(g3b)
        kb.phase_begin()
        gr3, br3 = self.load_rows("ln3_g", "ln3_b")
        self.make_ln_bufs(2)
        for tt in range(8):
            S = dict(self.ln_sets[tt % 2])
            S["x"] = yacc[tt]
            self.ln_core(S, gr3, br3, want_h32=True, want_hb=False)
            kb.dma("sp", self.out_buf.ap[tt * 128:(tt + 1) * 128, :], yacc[tt].t[:], yacc[tt], self.out_buf, partial=True)
        kb.phase_end()
        kb.freegrp(g3)

    def build(self):
        cfg = self.cfg
        nc = self.nc
        with ExitStack() as es:
            kb = KB(nc, es)
            self.kb = kb
            if cfg.get("start", 1) <= 1:
                self.din("x_own", [1024, D]); self.din("x_prev", [1024, D]); self.din("x_ctx", [4096, D])
                self.din("w_in", [D, 8448])
                self.din("ln_in_g", [1, D]); self.din("ln_in_b", [1, D]); self.din("ln1_g", [1, D]); self.din("ln1_b", [1, D])
                self.din("sbmask", [128, 4, 128]); self.din("swab", [128, 16, 256]); self.din("pm0", [128, 256]); self.din("attn_sinks", [1, 16])
                self.din("w_a_out", [1024, D]); self.din("w_b_out", [1024, D]); self.din("w_mix_out", [D, D])
            self.din("bcol_in", [128, 66]); self.din("bcol_qa_in", [128, 8]); self.din("b_row_in", [1, 1152])
            self.kT_scr = self.dscr("kT_scr", [8, 128, 4096], BF16)
            self.V_scr = self.dscr("V_scr", [32, 128, 1024], BF16)
            self.h0_scr = self.dscr("h0_scr", [1024, D], F32)
            stop = cfg.get("stop", 3)
            start = cfg.get("start", 1)
            if start <= 2 <= stop:
                self.din("mem", [256, D]); self.din("w_xq", [D, D]); self.din("w_xkv", [D, 2 * D]); self.din("w_xo", [D, D])
                self.din("ln2_g", [1, D]); self.din("ln2_b", [1, D])
            if start <= 3 <= stop:
                ne = cfg.get("n_exp", NE)
                self.din("w_router", [D, NE]); self.din("b_router", [1, NE]); self.din("w_up", [ne, D, 2 * D]); self.din("w_down", [ne, D, D])
                self.din("bup_col", [128, NE, 32]); self.din("b_down", [NE, D]); self.din("ln3_g", [1, D]); self.din("ln3_b", [1, D])
                self.din("c_iota", [128, 256])
            mk = lambda nm, st: (self.dout("out", [1024, D]) if stop == st else self.dscr(nm, [1024, D], F32))
            self.h1_scr = self.din("h1_in", [1024, D]) if start == 2 else mk("h1_scr", 1)
            self.h2_scr = self.din("h2_in", [1024, D]) if start == 3 else mk("h2_scr", 2)
            self.out_buf = mk("out_scr", 3)
            if cfg.get("dbg"):
                self.dout("dbg", [1024, D])
            self.load_consts()
            self.bcol = kb.sb("bcol", [128, 66], F32, glob=True)
            kb.dma("sp", self.bcol.t[:], self.ins["bcol_in"].ap, self.ins["bcol_in"], self.bcol)
            self.bcol_qa = kb.sb("bcol_qa", [128, 8], F32, glob=True)
            kb.dma("sp", self.bcol_qa.t[:], self.ins["bcol_qa_in"].ap, self.ins["bcol_qa_in"], self.bcol_qa)
            self.brow = kb.sb("brow", [1, 1152], BF16, glob=True)
            kb.dma("pool", self.brow.t[:], self.ins["b_row_in"].ap, self.ins["b_row_in"], self.brow)
            kb.barrier()
            if start <= 1 <= stop:
                self.stage1()
            if start <= 2 <= stop:
                self.stage2()
            if start <= 3 <= stop:
                self.stage3()
            kb.barrier()
        return nc


def t5_bucket_np(rel):
    half, max_exact = 16, 8
    base = np.where(rel > 0, half, 0)
    n = np.abs(rel)
    nf = np.maximum(n, 1).astype(np.float32)
    large = max_exact + (np.log(nf / max_exact) / np.float32(np.log(128 / max_exact)) * (half - max_exact)).astype(np.int32)
    large = np.minimum(large, half - 1)
    return base + np.where(n < max_exact, n, large)


def host_consts():
    ident = np.eye(128, dtype=np.float32)
    sp, s = np.meshgrid(np.arange(128), np.arange(128), indexing="ij")
    negtri = -(sp >= s).astype(np.float32)
    trix = (sp < s).astype(np.float32)
    return dict(c_ident=ident, c_negtri=negtri, c_trix=trix)


def prep_core_inputs(c, inp):
    b, cp = c // 4, c % 4
    x = inp["x"][b]
    blocks = [4 * i + cp for i in range(8)]
    x_own = np.concatenate([x[q * 128:(q + 1) * 128] for q in blocks], 0)
    x_prev = np.concatenate([x[(q - 1) * 128:q * 128] if q > 0 else np.zeros((128, D), np.float32) for q in blocks], 0)
    d = dict(x_own=np.ascontiguousarray(x_own), x_prev=np.ascontiguousarray(x_prev), x_ctx=np.ascontiguousarray(x))
    if "mem" in inp:
        d["mem"] = inp["mem"][b]
    s_, q_ = np.meshgrid(np.arange(128), np.arange(128), indexing="ij")
    m = np.full((128, 4, 128), NEG, np.float32)
    for r in range(4):
        if r < cp:
            m[:, r, :] = 0.0
        elif r == cp:
            m[:, r, :] = np.where(s_ < q_, 0.0, NEG)
    d["sbmask"] = m
    qi = np.arange(128)[:, None]
    kj = np.arange(256)[None, :]
    bucket = t5_bucket_np(kj - 128 - qi)
    bias = inp["rel_bias"][bucket]
    dchunk = (kj // 64 - 2) - qi // 64
    band = (dchunk <= 0) & (dchunk >= -2)
    tab = np.where(band[:, :, None], bias, np.float32(NEG)).astype(np.float32)
    d["swab"] = np.ascontiguousarray(np.transpose(tab, (0, 2, 1)))
    pm0 = np.zeros((128, 256), np.float32)
    if cp == 0:
        pm0[:, :128] = NEG
    d["pm0"] = pm0
    return d


def common_inputs(inp):
    b_in = inp["b_in"][0]
    bcol = np.ascontiguousarray(b_in.reshape(66, 128).T)
    qa = b_in[0:1024].reshape(2, 8, 64)
    bcol_qa = np.ascontiguousarray(np.transpose(qa, (1, 0, 2)).reshape(8, 128).T)
    d = dict(w_in=inp["w_in"][0], bcol_in=bcol, bcol_qa_in=bcol_qa, b_row_in=np.concatenate([b_in[1152:1280], b_in[3328:4352]])[None, :],
             ln_in_g=inp["ln_in_g"][None, :], ln_in_b=inp["ln_in_b"][None, :], ln1_g=inp["ln1_g"], ln1_b=inp["ln1_b"],
             attn_sinks=inp["attn_sinks"], w_a_out=inp["w_a_out"][0], w_b_out=inp["w_b_out"][0], w_mix_out=inp["w_mix_out"][0])
    d.update(host_consts())
    d["c_iota"] = np.tile(np.arange(256, dtype=np.float32)[None, :], (128, 1))
    for k in ("w_xq", "w_xkv", "w_xo", "w_router", "w_up", "w_down"):
        if k in inp:
            d[k] = inp[k][0]
    for k in ("ln2_g", "ln2_b", "ln3_g", "ln3_b", "b_router"):
        if k in inp:
            d[k] = inp[k]
    if "b_up" in inp:
        d["bup_col"] = np.ascontiguousarray(np.transpose(inp["b_up"][0].reshape(NE, 32, 128), (2, 0, 1)))
        d["b_down"] = inp["b_down"][0]
    return d


def run(inp, cfg):
    prog = Prog(cfg)
    nc = prog.build()
    com = common_inputs(inp)
    in_maps = []
    for c in range(8):
        d = dict(com)
        d.update(prep_core_inputs(c, inp))
        in_maps.append({k: np.ascontiguousarray(v, dtype=np.float32) for k, v in d.items() if k in prog.ins})
    res = run_bass_kernel_spmd(nc, in_maps, core_ids=list(range(8)))
    return res.results


def assemble(results, key="out"):
    out = np.zeros((2, 4096, D), np.float32)
    for c in range(8):
        b, cp = c // 4, c % 4
        r = results[c][key]
        for i in range(8):
            q = 4 * i + cp
            out[b, q * 128:(q + 1) * 128] = r[i * 128:(i + 1) * 128]
    return out


def kernel(**inputs):
    inp = {k: np.asarray(v) for k, v in inputs.items()}
    results = run(inp, dict())
    return assemble(results)
```
